# Optimizing a Trainium2 kernel written in Bass

```python
import jax, jax.numpy as jnp
from jax import lax
import numpy as np

D_MODEL = 2048
BATCH = 2
SEQ = 8192
DEPTH = 1

D_LRU = 2048
LRU_HEADS = 16
LRU_BLOCK = D_LRU // LRU_HEADS
CONV_WIDTH = 4
LRU_C = 8.0
LRU_A_MIN = 0.9
LRU_A_MAX = 0.999
RET_HEADS = 8
RET_DK = 256
RET_DV = 512
D_QK = RET_HEADS * RET_DK
D_RV = RET_HEADS * RET_DV
RET_CHUNK = 128
ROPE_THETA = 10000.0
N_EXPERTS = 64
TOP_K = 8
N_GROUPS = 8
TOPK_GROUPS = 4
D_EXPERT = 512
D_SHARED = 512
ROUTED_SCALE = 2.5
EXPERT_BLOCK = 128
DN_ALPHA = (2.0 * DEPTH) ** 0.25
DN_BETA = (8.0 * DEPTH) ** -0.25
LN_EPS = 1e-5
PROJ_SIZES = (D_LRU, D_LRU, D_QK, D_QK, D_RV, D_RV, D_MODEL, D_MODEL)
N_PROJ = sum(PROJ_SIZES)

kernel_name = "hybrid_rglru_retention_moe_deepnorm"


def layer_norm(x, g, b):
    xf = x.astype(jnp.float32)
    mu = xf.mean(-1, keepdims=True)
    var = jnp.square(xf - mu).mean(-1, keepdims=True)
    return ((xf - mu) * lax.rsqrt(var + LN_EPS) * g + b).astype(x.dtype)


def causal_conv(x, w, b):
    W = w.shape[0]
    S = x.shape[1]
    xp = jnp.pad(x, ((0, 0), (W - 1, 0), (0, 0)))
    out = b
    for t in range(W):
        out = out + xp[:, t:t + S] * w[t]
    return out


def _lin_combine(c1, c2):
    a1, b1 = c1
    a2, b2 = c2
    return a1 * a2, a2 * b1 + b2


def rg_lru(xa, wa, ba, wi, bi, lam):
    B, S, C = xa.shape
    xf = xa.astype(jnp.float32)
    xh = xf.reshape(B, S, LRU_HEADS, LRU_BLOCK)
    r = jax.nn.sigmoid(jnp.einsum('bshi,hij->bshj', xh, wa.astype(jnp.float32)) + ba).reshape(B, S, C)
    i = jax.nn.sigmoid(jnp.einsum('bshi,hij->bshj', xh, wi.astype(jnp.float32)) + bi).reshape(B, S, C)
    log_a = -LRU_C * r * jax.nn.softplus(-lam.astype(jnp.float32))
    a = jnp.exp(log_a)
    mult = jnp.sqrt(-jnp.expm1(2.0 * log_a))
    first = (jnp.arange(S) == 0)[None, :, None]
    mult = jnp.where(first, 1.0, mult)
    _, h = lax.associative_scan(_lin_combine, (a, mult * i * xf), axis=1)
    return h.astype(xa.dtype)


def rope(t, pos):
    half = t.shape[-1] // 2
    freq = ROPE_THETA ** (-jnp.arange(half, dtype=jnp.float32) / half)
    ang = pos.astype(jnp.float32)[:, None] * freq
    cos = jnp.cos(ang)[None, :, None, :]
    sin = jnp.sin(ang)[None, :, None, :]
    t1 = t[..., :half].astype(jnp.float32)
    t2 = t[..., half:].astype(jnp.float32)
    return jnp.concatenate([t1 * cos - t2 * sin, t1 * sin + t2 * cos], -1)


def chunk_retention(q, k, v):
    B, S, H, dk = q.shape
    dv = v.shape[-1]
    C = RET_CHUNK
    N = S // C
    log_g = jnp.log1p(-jnp.exp2(-5.0 - jnp.arange(H, dtype=jnp.float32)))
    idx = jnp.arange(C, dtype=jnp.float32)
    diff = idx[:, None] - idx[None, :]
    dmask = jnp.where(diff >= 0, jnp.exp(jnp.maximum(diff, 0.0)[None] * log_g[:, None, None]), 0.0)
    xi = jnp.exp((idx[None] + 1.0) * log_g[:, None])
    zeta = jnp.exp((C - 1.0 - idx[None]) * log_g[:, None])
    g_c = jnp.exp(C * log_g)

    def to_chunks(t):
        return t.reshape(B, N, C, H, t.shape[-1]).transpose(1, 0, 3, 2, 4)

    def step(state, blk):
        qc, kc, vc = blk
        s = jnp.einsum('bhid,bhjd->bhij', qc, kc) * dmask
        o = jnp.einsum('bhij,bhje->bhie', s, vc)
        o = o + jnp.einsum('bhid,bhde->bhie', qc, state) * xi[None, :, :, None]
        state = g_c[None, :, None, None] * state + jnp.einsum('bhjd,bhje->bhde', kc * zeta[None, :, :, None], vc)
        return state, o

    state0 = jnp.zeros((B, H, dk, dv), jnp.float32)
    _, o = lax.scan(step, state0, (to_chunks(q), to_chunks(k), to_chunks(v.astype(jnp.float32))))
    return o.transpose(1, 0, 3, 2, 4).reshape(B, S, H, dv)


def head_group_norm(o):
    mu = o.mean(-1, keepdims=True)
    var = jnp.square(o - mu).mean(-1, keepdims=True)
    return (o - mu) * lax.rsqrt(var + LN_EPS)


def moe_ffn(xt, w_router, router_bias, wg, wu, wd, wgs, wus, wds):
    T, D = xt.shape
    E = N_EXPERTS
    f32 = jnp.float32
    scores = jax.nn.sigmoid((xt @ w_router).astype(f32))
    biased = scores + router_bias.astype(f32)
    grp = lax.top_k(biased.reshape(T, N_GROUPS, E // N_GROUPS), 2)[0].sum(-1)
    _, gidx = lax.top_k(grp, TOPK_GROUPS)
    gmask = jax.nn.one_hot(gidx, N_GROUPS, dtype=f32).sum(1) > 0
    emask = jnp.repeat(gmask, E // N_GROUPS, axis=1)
    _, eidx = lax.top_k(jnp.where(emask, biased, -jnp.inf), TOP_K)
    wsel = jnp.take_along_axis(scores, eidx, axis=1)
    wsel = wsel / wsel.sum(-1, keepdims=True) * ROUTED_SCALE

    A = T * TOP_K
    flat_e = eidx.reshape(-1)
    flat_w = wsel.reshape(-1)
    order = jnp.argsort(flat_e)
    sorted_e = flat_e[order]
    sorted_tok = (order // TOP_K).astype(jnp.int32)
    sorted_w = flat_w[order]
    counts = jnp.zeros((E,), jnp.int32).at[flat_e].add(1)
    padded = (counts + EXPERT_BLOCK - 1) // EXPERT_BLOCK * EXPERT_BLOCK
    pad_end = jnp.cumsum(padded)
    pad_start = pad_end - padded
    start = jnp.cumsum(counts) - counts
    dest = pad_start[sorted_e] + jnp.arange(A, dtype=jnp.int32) - start[sorted_e]
    NB = (A + E * (EXPERT_BLOCK - 1) + EXPERT_BLOCK - 1) // EXPERT_BLOCK
    R = NB * EXPERT_BLOCK
    row_tok = jnp.full((R,), T, jnp.int32).at[dest].set(sorted_tok)
    row_w = jnp.zeros((R,), f32).at[dest].set(sorted_w)
    block_e = jnp.minimum(jnp.searchsorted(pad_end, jnp.arange(NB, dtype=jnp.int32) * EXPERT_BLOCK, side='right'), E - 1)
    xpad = jnp.concatenate([xt, jnp.zeros((1, D), xt.dtype)], 0)

    def step(acc, blk):
        tok, wr, e = blk
        xb = xpad[tok]
        hb = jax.nn.silu(xb @ wg[e]) * (xb @ wu[e])
        yb = (hb @ wd[e]).astype(f32) * wr[:, None]
        return acc.at[tok].add(yb), None

    acc, _ = lax.scan(step, jnp.zeros((T + 1, D), f32),
                      (row_tok.reshape(NB, EXPERT_BLOCK), row_w.reshape(NB, EXPERT_BLOCK), block_e))
    routed = acc[:T].astype(xt.dtype)
    shared = (jax.nn.silu(xt @ wgs) * (xt @ wus)) @ wds
    return routed + shared


def setup_inputs(seed: int = 0) -> dict:
    key = jax.random.key(seed)
    ks = jax.random.split(key, 26)
    L, D, E = DEPTH, D_MODEL, N_EXPERTS
    f32 = jnp.float32

    def nrm(k, shape, scale):
        return jax.random.normal(k, shape, f32) * scale

    a8 = jax.random.uniform(ks[9], (L, D_LRU), f32, LRU_A_MIN, LRU_A_MAX)
    a = a8 ** (1.0 / LRU_C)
    return {
        "x": nrm(ks[0], (BATCH, SEQ, D), 1.0),
        "w_in": nrm(ks[1], (L, D, N_PROJ), D ** -0.5),
        "conv_w": nrm(ks[2], (L, CONV_WIDTH, D_LRU), CONV_WIDTH ** -0.5),
        "conv_b": nrm(ks[3], (L, D_LRU), 0.02),
        "lru_wa": nrm(ks[4], (L, LRU_HEADS, LRU_BLOCK, LRU_BLOCK), LRU_BLOCK ** -0.5),
        "lru_ba": nrm(ks[5], (L, LRU_HEADS, LRU_BLOCK), 0.02),
        "lru_wi": nrm(ks[6], (L, LRU_HEADS, LRU_BLOCK, LRU_BLOCK), LRU_BLOCK ** -0.5),
        "lru_bi": nrm(ks[7], (L, LRU_HEADS, LRU_BLOCK), 0.02),
        "lru_lambda": jnp.log(a) - jnp.log1p(-a),
        "ret_gn_gain": 1.0 + nrm(ks[8], (L, D_RV), 0.02),
        "w_lru_out": nrm(ks[10], (L, D_LRU, D), D_LRU ** -0.5 * DN_BETA),
        "w_ret_out": nrm(ks[11], (L, D_RV, D), D_RV ** -0.5 * DN_BETA),
        "b_gate": nrm(ks[12], (L, 2 * D), 0.02),
        "w_o": nrm(ks[13], (L, D, D), D ** -0.5 * DN_BETA),
        "ln1_g": 1.0 + nrm(ks[14], (L, D), 0.02),
        "ln1_b": nrm(ks[15], (L, D), 0.02),
        "w_router": nrm(ks[16], (L, D, E), D ** -0.5),
        "router_bias": nrm(ks[17], (L, E), 0.01),
        "w_gate_e": nrm(ks[18], (L, E, D, D_EXPERT), D ** -0.5),
        "w_up_e": nrm(ks[19], (L, E, D, D_EXPERT), D ** -0.5),
        "w_down_e": nrm(ks[20], (L, E, D_EXPERT, D), D_EXPERT ** -0.5 * DN_BETA),
        "w_gate_s": nrm(ks[21], (L, D, D_SHARED), D ** -0.5),
        "w_up_s": nrm(ks[22], (L, D, D_SHARED), D ** -0.5),
        "w_down_s": nrm(ks[23], (L, D_SHARED, D), D_SHARED ** -0.5 * DN_BETA),
        "ln2_g": 1.0 + nrm(ks[24], (L, D), 0.02),
        "ln2_b": nrm(ks[25], (L, D), 0.02),
    }


def reference(x, w_in, conv_w, conv_b, lru_wa, lru_ba, lru_wi, lru_bi, lru_lambda, ret_gn_gain,
              w_lru_out, w_ret_out, b_gate, w_o, ln1_g, ln1_b, w_router, router_bias,
              w_gate_e, w_up_e, w_down_e, w_gate_s, w_up_s, w_down_s, ln2_g, ln2_b):
    B, S, D = x.shape
    pos = jnp.arange(S, dtype=jnp.int32)
    offs = [0]
    for n in PROJ_SIZES:
        offs.append(offs[-1] + n)
    for l in range(DEPTH):
        proj = x @ w_in[l]
        lru_x, lru_y, q, k, v, g, gl_a, gl_b = [proj[..., offs[j]:offs[j + 1]] for j in range(len(PROJ_SIZES))]

        xa = causal_conv(lru_x, conv_w[l], conv_b[l])
        ha = rg_lru(xa, lru_wa[l], lru_ba[l], lru_wi[l], lru_bi[l], lru_lambda[l])
        ya = (ha * jax.nn.gelu(lru_y, approximate=True)) @ w_lru_out[l]

        qh = rope(q.reshape(B, S, RET_HEADS, RET_DK), pos)
        kh = rope(k.reshape(B, S, RET_HEADS, RET_DK), pos) * (RET_DK ** -0.5)
        vh = v.reshape(B, S, RET_HEADS, RET_DV)
        oh = head_group_norm(chunk_retention(qh, kh, vh)).reshape(B, S, D_RV) * ret_gn_gain[l]
        yb = (jax.nn.silu(g) * oh.astype(x.dtype)) @ w_ret_out[l]

        mixed = jax.nn.sigmoid(gl_a + b_gate[l, :D]) * ya + jax.nn.sigmoid(gl_b + b_gate[l, D:]) * yb
        h = layer_norm(DN_ALPHA * x + mixed @ w_o[l], ln1_g[l], ln1_b[l])

        f = moe_ffn(h.reshape(B * S, D), w_router[l], router_bias[l], w_gate_e[l], w_up_e[l], w_down_e[l],
                    w_gate_s[l], w_up_s[l], w_down_s[l]).reshape(B, S, D)
        x = layer_norm(DN_ALPHA * h + f, ln2_g[l], ln2_b[l])
    return x
```

```python
import os
import types
import numpy as np
from contextlib import ExitStack
import concourse.bass as bass
import concourse.mybir as mybir
from concourse.bass_utils import run_bass_kernel_spmd

F32 = mybir.dt.float32
BF16 = mybir.dt.bfloat16
AF = mybir.ActivationFunctionType
ALU = mybir.AluOpType
AX = mybir.AxisListType

D = 2048
SEQ = 8192
NB = 2048
TB = 512
NE = 64
DN_ALPHA = 2.0 ** 0.25
LN_EPS = 1e-5
GAMMA = [1.0 - 2.0 ** (-5.0 - h) for h in range(8)]
_LAST = []
N_EXP = int(os.environ.get("MK_NEXP", "65"))
_PH = os.environ.get("MK_PH", "0,1,1b,2").split(",")
_BLKS = [int(v) for v in os.environ.get("MK_BLKS", ",".join(str(i) for i in range(16))).split(",")]
_LH = int(os.environ.get("MK_LH", "16"))
_RH = int(os.environ.get("MK_RH", "8"))


def freeze(fn):
    if fn.__closure__ is None:
        return fn
    cells = []
    for c in fn.__closure__:
        try:
            cells.append(types.CellType(c.cell_contents))
        except ValueError:
            cells.append(c)
    return types.FunctionType(fn.__code__, fn.__globals__, fn.__name__, fn.__defaults__, tuple(cells))


class Src:
    def __init__(self, sem, step, name):
        self.sem, self.step, self.count, self.name = sem, step, 0, name


class Buf:
    __slots__ = ("w", "r", "name")

    def __init__(self, name=""):
        self.w, self.r, self.name = None, {}, name


class Sched:
    ENGS = ("pe", "act", "dve", "pool", "sp")

    def __init__(self, nc, stack):
        self.nc, self.stack = nc, stack
        self.streams = {e: [] for e in self.ENGS}
        self.src = {}
        for e in self.ENGS:
            self.src[e] = Src(stack.enter_context(nc.semaphore("s_" + e)), 1, e)
        self.seen = {e: {} for e in self.ENGS}
        self.dma_srcs = []
        self.n_inst = 0

    def dma_src(self, name):
        s = Src(self.stack.enter_context(self.nc.semaphore("d_" + name)), 16, name)
        self.dma_srcs.append(s)
        return s

    def _waits(self, eng, reads, writes):
        need = {}
        me = self.src[eng]

        def add(ev):
            if ev is None:
                return
            s, c = ev
            if s is me and eng == "pe":
                return
            if need.get(s, 0) < c:
                need[s] = c

        for b in reads:
            add(b.w)
        for b in writes:
            add(b.w)
            for s, c in b.r.items():
                if s is not me:
                    add((s, c))
        out = []
        seen = self.seen[eng]
        for s, c in need.items():
            if seen.get(s, 0) < c:
                seen[s] = c
                out.append((s, c))
        return out

    def op(self, eng, fn, reads=(), writes=()):
        self.group(eng, [fn], reads, writes)

    def group(self, eng, fns, reads=(), writes=()):
        waits = self._waits(eng, reads, writes)
        me = self.src[eng]
        me.count += 1
        cnt = me.count
        st = self.streams[eng]
        for i, fn in enumerate(fns):
            last = i == len(fns) - 1
            st.append((waits if i == 0 else (), freeze(fn), me if last else None, 1))
        for b in reads:
            b.r[me] = cnt
        for b in writes:
            b.w = (me, cnt)
            b.r = {}
        self.n_inst += len(fns)

    def dma(self, q, fn, dsrc, reads=(), writes=()):
        waits = self._waits(q, reads, writes)
        dsrc.count += 16
        cnt = dsrc.count
        self.streams[q].append((waits, freeze(fn), dsrc, 16))
        for b in reads:
            b.r[dsrc] = cnt
        for b in writes:
            b.w = (dsrc, cnt)
            b.r = {}
        self.n_inst += 1

    def barrier(self):
        allsrc = [self.src[e] for e in self.ENGS] + self.dma_srcs
        for e in self.ENGS:
            waits = []
            for s in allsrc:
                if s is self.src[e] or s.count == 0:
                    continue
                if self.seen[e].get(s, 0) < s.count:
                    self.seen[e][s] = s.count
                    waits.append((s, s.count))
            if waits:
                self.streams[e].append((waits, None, None, 0))

    def finish(self):
        nc = self.nc
        eh = {"pe": "tensor", "act": "scalar", "dve": "vector", "pool": "gpsimd", "sp": "sync"}
        self.barrier()
        with nc.Block() as block:
            for e in self.ENGS:
                stream = self.streams[e]

                def body(engine, stream=stream):
                    for waits, fn, src, inc in stream:
                        for s, c in waits:
                            engine.wait_ge(s.sem, c)
                        if fn is not None:
                            ins = fn(engine)
                            if src is not None:
                                ins.then_inc(src.sem, inc)

                getattr(block, eh[e])(body)


class Ring:
    def __init__(self, nc, st, name, n, shape, dt, psum=False):
        self.t, self.b, self.i = [], [], 0
        for k in range(n):
            nm = "%s%d" % (name, k)
            if psum:
                self.t.append(st.enter_context(nc.psum_tensor(nm, shape, dt)))
            else:
                self.t.append(st.enter_context(nc.sbuf_tensor(nm, shape, dt)))
            self.b.append(Buf(nm))

    def next(self):
        k = self.i % len(self.t)
        self.i += 1
        return self.t[k], self.b[k]


class DRing:
    def __init__(self, S, nc, st, name, n, shape, dt):
        self.r = Ring(nc, st, name, n, shape, dt)
        self.s = [S.dma_src("%s%d" % (name, k)) for k in range(n)]

    def next(self):
        k = self.r.i % len(self.s)
        t, b = self.r.next()
        return t, b, self.s[k]


def build():
    nc = bass.Bass("TRN2", target_bir_lowering=False)
    ein = lambda n, s, d=F32: nc.dram_tensor(n, list(s), d, kind="ExternalInput").ap()
    scr = lambda n, s, d=BF16: nc.dram_tensor(n, list(s), d).ap()
    xT = ein("xT", [16, 128, 16, TB])
    xres = ein("xres", [NB, D])
    w_in_l = ein("w_in_l", [16, 128, 2, 16, 128])
    w_in_r = ein("w_in_r", [8, 6, 128, 16, 256])
    w3 = ein("w3", [16, 128, 80, 128])
    wo = ein("wo", [4, 128, 16, 512])
    wge = ein("wge", [N_EXP, 128, 16, 512])
    wue = ein("wue", [N_EXP, 128, 16, 512])
    wde = ein("wde", [N_EXP, 128, 4, 2048])
    wr = ein("wr", [128, 16, 64])
    lrup = ein("lrup", [128, 16, 8])
    wai = ein("wai", [128, 16, 2, 128])
    bg = ein("bg", [128, 2, 16])
    cosT = ein("cosT", [16, 128, TB])
    sinT = ein("sinT", [16, 128, TB])
    flags = ein("flags", [128, 2, 16])
    dmaskT = ein("dmaskT", [128, 8, 128])
    xizeta = ein("xizeta", [128, 2, 8])
    gain_bc = ein("gain_bc", [128, 4096])
    lnp = ein("lnp", [128, 4, D])
    rb_bc = ein("rb_bc", [128, 64])
    ident = ein("ident", [128, 128])
    out = nc.dram_tensor("out", [NB, D], F32, kind="ExternalOutput").ap()
    b_in_l = scr("b_in_l", [16, 128, 2, 16, 128])
    b_in_r = scr("b_in_r", [8, 6, 128, 16, 256])
    b_w3 = scr("b_w3", [16, 128, 80, 128])
    b_wo = scr("b_wo", [4, 128, 16, 512])
    b_wge = scr("b_wge", [N_EXP, 128, 16, 512])
    b_wue = scr("b_wue", [N_EXP, 128, 16, 512])
    b_wde = scr("b_wde", [N_EXP, 128, 4, 2048])
    _dbg = os.environ.get("MK_DBG", "0") == "1"
    dscr = (lambda n, s, d=BF16: nc.dram_tensor(n, list(s), d, kind="ExternalOutput").ap()) if _dbg else scr
    Hs = dscr("Hs", [NB, D], F32)
    UA = dscr("UA", [4, 128, 16, TB])
    UB = dscr("UB", [4, 128, 32, TB])

    with ExitStack() as top:
        S = Sched(nc, top)
        dr_w = Buf("dram_w")
        dr_H = Buf("dram_H")
        dr_out = Buf("dram_out")
        PS = Ring(nc, top, "ps", 6, [128, 512], F32, psum=True)
        PT = Ring(nc, top, "pt", 2, [128, 8, 128], BF16, psum=True)

        dout = S.dma_src("dout")

        with ExitStack() as st:
            stg = DRing(S, nc, st, "cst", 3, [128, 4096], F32)
            cvb = Ring(nc, st, "cvb", 3, [128, 4096], BF16)
            cvs = [S.dma_src("cvo%d" % k) for k in range(3)]
            cnt = [0]

            def convert(src2d, dst2d, ncols):
                for c0 in range(0, ncols, 4096):
                    cw = min(4096, ncols - c0)
                    t, tb, ts = stg.next()
                    S.dma("sp", lambda e, t=t, c0=c0, cw=cw: e.dma_start(out=t[:, 0:cw], in_=src2d[:, c0:c0 + cw]), ts, writes=[tb])
                    k = cnt[0] % 3
                    o, ob = cvb.next()
                    eng = ("pool", "act", "dve")[cnt[0] % 3]
                    cnt[0] += 1
                    if eng == "act":
                        S.op("act", lambda e, o=o, t=t, cw=cw: e.copy(out=o[:, 0:cw], in_=t[:, 0:cw]), reads=[tb], writes=[ob])
                    else:
                        S.op(eng, lambda e, o=o, t=t, cw=cw: e.tensor_copy(out=o[:, 0:cw], in_=t[:, 0:cw]), reads=[tb], writes=[ob])
                    S.dma("act" if k == 1 else "sp", lambda e, o=o, c0=c0, cw=cw: e.dma_start(out=dst2d[:, c0:c0 + cw], in_=o[:, 0:cw]), cvs[k], reads=[ob], writes=[])

            for g in range(16 if "0" in _PH else 0):
                convert(w_in_l[g].rearrange("p a k c -> p (a k c)"), b_in_l[g].rearrange("p a k c -> p (a k c)"), 4096)
            for h in range(8 if "0" in _PH else 0):
                for j in range(6):
                    convert(w_in_r[h, j].rearrange("p k c -> p (k c)"), b_in_r[h, j].rearrange("p k c -> p (k c)"), 4096)
            for g in range(16 if "0" in _PH else 0):
                convert(w3[g].rearrange("p k c -> p (k c)"), b_w3[g].rearrange("p k c -> p (k c)"), 10240)
            for g in range(4 if "0" in _PH else 0):
                convert(wo[g].rearrange("p k c -> p (k c)"), b_wo[g].rearrange("p k c -> p (k c)"), 8192)
            for e_ in range(N_EXP if "0" in _PH else 0):
                convert(wge[e_].rearrange("p k c -> p (k c)"), b_wge[e_].rearrange("p k c -> p (k c)"), 8192)
                convert(wue[e_].rearrange("p k c -> p (k c)"), b_wue[e_].rearrange("p k c -> p (k c)"), 8192)
                convert(wde[e_].rearrange("p k c -> p (k c)"), b_wde[e_].rearrange("p k c -> p (k c)"), 8192)
            S.barrier()

        with ExitStack() as st:
            sb = lambda n, s, d=F32: st.enter_context(nc.sbuf_tensor(n, list(s), d))
            cq = S.dma_src("cq")
            cb_ = Buf("consts")
            lp = sb("lp", [128, 16, 8])
            wab = sb("wab", [128, 16, 2, 128], BF16)
            bgt = sb("bgt", [128, 2, 16])
            flg = sb("flg", [128, 2, 16])
            dmk = sb("dmk", [128, 8, 128])
            xz = sb("xz", [128, 2, 8])
            idf = sb("idf", [128, 128])
            idb = sb("idb", [128, 128], BF16)
            lrc = sb("lrc", [128, 2, 16])
            ltmp = sb("ltmp", [128, 16])
            hst = sb("hst", [128, 16])
            carry = sb("carry", [128, 16, 4])
            Sst = sb("Sst", [128, 8, 2, 512])
            for t_, a_ in ((lp, lrup), (bgt, bg), (flg, flags), (dmk, dmaskT), (xz, xizeta), (idf, ident)):
                S.dma("sp", lambda e, t_=t_, a_=a_: e.dma_start(out=t_[:], in_=a_), cq, writes=[cb_])
            S.op("dve", lambda e: e.tensor_copy(out=idb[:], in_=idf[:]), reads=[cb_], writes=[cb_])
            S.op("act", lambda e: e.activation(out=ltmp[:], in_=lp[:, :, 7], func=AF.Exp, scale=-1.0), reads=[cb_], writes=[cb_])
            S.op("act", lambda e: e.activation(out=ltmp[:], in_=ltmp[:], func=AF.Ln, bias=1.0), reads=[cb_], writes=[cb_])
            S.op("dve", lambda e: e.tensor_scalar(out=lrc[:, 0, :], in0=ltmp[:], scalar1=-8.0, scalar2=None, op0=ALU.mult), reads=[cb_], writes=[cb_])
            S.op("dve", lambda e: e.tensor_scalar(out=lrc[:, 1, :], in0=ltmp[:], scalar1=-16.0, scalar2=None, op0=ALU.mult), reads=[cb_], writes=[cb_])
            S.op("pool", lambda e: e.memset(hst[:], 0.0), writes=[cb_])
            S.op("pool", lambda e: e.memset(carry[:], 0.0), writes=[cb_])
            S.op("pool", lambda e: e.memset(Sst[:], 0.0), writes=[cb_])
            hst_b = [Buf("hst%d" % h) for h in range(16)]
            car_b = [Buf("car%d" % h) for h in range(16)]
            S_b = [Buf("S%d" % h) for h in range(8)]
            for bl in hst_b + car_b + S_b:
                bl.w = cb_.w

            xstg = DRing(S, nc, st, "xstg", 2, [128, 4, TB], F32)
            for hf in range(2):
                t, tb, ts = xstg.next()
                S.dma("sp", lambda e, t=t, hf=hf: e.dma_start(out=t[:].rearrange("p a b -> p (a b)"), in_=wai[:, hf * 8:(hf + 1) * 8].rearrange("p h a o -> p (h a o)")), ts, writes=[tb])
                S.op("dve", lambda e, t=t, hf=hf: e.tensor_copy(out=wab[:, hf * 8:(hf + 1) * 8].rearrange("p h a o -> p (h a o)"), in_=t[:].rearrange("p a b -> p (a b)")), reads=[tb], writes=[cb_])
            xTb = Ring(nc, st, "xTb", 1, [128, 16, TB], BF16)
            cs = DRing(S, nc, st, "cs", 1, [128, 2, TB], F32)
            wl = DRing(S, nc, st, "wl", 2, [128, 2, 16, 128], BF16)
            wq = DRing(S, nc, st, "wq", 5, [128, 16, 256], BF16)
            L = {n: Ring(nc, st, "l_" + n, 1, [128, TB], F32) for n in ("xa", "r", "i", "a", "m", "h", "gy")}
            Lxs = Ring(nc, st, "l_xs", 1, [128, TB + 4], F32)
            Lxab = Ring(nc, st, "l_xab", 2, [128, TB], BF16)
            Lin = Ring(nc, st, "l_in", 2, [128, 1], F32)
            Rt = Ring(nc, st, "r_t", 4, [128, TB], F32)
            Rq = Ring(nc, st, "r_q", 2, [128, 2, TB], BF16)
            Rk = Ring(nc, st, "r_k", 2, [128, 2, TB], BF16)
            Rkz = Ring(nc, st, "r_kz", 2, [128, 256], BF16)
            Rv = Ring(nc, st, "r_v", 2, [128, 512], BF16)
            Rs = Ring(nc, st, "r_s", 2, [128, 128], BF16)
            Ro = Ring(nc, st, "r_o", 2, [128, 512], F32)
            Rsg = Ring(nc, st, "r_sg", 1, [128, 512], F32)
            Rub = Ring(nc, st, "r_ub", 2, [128, 512], BF16)
            Rst = Ring(nc, st, "r_st", 2, [128, 8], F32)
            Sb = Ring(nc, st, "Sb", 2, [128, 2, 512], BF16)
            gn = DRing(S, nc, st, "gn", 2, [128, 512], F32)
            uar = Ring(nc, st, "uar", 2, [128, TB], BF16)
            ubr = Ring(nc, st, "ubr", 2, [128, 4, TB], BF16)
            uas = [S.dma_src("uas%d" % k) for k in range(2)]
            ubs = [S.dma_src("ubs%d" % k) for k in range(2)]
            uacnt = [0, 0]

            print("phase1 sbuf remaining", nc.sbuf_bytes_remaining, flush=True)

            def mmgroup(ps, psb, pairs, reads):
                n = len(pairs)
                S.group("pe", [
                    (lambda e, l=l, r=r, i=i: e.matmul(ps, lhsT=l, rhs=r, start=(i == 0), stop=(i == n - 1)))
                    for i, (l, r) in enumerate(pairs)], reads=reads, writes=[psb])

            for blk in (_BLKS if "1" in _PH else []):
                main = blk >= 12
                xb, xbb = xTb.next()
                for hf in range(4):
                    t, tb, ts = xstg.next()
                    S.dma("sp", lambda e, t=t, hf=hf, blk=blk: e.dma_start(out=t[:], in_=xT[blk, :, hf * 4:(hf + 1) * 4, :]), ts, writes=[tb])
                    S.op("pool", lambda e, t=t, xb=xb, hf=hf: e.tensor_copy(out=xb[:, hf * 4:(hf + 1) * 4, :], in_=t[:]), reads=[tb], writes=[xbb])
                ct, ctb, cts = cs.next()
                S.dma("act", lambda e, ct=ct, blk=blk: e.dma_start(out=ct[:, 0, :], in_=cosT[blk]), cts, writes=[ctb])
                S.dma("act", lambda e, ct=ct, blk=blk: e.dma_start(out=ct[:, 1, :], in_=sinT[blk]), cts, writes=[ctb])

                for h in range(_LH):
                    w, wb_, ws = wl.next()
                    if main:
                        S.dma("sp", lambda e, w=w, h=h: e.dma_start(out=w[:], in_=b_in_l[h]), ws, reads=[dr_w], writes=[wb_])
                    else:
                        S.dma("sp", lambda e, w=w, h=h: e.dma_start(out=w[:, 0], in_=b_in_l[h, :, 0]), ws, reads=[dr_w], writes=[wb_])
                    px, pxb = PS.next()
                    mmgroup(px[:], pxb, [(w[:, 0, kc, :], xb[:, kc, :]) for kc in range(16)], [wb_, xbb])
                    xs, xsb = Lxs.next()
                    S.op("act", lambda e, xs=xs, px=px: e.copy(out=xs[:, 4:TB + 4], in_=px[:]), reads=[pxb], writes=[xsb])
                    S.op("pool", lambda e, xs=xs, h=h: e.tensor_copy(out=xs[:, 0:4], in_=carry[:, h, :]), reads=[car_b[h]], writes=[xsb])
                    S.op("pool", lambda e, xs=xs, h=h: e.tensor_copy(out=carry[:, h, :], in_=xs[:, TB:TB + 4]), reads=[xsb], writes=[car_b[h]])
                    xa, xab_ = L["xa"].next()
                    S.op("dve", lambda e, xa=xa, xs=xs, h=h: e.tensor_scalar(out=xa[:], in0=xs[:, 4:TB + 4], scalar1=lp[:, h, 3:4], scalar2=lp[:, h, 4:5], op0=ALU.mult, op1=ALU.add), reads=[xsb], writes=[xab_])
                    for j in range(3):
                        S.op("dve", lambda e, xa=xa, xs=xs, h=h, j=j: e.scalar_tensor_tensor(out=xa[:], in0=xs[:, 1 + j:TB + 1 + j], scalar=lp[:, h, j:j + 1], in1=xa[:], op0=ALU.mult, op1=ALU.add), reads=[xsb, xab_], writes=[xab_])
                    xq, xqb = Lxab.next()
                    S.op("act", lambda e, xq=xq, xa=xa: e.copy(out=xq[:], in_=xa[:]), reads=[xab_], writes=[xqb])
                    pr, prb = PS.next()
                    S.op("pe", lambda e, pr=pr, xq=xq, h=h: e.matmul(pr[:], lhsT=wab[:, h, 0, :], rhs=xq[:], start=True, stop=True), reads=[xqb], writes=[prb])
                    pi, pib = PS.next()
                    S.op("pe", lambda e, pi=pi, xq=xq, h=h: e.matmul(pi[:], lhsT=wab[:, h, 1, :], rhs=xq[:], start=True, stop=True), reads=[xqb], writes=[pib])
                    r, rb = L["r"].next()
                    S.op("act", lambda e, r=r, pr=pr, h=h: e.activation(out=r[:], in_=pr[:], func=AF.Sigmoid, bias=lp[:, h, 5:6]), reads=[prb], writes=[rb])
                    ig, igb = L["i"].next()
                    S.op("act", lambda e, ig=ig, pi=pi, h=h: e.activation(out=ig[:], in_=pi[:], func=AF.Sigmoid, bias=lp[:, h, 6:7]), reads=[pib], writes=[igb])
                    a, ab = L["a"].next()
                    S.op("act", lambda e, a=a, r=r, h=h: e.activation(out=a[:], in_=r[:], func=AF.Exp, scale=lrc[:, 0, h:h + 1]), reads=[rb], writes=[ab])
                    m, mb = L["m"].next()
                    S.op("act", lambda e, m=m, r=r, h=h: e.activation(out=m[:], in_=r[:], func=AF.Exp, scale=lrc[:, 1, h:h + 1]), reads=[rb], writes=[mb])
                    S.op("act", lambda e, m=m: e.activation(out=m[:], in_=m[:], func=AF.Sqrt, scale=-1.0, bias=1.0), reads=[mb], writes=[mb])
                    S.op("dve", lambda e, m=m, blk=blk: e.tensor_scalar(out=m[:, 0:1], in0=m[:, 0:1], scalar1=flg[:, 1, blk:blk + 1], scalar2=flg[:, 0, blk:blk + 1], op0=ALU.mult, op1=ALU.add), reads=[mb], writes=[mb])
                    S.op("dve", lambda e, m=m, ig=ig: e.tensor_tensor(out=m[:], in0=m[:], in1=ig[:], op=ALU.mult), reads=[mb, igb], writes=[mb])
                    S.op("dve", lambda e, m=m, xa=xa: e.tensor_tensor(out=m[:], in0=m[:], in1=xa[:], op=ALU.mult), reads=[mb, xab_], writes=[mb])
                    ini, inib = Lin.next()
                    S.op("dve", lambda e, ini=ini, h=h, blk=blk: e.tensor_tensor(out=ini[:], in0=hst[:, h:h + 1], in1=flg[:, 1, blk:blk + 1], op=ALU.mult), reads=[hst_b[h]], writes=[inib])
                    hh, hb = L["h"].next()
                    S.op("dve", lambda e, hh=hh, a=a, m=m, ini=ini: e.tensor_tensor_scan(out=hh[:], data0=a[:], data1=m[:], initial=ini[:], op0=ALU.mult, op1=ALU.add), reads=[ab, mb, inib], writes=[hb])
                    S.op("pool", lambda e, hh=hh, h=h: e.tensor_copy(out=hst[:, h:h + 1], in_=hh[:, TB - 1:TB]), reads=[hb], writes=[hst_b[h]])
                    if main:
                        py, pyb = PS.next()
                        mmgroup(py[:], pyb, [(w[:, 1, kc, :], xb[:, kc, :]) for kc in range(16)], [wb_, xbb])
                        gy, gyb = L["gy"].next()
                        S.op("act", lambda e, gy=gy, py=py: e.activation(out=gy[:], in_=py[:], func=AF.Gelu_apprx_tanh), reads=[pyb], writes=[gyb])
                        k_ = uacnt[0] % 2
                        uacnt[0] += 1
                        ut, utb = uar.next()
                        S.op("dve", lambda e, gy=gy, hh=hh, ut=ut: e.tensor_tensor(out=ut[:], in0=hh[:], in1=gy[:], op=ALU.mult), reads=[hb, gyb], writes=[utb])
                        S.dma("act", lambda e, ut=ut, h=h, blk=blk: e.dma_start(out=UA[blk - 12, :, h, :], in_=ut[:]), uas[k_], reads=[utb], writes=[])

                for h in range(_RH):
                    gc = GAMMA[h] ** 128
                    wts = {}
                    for j, nm in enumerate(("q", "k", "v0", "v1", "g0", "g1")):
                        if not main and nm in ("q", "g0", "g1"):
                            continue
                        w, wb_, ws = wq.next() if nm in ("q", "k") else (None, None, None)
                        if nm in ("q", "k"):
                            S.dma("sp", lambda e, w=w, h=h, j=j: e.dma_start(out=w[:], in_=b_in_r[h, j]), ws, reads=[dr_w], writes=[wb_])
                            wts[nm] = (w, wb_)
                    def proj_rope(nm, ring):
                        w, wb_ = wts[nm]
                        p0, p0b = PS.next()
                        mmgroup(p0[:], p0b, [(w[:, kc, 0:128], xb[:, kc, :]) for kc in range(16)], [wb_, xbb])
                        p1, p1b = PS.next()
                        mmgroup(p1[:], p1b, [(w[:, kc, 128:256], xb[:, kc, :]) for kc in range(16)], [wb_, xbb])
                        o, ob = ring.next()
                        t1, t1b = Rt.next()
                        t2, t2b = Rt.next()
                        S.op("dve", lambda e: e.tensor_tensor(out=t1[:], in0=p0[:], in1=ct[:, 0, :], op=ALU.mult), reads=[p0b, ctb], writes=[t1b])
                        S.op("dve", lambda e: e.tensor_tensor(out=t2[:], in0=p1[:], in1=ct[:, 1, :], op=ALU.mult), reads=[p1b, ctb], writes=[t2b])
                        S.op("pool", lambda e: e.tensor_tensor(out=o[:, 0, :], in0=t1[:], in1=t2[:], op=ALU.subtract), reads=[t1b, t2b], writes=[ob])
                        t3, t3b = Rt.next()
                        t4, t4b = Rt.next()
                        S.op("dve", lambda e: e.tensor_tensor(out=t3[:], in0=p0[:], in1=ct[:, 1, :], op=ALU.mult), reads=[p0b, ctb], writes=[t3b])
                        S.op("dve", lambda e: e.tensor_tensor(out=t4[:], in0=p1[:], in1=ct[:, 0, :], op=ALU.mult), reads=[p1b, ctb], writes=[t4b])
                        S.op("pool", lambda e: e.tensor_tensor(out=o[:, 1, :], in0=t3[:], in1=t4[:], op=ALU.add), reads=[t3b, t4b], writes=[ob])
                        return o, ob

                    kr, krb = proj_rope("k", Rk)
                    if main:
                        qr, qrb = proj_rope("q", Rq)
                    wv = []
                    for j in (2, 3):
                        w, wb_, ws = wq.next()
                        S.dma("sp", lambda e, w=w, h=h, j=j: e.dma_start(out=w[:], in_=b_in_r[h, j]), ws, reads=[dr_w], writes=[wb_])
                        wv.append((w, wb_))
                    wg_ = []
                    if main:
                        gt, gtb, gts = gn.next()
                        S.dma("act", lambda e, gt=gt, h=h: e.dma_start(out=gt[:], in_=gain_bc[:, h * 512:(h + 1) * 512]), gts, writes=[gtb])
                    if main:
                        k_ = uacnt[1] % 2
                        uacnt[1] += 1
                        ubt, ubtb = ubr.next()
                    for c in range(4):
                        tok = slice(c * 128, (c + 1) * 128)
                        pv, pvb = PS.next()
                        for hf in range(2):
                            n = 16
                            S.group("pe", [
                                (lambda e, kc=kc, hf=hf: e.matmul(pv[:, hf * 256:(hf + 1) * 256], lhsT=xb[:, kc, tok], rhs=wv[hf][0][:, kc, :], start=(kc == 0), stop=(kc == 15)))
                                for kc in range(16)], reads=[xbb, wv[hf][1]], writes=[pvb])
                        vb, vbb = Rv.next()
                        S.op("act", lambda e, vb=vb, pv=pv: e.copy(out=vb[:], in_=pv[:]), reads=[pvb], writes=[vbb])
                        kz, kzb = Rkz.next()
                        pt, ptb_ = PT.next()
                        S.group("pe", [(lambda e, pt=pt, j=j: e.transpose(pt[:, j, :], kr[:, j, tok], idb[:])) for j in range(2)], reads=[krb], writes=[ptb_])
                        S.op("dve", lambda e, pt=pt, kz=kz: e.tensor_scalar(out=kz[:].rearrange("p (a b) -> p a b", b=128), in0=pt[:, 0:2, :], scalar1=xz[:, 1, h:h + 1], scalar2=None, op0=ALU.mult), reads=[ptb_], writes=[kzb])
                        if main:
                            psc, pscb = PS.next()
                            mmgroup(psc[:, 0:128], pscb, [(kr[:, j, tok], qr[:, j, tok]) for j in range(2)], [krb, qrb])
                            sm, smb = Rs.next()
                            S.op("dve", lambda e, sm=sm, psc=psc: e.tensor_tensor(out=sm[:], in0=psc[:, 0:128], in1=dmk[:, h, :], op=ALU.mult), reads=[pscb], writes=[smb])
                            po, pob = PS.next()
                            S.op("pe", lambda e, po=po, sm=sm, vb=vb: e.matmul(po[:], lhsT=sm[:], rhs=vb[:], start=True, stop=True), reads=[smb, vbb], writes=[pob])
                            sbf, sbfb = Sb.next()
                            S.op("pool", lambda e, sbf=sbf: e.tensor_copy(out=sbf[:], in_=Sst[:, h]), reads=[S_b[h]], writes=[sbfb])
                            pc, pcb = PS.next()
                            mmgroup(pc[:], pcb, [(qr[:, j, tok], sbf[:, j, :]) for j in range(2)], [qrb, sbfb])
                            o1, o1b = Ro.next()
                            S.op("act", lambda e, o1=o1, po=po: e.copy(out=o1[:], in_=po[:]), reads=[pob], writes=[o1b])
                            o2, o2b = Ro.next()
                            S.op("dve", lambda e, o2=o2, pc=pc, o1=o1: e.scalar_tensor_tensor(out=o2[:], in0=pc[:], scalar=xz[:, 0, h:h + 1], in1=o1[:], op0=ALU.mult, op1=ALU.add), reads=[pcb, o1b], writes=[o2b])
                            stt, sttb = Rst.next()
                            S.op("dve", lambda e, stt=stt, o2=o2: e.bn_stats(out=stt[:, 0:6], in_=o2[:]), reads=[o2b], writes=[sttb])
                            S.op("dve", lambda e, stt=stt: e.bn_aggr(out=stt[:, 6:8], in_=stt[:, 0:6]), reads=[sttb], writes=[sttb])
                            S.op("dve", lambda e, stt=stt: e.tensor_scalar(out=stt[:, 7:8], in0=stt[:, 7:8], scalar1=LN_EPS, scalar2=None, op0=ALU.add), reads=[sttb], writes=[sttb])
                            S.op("act", lambda e, stt=stt: e.activation(out=stt[:, 7:8], in_=stt[:, 7:8], func=AF.Sqrt), reads=[sttb], writes=[sttb])
                            S.op("dve", lambda e, stt=stt: e.reciprocal(out=stt[:, 7:8], in_=stt[:, 7:8]), reads=[sttb], writes=[sttb])
                            S.op("dve", lambda e, stt=stt, o2=o2: e.tensor_scalar(out=o2[:], in0=o2[:], scalar1=stt[:, 6:7], scalar2=stt[:, 7:8], op0=ALU.subtract, op1=ALU.mult), reads=[sttb, o2b], writes=[o2b])
                            S.op("pool", lambda e, o2=o2, gt=gt: e.tensor_tensor(out=o2[:], in0=o2[:], in1=gt[:], op=ALU.mult), reads=[o2b, gtb], writes=[o2b])
                            if c == 0:
                                for j in (4, 5):
                                    w, wb_, ws = wq.next()
                                    S.dma("sp", lambda e, w=w, h=h, j=j: e.dma_start(out=w[:], in_=b_in_r[h, j]), ws, reads=[dr_w], writes=[wb_])
                                    wg_.append((w, wb_))
                            pg, pgb = PS.next()
                            for hf in range(2):
                                S.group("pe", [
                                    (lambda e, kc=kc, hf=hf: e.matmul(pg[:, hf * 256:(hf + 1) * 256], lhsT=xb[:, kc, tok], rhs=wg_[hf][0][:, kc, :], start=(kc == 0), stop=(kc == 15)))
                                    for kc in range(16)], reads=[xbb, wg_[hf][1]], writes=[pgb])
                            sg, sgb = Rsg.next()
                            S.op("act", lambda e, sg=sg, pg=pg: e.activation(out=sg[:], in_=pg[:], func=AF.Silu), reads=[pgb], writes=[sgb])
                            ub, ubb = Rub.next()
                            S.op("dve", lambda e, ub=ub, o2=o2, sg=sg: e.tensor_tensor(out=ub[:], in0=o2[:], in1=sg[:], op=ALU.mult), reads=[o2b, sgb], writes=[ubb])
                            pt, ptb_ = PT.next()
                            S.group("pe", [(lambda e, pt=pt, ub=ub, q4=q4: e.transpose(pt[:, q4, :], ub[:, q4 * 128:(q4 + 1) * 128], idb[:])) for q4 in range(4)], reads=[ubb], writes=[ptb_])
                            S.op("act", lambda e, pt=pt: e.copy(out=ubt[:, :, tok], in_=pt[:, 0:4, :]), reads=[ptb_], writes=[ubtb])
                        for j in range(2):
                            pd, pdb = PS.next()
                            S.op("pe", lambda e, pd=pd, kz=kz, vb=vb, j=j: e.matmul(pd[:], lhsT=kz[:, j * 128:(j + 1) * 128], rhs=vb[:], start=True, stop=True), reads=[kzb, vbb], writes=[pdb])
                            S.op("dve", lambda e, pd=pd, j=j: e.scalar_tensor_tensor(out=Sst[:, h, j, :], in0=Sst[:, h, j, :], scalar=gc, in1=pd[:], op0=ALU.mult, op1=ALU.add), reads=[pdb, S_b[h]], writes=[S_b[h]])

                    if main:
                        S.dma("act", lambda e, ubt=ubt, h=h, blk=blk: e.dma_start(out=UB[blk - 12, :, h * 4:(h + 1) * 4, :], in_=ubt[:]), ubs[k_], reads=[ubtb], writes=[])
            S.barrier()

        with ExitStack() as st:
            sb = lambda n, s, d=F32: st.enter_context(nc.sbuf_tensor(n, list(s), d))
            cq = S.dma_src("cq1b")
            bgt = sb("bgt2", [128, 2, 16])
            cb_ = Buf("consts1b")
            S.dma("sp", lambda e: e.dma_start(out=bgt[:], in_=bg), cq, writes=[cb_])
            xstg = DRing(S, nc, st, "xstg2", 2, [128, 2, TB], F32)
            xTb = Ring(nc, st, "xTb2", 1, [128, 16, TB], BF16)
            uaT = sb("uaT", [128, 16, TB], BF16)
            ubT = sb("ubT", [128, 32, TB], BF16)
            mxT = sb("mxT", [128, 16, TB], BF16)
            ua_b, ub_b, mx_b = Buf("uaT"), Buf("ubT"), Buf("mxT")
            uq = S.dma_src("uq")
            uq2 = S.dma_src("uq2")
            w3r = DRing(S, nc, st, "w3r", 5, [128, 20, 128], BF16)
            wor = DRing(S, nc, st, "wor", 3, [128, 8, 512], BF16)
            xrr = DRing(S, nc, st, "xrr", 1, [128, 4, D], F32)
            lnt = sb("lnt", [128, 2, D])
            lnb = cb_
            S.dma("act", lambda e: e.dma_start(out=lnt[:], in_=lnp[:, 0:2, :]), cq, writes=[lnb])
            G1 = Ring(nc, st, "g1", 1, [128, TB], F32)
            G2 = Ring(nc, st, "g2", 1, [128, TB], F32)
            Zst = Ring(nc, st, "zst", 2, [128, 32], F32)
            hsrc = S.dma_src("hs")

            def mmgroup(ps, psb, pairs, reads):
                n = len(pairs)
                S.group("pe", [
                    (lambda e, l=l, r=r, i=i: e.matmul(ps, lhsT=l, rhs=r, start=(i == 0), stop=(i == n - 1)))
                    for i, (l, r) in enumerate(pairs)], reads=reads, writes=[psb])

            for bi in range(4 if "1b" in _PH else 0):
                xb, xbb = xTb.next()
                for hf in range(8):
                    t, tb, ts = xstg.next()
                    S.dma("sp", lambda e, t=t, hf=hf, bi=bi: e.dma_start(out=t[:], in_=xT[12 + bi, :, hf * 2:(hf + 1) * 2, :]), ts, writes=[tb])
                    S.op("pool", lambda e, t=t, xb=xb, hf=hf: e.tensor_copy(out=xb[:, hf * 2:(hf + 1) * 2, :], in_=t[:]), reads=[tb], writes=[xbb])
                S.dma("act", lambda e, bi=bi: e.dma_start(out=uaT[:], in_=UA[bi]), uq, writes=[ua_b])
                S.dma("act", lambda e, bi=bi: e.dma_start(out=ubT[:], in_=UB[bi]), uq2, writes=[ub_b])
                for nt in range(16):
                    pcs = []
                    for q_ in range(4):
                        w, wb_, ws = w3r.next()
                        S.dma("sp", lambda e, w=w, nt=nt, q_=q_: e.dma_start(out=w[:], in_=b_w3[nt, :, q_ * 20:(q_ + 1) * 20, :]), ws, writes=[wb_])
                        pcs.append((w, wb_))
                    wsl = lambda kc: pcs[kc // 20][0][:, kc % 20, :]
                    wbs = lambda lo, hi: [pcs[q_][1] for q_ in range(lo // 20, (hi - 1) // 20 + 1)]
                    pa, pab = PS.next()
                    mmgroup(pa[:], pab, [(wsl(kc), uaT[:, kc, :]) for kc in range(16)], wbs(0, 16) + [ua_b])
                    pb, pbb = PS.next()
                    mmgroup(pb[:], pbb, [(wsl(16 + kc), ubT[:, kc, :]) for kc in range(32)], wbs(16, 48) + [ub_b])
                    pga, pgab = PS.next()
                    mmgroup(pga[:], pgab, [(wsl(48 + kc), xb[:, kc, :]) for kc in range(16)], wbs(48, 64) + [xbb])
                    pgb_, pgbb = PS.next()
                    mmgroup(pgb_[:], pgbb, [(wsl(64 + kc), xb[:, kc, :]) for kc in range(16)], wbs(64, 80) + [xbb])
                    s1, s1b = G1.next()
                    S.op("act", lambda e, s1=s1, pga=pga, nt=nt: e.activation(out=s1[:], in_=pga[:], func=AF.Sigmoid, bias=bgt[:, 0, nt:nt + 1]), reads=[pgab], writes=[s1b])
                    s2, s2b = G2.next()
                    S.op("act", lambda e, s2=s2, pgb_=pgb_, nt=nt: e.activation(out=s2[:], in_=pgb_[:], func=AF.Sigmoid, bias=bgt[:, 1, nt:nt + 1]), reads=[pgbb], writes=[s2b])
                    S.op("dve", lambda e, s1=s1, pa=pa: e.tensor_tensor(out=s1[:], in0=s1[:], in1=pa[:], op=ALU.mult), reads=[s1b, pab], writes=[s1b])
                    S.op("dve", lambda e, s2=s2, pb=pb: e.tensor_tensor(out=s2[:], in0=s2[:], in1=pb[:], op=ALU.mult), reads=[s2b, pbb], writes=[s2b])
                    S.op("pool", lambda e, s1=s1, s2=s2, nt=nt: e.tensor_tensor(out=mxT[:, nt, :], in0=s1[:], in1=s2[:], op=ALU.add), reads=[s1b, s2b], writes=[mx_b])
                xr, xrb, xrs = xrr.next()
                S.dma("act", lambda e, xr=xr, bi=bi: e.dma_start(out=xr[:], in_=xres[bi * TB:(bi + 1) * TB, :].rearrange("(t p) d -> p t d", p=128)), xrs, reads=[dr_H], writes=[xrb])
                for nb in range(4):
                    pcs = []
                    for q_ in range(2):
                        w, wb_, ws = wor.next()
                        S.dma("sp", lambda e, w=w, nb=nb, q_=q_: e.dma_start(out=w[:], in_=b_wo[nb, :, q_ * 8:(q_ + 1) * 8, :]), ws, writes=[wb_])
                        pcs.append((w, wb_))
                    for tt in range(4):
                        pz, pzb = PS.next()
                        mmgroup(pz[:], pzb, [(mxT[:, kc, tt * 128:(tt + 1) * 128], pcs[kc // 8][0][:, kc % 8, :]) for kc in range(16)], [pcs[0][1], pcs[1][1], mx_b])
                        S.op("dve", lambda e, xr=xr, pz=pz, tt=tt, nb=nb: e.scalar_tensor_tensor(out=xr[:, tt, nb * 512:(nb + 1) * 512], in0=xr[:, tt, nb * 512:(nb + 1) * 512], scalar=DN_ALPHA, in1=pz[:], op0=ALU.mult, op1=ALU.add), reads=[pzb, xrb], writes=[xrb])
                for tt in range(4):
                    zs, zsb = Zst.next()
                    for q4 in range(4):
                        S.op("dve", lambda e, zs=zs, xr=xr, tt=tt, q4=q4: e.bn_stats(out=zs[:, q4 * 6:(q4 + 1) * 6], in_=xr[:, tt, q4 * 512:(q4 + 1) * 512]), reads=[xrb], writes=[zsb])
                    S.op("dve", lambda e, zs=zs: e.bn_aggr(out=zs[:, 24:26], in_=zs[:, 0:24]), reads=[zsb], writes=[zsb])
                    S.op("dve", lambda e, zs=zs: e.tensor_scalar(out=zs[:, 25:26], in0=zs[:, 25:26], scalar1=LN_EPS, scalar2=None, op0=ALU.add), reads=[zsb], writes=[zsb])
                    S.op("act", lambda e, zs=zs: e.activation(out=zs[:, 25:26], in_=zs[:, 25:26], func=AF.Sqrt), reads=[zsb], writes=[zsb])
                    S.op("dve", lambda e, zs=zs: e.reciprocal(out=zs[:, 25:26], in_=zs[:, 25:26]), reads=[zsb], writes=[zsb])
                    S.op("dve", lambda e, zs=zs, xr=xr, tt=tt: e.tensor_scalar(out=xr[:, tt, :], in0=xr[:, tt, :], scalar1=zs[:, 24:25], scalar2=zs[:, 25:26], op0=ALU.subtract, op1=ALU.mult), reads=[zsb, xrb], writes=[xrb])
                    S.op("pool", lambda e, xr=xr, tt=tt: e.tensor_tensor(out=xr[:, tt, :], in0=xr[:, tt, :], in1=lnt[:, 0, :], op=ALU.mult), reads=[xrb, lnb], writes=[xrb])
                    S.op("pool", lambda e, xr=xr, tt=tt: e.tensor_tensor(out=xr[:, tt, :], in0=xr[:, tt, :], in1=lnt[:, 1, :], op=ALU.add), reads=[xrb, lnb], writes=[xrb])
                S.dma("sp", lambda e, xr=xr, bi=bi: e.dma_start(out=Hs[bi * TB:(bi + 1) * TB, :].rearrange("(t p) d -> p t d", p=128), in_=xr[:]), hsrc, reads=[xrb], writes=[dr_H])
            S.barrier()

        with ExitStack() as st:
            sb = lambda n, s, d=F32: st.enter_context(nc.sbuf_tensor(n, list(s), d))
            cq2 = S.dma_src("cq2")
            c2b = Buf("consts2")
            wrt = sb("wrt", [128, 16, 64])
            rbt = sb("rbt", [128, 64])
            idf2 = sb("idf2", [128, 128])
            ln2 = sb("ln2", [128, 2, D])
            wall = sb("wall", [128, 16, 65])
            wall_b = Buf("wall")
            for t_, a_ in ((wrt, wr), (rbt, rb_bc), (idf2, ident), (ln2, lnp[:, 2:4, :])):
                S.dma("sp", lambda e, t_=t_, a_=a_: e.dma_start(out=t_[:], in_=a_), cq2, writes=[c2b])
            hT = Ring(nc, st, "hT32", 1, [128, 16, 128], F32)
            hld = DRing(S, nc, st, "hld", 1, [128, 4, D], F32)
            R1 = Ring(nc, st, "rt1", 2, [128, 64], F32)
            R2 = Ring(nc, st, "rt2", 2, [128, 64], F32)
            R3 = Ring(nc, st, "rt3", 2, [128, 64], F32)
            R8 = Ring(nc, st, "rt8", 2, [128, 4, 8], F32)
            hTb = sb("hTb", [128, 16, TB], BF16)
            hTb_b = Buf("hTb")
            yacc = sb("yacc", [128, 4, D])
            yb_ = Buf("yacc")
            h1T = Ring(nc, st, "h1T", 1, [128, 4, TB], BF16)
            Rsg2 = Ring(nc, st, "sg2", 1, [128, TB], F32)
            wgu = DRing(S, nc, st, "wgu", 3, [128, 16, 512], BF16)
            wdr = DRing(S, nc, st, "wdr", 2, [128, 4, D], BF16)
            Zs2 = Ring(nc, st, "zs2", 2, [128, 32], F32)
            osrc = S.dma_src("osrc")
            S.op("pool", lambda e: e.memset(wall[:], 1.0), writes=[wall_b])

            def mmgroup(ps, psb, pairs, reads):
                n = len(pairs)
                S.group("pe", [
                    (lambda e, l=l, r=r, i=i: e.matmul(ps, lhsT=l, rhs=r, start=(i == 0), stop=(i == n - 1)))
                    for i, (l, r) in enumerate(pairs)], reads=reads, writes=[psb])

            for bi in range(4 if "2" in _PH else 0):
                hx, hxb, hxs = hld.next()
                S.dma("sp", lambda e, hx=hx, bi=bi: e.dma_start(out=hx[:], in_=Hs[bi * TB:(bi + 1) * TB, :].rearrange("(t p) d -> p t d", p=128)), hxs, reads=[dr_H, dr_out], writes=[hxb])
                for tt in range(4):
                    tg = bi * 4 + tt
                    h32, h32b = hT.next()
                    for k4 in range(4):
                        pp, ppb = PS.next()
                        S.group("pe", [(lambda e, pp=pp, hx=hx, tt=tt, kc=k4 * 4 + q4, q4=q4: e.transpose(pp[:, q4 * 128:(q4 + 1) * 128], hx[:, tt, kc * 128:(kc + 1) * 128], idf2[:])) for q4 in range(4)], reads=[hxb, c2b], writes=[ppb])
                        S.op("act", lambda e, pp=pp, h32=h32, k4=k4: e.copy(out=h32[:, k4 * 4:(k4 + 1) * 4, :], in_=pp[:].rearrange("p (a b) -> p a b", b=128)), reads=[ppb], writes=[h32b])
                        S.op("pool", lambda e, h32=h32, k4=k4, tt=tt: e.tensor_copy(out=hTb[:, k4 * 4:(k4 + 1) * 4, tt * 128:(tt + 1) * 128], in_=h32[:, k4 * 4:(k4 + 1) * 4, :]), reads=[h32b], writes=[hTb_b])
                    pl, plb = PS.next()
                    mmgroup(pl[:, 0:64], plb, [(h32[:, kc, :], wrt[:, kc, :]) for kc in range(16)], [h32b, c2b])
                    sc, scb = R1.next()
                    S.op("act", lambda e, sc=sc, pl=pl: e.activation(out=sc[:], in_=pl[:, 0:64], func=AF.Sigmoid), reads=[plb], writes=[scb])
                    bs, bsb = R2.next()
                    S.op("dve", lambda e, bs=bs, sc=sc: e.tensor_tensor(out=bs[:], in0=sc[:], in1=rbt[:], op=ALU.add), reads=[scb, c2b], writes=[bsb])
                    t8, t8b = R3.next()
                    for g in range(8):
                        S.op("dve", lambda e, t8=t8, bs=bs, g=g: e.max(out=t8[:, g * 8:(g + 1) * 8], in_=bs[:, g * 8:(g + 1) * 8]), reads=[bsb], writes=[t8b])
                    sm8, sm8b = R8.next()
                    t8v = t8[:].rearrange("p (g k) -> p g k", k=8)
                    S.op("dve", lambda e, sm8=sm8, t8v=t8v: e.tensor_tensor(out=sm8[:, 0, :], in0=t8v[:, :, 0], in1=t8v[:, :, 1], op=ALU.add), reads=[t8b], writes=[sm8b])
                    S.op("dve", lambda e, sm8=sm8: e.max(out=sm8[:, 1, :], in_=sm8[:, 0, :]), reads=[sm8b], writes=[sm8b])
                    S.op("dve", lambda e, sm8=sm8: e.tensor_scalar(out=sm8[:, 2, :], in0=sm8[:, 0, :], scalar1=sm8[:, 1, 3:4], scalar2=None, op0=ALU.is_ge), reads=[sm8b], writes=[sm8b])
                    S.op("dve", lambda e, sm8=sm8: e.tensor_scalar(out=sm8[:, 2, :], in0=sm8[:, 2, :], scalar1=-1.0, scalar2=1e9, op0=ALU.add, op1=ALU.mult), reads=[sm8b], writes=[sm8b])
                    bm, bmb = R3.next()
                    S.op("dve", lambda e, bm=bm, bs=bs, sm8=sm8: e.tensor_tensor(out=bm[:].rearrange("p (g k) -> p g k", k=8), in0=bs[:].rearrange("p (g k) -> p g k", k=8), in1=sm8[:, 2, :].unsqueeze(2).to_broadcast([128, 8, 8]), op=ALU.add), reads=[bsb, sm8b], writes=[bmb])
                    S.op("dve", lambda e, sm8=sm8, bm=bm: e.max(out=sm8[:, 3, :], in_=bm[:]), reads=[bmb], writes=[sm8b])
                    S.op("dve", lambda e, sm8=sm8, bm=bm: e.tensor_scalar(out=bm[:], in0=bm[:], scalar1=sm8[:, 3, 7:8], scalar2=None, op0=ALU.is_ge), reads=[bmb, sm8b], writes=[bmb])
                    S.op("dve", lambda e, bm=bm, sc=sc: e.tensor_tensor(out=bm[:], in0=bm[:], in1=sc[:], op=ALU.mult), reads=[bmb, scb], writes=[bmb])
                    S.op("dve", lambda e, sm8=sm8, bm=bm: e.reduce_sum(out=sm8[:, 1, 0:1], in_=bm[:], axis=AX.X), reads=[bmb], writes=[sm8b])
                    S.op("dve", lambda e, sm8=sm8: e.reciprocal(out=sm8[:, 1, 1:2], in_=sm8[:, 1, 0:1]), reads=[sm8b], writes=[sm8b])
                    S.op("dve", lambda e, sm8=sm8, bm=bm, tg=tg: e.tensor_scalar(out=wall[:, tg, 0:64], in0=bm[:], scalar1=sm8[:, 1, 1:2], scalar2=2.5, op0=ALU.mult, op1=ALU.mult), reads=[bmb, sm8b], writes=[wall_b])
                for ex in range(N_EXP):
                    wg, wgb, wgs = wgu.next()
                    S.dma("sp", lambda e, wg=wg, ex=ex: e.dma_start(out=wg[:], in_=b_wge[ex]), wgs, reads=[dr_w], writes=[wgb])
                    wu, wub, wus = wgu.next()
                    S.dma("act", lambda e, wu=wu, ex=ex: e.dma_start(out=wu[:], in_=b_wue[ex]), wus, reads=[dr_w], writes=[wub])
                    wd, wdb, wds = wdr.next()
                    S.dma("sp", lambda e, wd=wd, ex=ex: e.dma_start(out=wd[:], in_=b_wde[ex]), wds, reads=[dr_w], writes=[wdb])
                    h1, h1b = h1T.next()
                    for ft in range(4):
                        pg, pgb = PS.next()
                        mmgroup(pg[:], pgb, [(wg[:, kc, ft * 128:(ft + 1) * 128], hTb[:, kc, :]) for kc in range(16)], [wgb, hTb_b])
                        pu, pub = PS.next()
                        mmgroup(pu[:], pub, [(wu[:, kc, ft * 128:(ft + 1) * 128], hTb[:, kc, :]) for kc in range(16)], [wub, hTb_b])
                        sg, sgb = Rsg2.next()
                        S.op("act", lambda e, sg=sg, pg=pg: e.activation(out=sg[:], in_=pg[:], func=AF.Silu), reads=[pgb], writes=[sgb])
                        S.op("dve", lambda e, h1=h1, sg=sg, pu=pu, ft=ft: e.tensor_tensor(out=h1[:, ft, :], in0=sg[:], in1=pu[:], op=ALU.mult), reads=[sgb, pub], writes=[h1b])
                    for tt in range(4):
                        tg = bi * 4 + tt
                        for nb in range(4):
                            py, pyb = PS.next()
                            mmgroup(py[:], pyb, [(h1[:, ft, tt * 128:(tt + 1) * 128], wd[:, ft, nb * 512:(nb + 1) * 512]) for ft in range(4)], [h1b, wdb])
                            ysl = yacc[:, tt, nb * 512:(nb + 1) * 512]
                            if ex == 0:
                                S.op("dve", lambda e, py=py, ysl=ysl, tg=tg, ex=ex: e.tensor_scalar(out=ysl, in0=py[:], scalar1=wall[:, tg, ex:ex + 1], scalar2=None, op0=ALU.mult), reads=[pyb, wall_b], writes=[yb_])
                            else:
                                S.op("dve", lambda e, py=py, ysl=ysl, tg=tg, ex=ex: e.scalar_tensor_tensor(out=ysl, in0=py[:], scalar=wall[:, tg, ex:ex + 1], in1=ysl, op0=ALU.mult, op1=ALU.add), reads=[pyb, wall_b, yb_], writes=[yb_])
                for tt in range(4):
                    S.op("dve", lambda e, hx=hx, tt=tt: e.scalar_tensor_tensor(out=yacc[:, tt, :], in0=hx[:, tt, :], scalar=DN_ALPHA, in1=yacc[:, tt, :], op0=ALU.mult, op1=ALU.add), reads=[hxb, yb_], writes=[yb_])
                    zs, zsb = Zs2.next()
                    for q4 in range(4):
                        S.op("dve", lambda e, zs=zs, tt=tt, q4=q4: e.bn_stats(out=zs[:, q4 * 6:(q4 + 1) * 6], in_=yacc[:, tt, q4 * 512:(q4 + 1) * 512]), reads=[yb_], writes=[zsb])
                    S.op("dve", lambda e, zs=zs: e.bn_aggr(out=zs[:, 24:26], in_=zs[:, 0:24]), reads=[zsb], writes=[zsb])
                    S.op("dve", lambda e, zs=zs: e.tensor_scalar(out=zs[:, 25:26], in0=zs[:, 25:26], scalar1=LN_EPS, scalar2=None, op0=ALU.add), reads=[zsb], writes=[zsb])
                    S.op("act", lambda e, zs=zs: e.activation(out=zs[:, 25:26], in_=zs[:, 25:26], func=AF.Sqrt), reads=[zsb], writes=[zsb])
                    S.op("dve", lambda e, zs=zs: e.reciprocal(out=zs[:, 25:26], in_=zs[:, 25:26]), reads=[zsb], writes=[zsb])
                    S.op("dve", lambda e, zs=zs, tt=tt: e.tensor_scalar(out=yacc[:, tt, :], in0=yacc[:, tt, :], scalar1=zs[:, 24:25], scalar2=zs[:, 25:26], op0=ALU.subtract, op1=ALU.mult), reads=[zsb, yb_], writes=[yb_])
                    S.op("pool", lambda e, tt=tt: e.tensor_tensor(out=yacc[:, tt, :], in0=yacc[:, tt, :], in1=ln2[:, 0, :], op=ALU.mult), reads=[yb_, c2b], writes=[yb_])
                    S.op("pool", lambda e, tt=tt: e.tensor_tensor(out=yacc[:, tt, :], in0=yacc[:, tt, :], in1=ln2[:, 1, :], op=ALU.add), reads=[yb_, c2b], writes=[yb_])
                S.dma("sp", lambda e, bi=bi: e.dma_start(out=out[bi * TB:(bi + 1) * TB, :].rearrange("(t p) d -> p t d", p=128), in_=yacc[:]), osrc, reads=[yb_], writes=[dr_out])
            S.barrier()
        S.finish()
        print("instructions:", S.n_inst, flush=True)
    return nc


def _consts(p):
    half = 128
    freq = (10000.0 ** (-np.arange(half, dtype=np.float64) / half))
    cosT = np.zeros((16, 128, TB), np.float32)
    sinT = np.zeros((16, 128, TB), np.float32)
    flags = np.zeros((128, 2, 16), np.float32)
    flags[:, 1, :] = 1.0
    for blk in range(16):
        q = p - 3 + blk // 4
        if q < 0:
            continue
        pos = q * NB + (blk % 4) * TB + np.arange(TB, dtype=np.float64)
        ang = (pos[None, :].astype(np.float32) * freq[:, None].astype(np.float32)).astype(np.float32)
        cosT[blk] = np.cos(ang)
        sinT[blk] = np.sin(ang)
        if q == 0 and blk % 4 == 0:
            flags[:, 0, blk] = 1.0
            flags[:, 1, blk] = 0.0
    return cosT, sinT, flags


def kernel(x, w_in, conv_w, conv_b, lru_wa, lru_ba, lru_wi, lru_bi, lru_lambda, ret_gn_gain,
           w_lru_out, w_ret_out, b_gate, w_o, ln1_g, ln1_b, w_router, router_bias,
           w_gate_e, w_up_e, w_down_e, w_gate_s, w_up_s, w_down_s, ln2_g, ln2_b):
    import time
    _t0 = time.time()
    f = lambda a: np.ascontiguousarray(np.asarray(a, dtype=np.float32))
    x = f(x)
    w_in = f(w_in)[0]
    kp = lambda w: w.reshape(16, 128, -1).transpose(1, 0, 2)
    wx, wy = w_in[:, 0:2048], w_in[:, 2048:4096]
    wq_, wk_ = w_in[:, 4096:6144], w_in[:, 6144:8192]
    wv_, wg_ = w_in[:, 8192:12288], w_in[:, 12288:16384]
    wga, wgb = w_in[:, 16384:18432], w_in[:, 18432:20480]
    w_in_l = np.stack([np.stack([kp(wx[:, h * 128:(h + 1) * 128]), kp(wy[:, h * 128:(h + 1) * 128])], 1) for h in range(16)])
    w_in_r = np.stack([np.stack([
        kp(wq_[:, h * 256:(h + 1) * 256]), kp(wk_[:, h * 256:(h + 1) * 256]),
        kp(wv_[:, h * 512:h * 512 + 256]), kp(wv_[:, h * 512 + 256:(h + 1) * 512]),
        kp(wg_[:, h * 512:h * 512 + 256]), kp(wg_[:, h * 512 + 256:(h + 1) * 512])]) for h in range(8)])
    wlo, wro, wo_ = f(w_lru_out)[0], f(w_ret_out)[0], f(w_o)[0]
    kp2 = lambda w, n: w.reshape(n, 128, -1).transpose(1, 0, 2)
    w3 = np.stack([np.concatenate([
        kp2(wlo[:, nt * 128:(nt + 1) * 128], 16), kp2(wro[:, nt * 128:(nt + 1) * 128], 32),
        kp2(wga[:, nt * 128:(nt + 1) * 128], 16), kp2(wgb[:, nt * 128:(nt + 1) * 128], 16)], 1) for nt in range(16)])
    wo_t = np.stack([kp(wo_[:, nb * 512:(nb + 1) * 512]) for nb in range(4)])
    wge = np.concatenate([f(w_gate_e)[0], f(w_gate_s)], 0).reshape(65, 16, 128, 512).transpose(0, 2, 1, 3)[:N_EXP]
    wue = np.concatenate([f(w_up_e)[0], f(w_up_s)], 0).reshape(65, 16, 128, 512).transpose(0, 2, 1, 3)[:N_EXP]
    wde = np.concatenate([f(w_down_e)[0], f(w_down_s)], 0).reshape(65, 4, 128, 2048).transpose(0, 2, 1, 3)[:N_EXP]
    wr_t = kp(f(w_router)[0])
    chp = lambda v: f(v).reshape(16, 128).T
    cw = f(conv_w)[0]
    lrup = np.stack([chp(cw[0]), chp(cw[1]), chp(cw[2]), chp(cw[3]), chp(conv_b), chp(lru_ba), chp(lru_bi), chp(lru_lambda)], 2)
    wai = np.stack([f(lru_wa)[0].transpose(1, 0, 2), f(lru_wi)[0].transpose(1, 0, 2)], 2)
    bgv = f(b_gate)[0]
    bg = np.stack([bgv[:2048].reshape(16, 128).T, bgv[2048:].reshape(16, 128).T], 1)
    bc = lambda v, n: np.ascontiguousarray(np.broadcast_to(f(v).reshape(1, -1), (128, n)))
    lnp = np.stack([bc(ln1_g, D), bc(ln1_b, D), bc(ln2_g, D), bc(ln2_b, D)], 1)
    idx = np.arange(128, dtype=np.float64)
    dmaskT = np.zeros((128, 8, 128), np.float32)
    xizeta = np.zeros((128, 2, 8), np.float32)
    for h in range(8):
        lg = np.log1p(-np.exp2(-5.0 - h))
        diff = idx[None, :] - idx[:, None]
        dmaskT[:, h, :] = np.where(diff >= 0, np.exp(np.maximum(diff, 0) * lg), 0.0) / 16.0
        xizeta[:, 0, h] = np.exp((idx + 1.0) * lg)
        xizeta[:, 1, h] = np.exp((127.0 - idx) * lg) / 16.0
    shared = dict(w_in_l=w_in_l, w_in_r=w_in_r, w3=w3, wo=wo_t, wge=wge, wue=wue, wde=wde, wr=wr_t,
                  lrup=lrup, wai=wai, bg=bg, dmaskT=dmaskT, xizeta=xizeta, gain_bc=bc(ret_gn_gain, 4096),
                  lnp=lnp, rb_bc=bc(router_bias, 64), ident=np.eye(128, dtype=np.float32))
    shared = {k: np.ascontiguousarray(v, dtype=np.float32) for k, v in shared.items()}
    in_maps = []
    for c in range(8):
        b, p = divmod(c, 4)
        cosT, sinT, flags = _consts(p)
        xT = np.zeros((16, 128, 16, TB), np.float32)
        for blk in range(16):
            q = p - 3 + blk // 4
            if q < 0:
                continue
            t0 = q * NB + (blk % 4) * TB
            xT[blk] = x[b, t0:t0 + TB, :].reshape(TB, 16, 128).transpose(2, 1, 0)
        m = dict(shared)
        m.update(xT=xT, xres=np.ascontiguousarray(x[b, p * NB:(p + 1) * NB, :]), cosT=cosT, sinT=sinT, flags=flags)
        in_maps.append(m)
    print("[kernel] host layout %.1fs" % (time.time() - _t0), flush=True)
    nc = build()
    print("[kernel] build %.1fs" % (time.time() - _t0), flush=True)
    res = run_bass_kernel_spmd(nc, in_maps, core_ids=list(range(8)))
    print("[kernel] run done %.1fs" % (time.time() - _t0), flush=True)
    if os.environ.get("MK_DBG", "0") == "1":
        _LAST.clear()
        _LAST.append(res.results)
    outp = np.zeros((2, SEQ, D), np.float32)
    for c in range(8):
        b, p = divmod(c, 4)
        outp[b, p * NB:(p + 1) * NB, :] = res.results[c]["out"]
    return outp
```

```python
import os
import types
import numpy as np
from contextlib import ExitStack
import concourse.bass as bass
import concourse.mybir as mybir
from concourse.bass_utils import run_bass_kernel_spmd

F32 = mybir.dt.float32
BF16 = mybir.dt.bfloat16
AF = mybir.ActivationFunctionType
ALU = mybir.AluOpType
AX = mybir.AxisListType

D = 2048
SEQ = 8192
NB = 2048
TB = 512
NE = 64
DN_ALPHA = 2.0 ** 0.25
LN_EPS = 1e-5
GAMMA = [1.0 - 2.0 ** (-5.0 - h) for h in range(8)]
_LAST = []
N_EXP = int(os.environ.get("MK_NEXP", "65"))
_PH = os.environ.get("MK_PH", "0,1,1b,2").split(",")
_BLKS = [int(v) for v in os.environ.get("MK_BLKS", ",".join(str(i) for i in range(16))).split(",")]
_LH = int(os.environ.get("MK_LH", "16"))
_RH = int(os.environ.get("MK_RH", "8"))


def freeze(fn):
    if fn.__closure__ is None:
        return fn
    cells = []
    for c in fn.__closure__:
        try:
            cells.append(types.CellType(c.cell_contents))
        except ValueError:
            cells.append(c)
    return types.FunctionType(fn.__code__, fn.__globals__, fn.__name__, fn.__defaults__, tuple(cells))


class Src:
    def __init__(self, sem, step, name):
        self.sem, self.step, self.count, self.name = sem, step, 0, name


class Buf:
    __slots__ = ("w", "r", "name")

    def __init__(self, name=""):
        self.w, self.r, self.name = None, {}, name


class Sched:
    ENGS = ("pe", "act", "dve", "pool", "sp")

    def __init__(self, nc, stack):
        self.nc, self.stack = nc, stack
        self.streams = {e: [] for e in self.ENGS}
        self.src = {}
        for e in self.ENGS:
            self.src[e] = Src(stack.enter_context(nc.semaphore("s_" + e)), 1, e)
        self.seen = {e: {} for e in self.ENGS}
        self.dma_srcs = []
        self.n_inst = 0

    def dma_src(self, name):
        s = Src(self.stack.enter_context(self.nc.semaphore("d_" + name)), 16, name)
        self.dma_srcs.append(s)
        return s

    def _waits(self, eng, reads, writes):
        need = {}
        me = self.src[eng]

        def add(ev):
            if ev is None:
                return
            s, c = ev
            if s is me and eng == "pe":
                return
            if need.get(s, 0) < c:
                need[s] = c

        for b in reads:
            add(b.w)
        for b in writes:
            add(b.w)
            for s, c in b.r.items():
                if s is not me:
                    add((s, c))
        out = []
        seen = self.seen[eng]
        for s, c in need.items():
            if seen.get(s, 0) < c:
                seen[s] = c
                out.append((s, c))
        return out

    def op(self, eng, fn, reads=(), writes=()):
        self.group(eng, [fn], reads, writes)

    def group(self, eng, fns, reads=(), writes=()):
        waits = self._waits(eng, reads, writes)
        me = self.src[eng]
        me.count += 1
        cnt = me.count
        st = self.streams[eng]
        for i, fn in enumerate(fns):
            last = i == len(fns) - 1
            st.append((waits if i == 0 else (), freeze(fn), me if last else None, 1))
        for b in reads:
            b.r[me] = cnt
        for b in writes:
            b.w = (me, cnt)
            b.r = {}
        self.n_inst += len(fns)

    def dma(self, q, fn, dsrc, reads=(), writes=()):
        waits = self._waits(q, reads, writes)
        dsrc.count += 16
        cnt = dsrc.count
        self.streams[q].append((waits, freeze(fn), dsrc, 16))
        for b in reads:
            b.r[dsrc] = cnt
        for b in writes:
            b.w = (dsrc, cnt)
            b.r = {}
        self.n_inst += 1

    def barrier(self):
        allsrc = [self.src[e] for e in self.ENGS] + self.dma_srcs
        for e in self.ENGS:
            waits = []
            for s in allsrc:
                if s is self.src[e] or s.count == 0:
                    continue
                if self.seen[e].get(s, 0) < s.count:
                    self.seen[e][s] = s.count
                    waits.append((s, s.count))
            if waits:
                self.streams[e].append((waits, None, None, 0))

    def finish(self):
        nc = self.nc
        eh = {"pe": "tensor", "act": "scalar", "dve": "vector", "pool": "gpsimd", "sp": "sync"}
        self.barrier()
        with nc.Block() as block:
            for e in self.ENGS:
                stream = self.streams[e]

                def body(engine, stream=stream):
                    for waits, fn, src, inc in stream:
                        for s, c in waits:
                            engine.wait_ge(s.sem, c)
                        if fn is not None:
                            ins = fn(engine)
                            if src is not None:
                                ins.then_inc(src.sem, inc)

                getattr(block, eh[e])(body)


class Ring:
    def __init__(self, nc, st, name, n, shape, dt, psum=False):
        self.t, self.b, self.i = [], [], 0
        for k in range(n):
            nm = "%s%d" % (name, k)
            if psum:
                self.t.append(st.enter_context(nc.psum_tensor(nm, shape, dt)))
            else:
                self.t.append(st.enter_context(nc.sbuf_tensor(nm, shape, dt)))
            self.b.append(Buf(nm))

    def next(self):
        k = self.i % len(self.t)
        self.i += 1
        return self.t[k], self.b[k]


class DRing:
    def __init__(self, S, nc, st, name, n, shape, dt):
        self.r = Ring(nc, st, name, n, shape, dt)
        self.s = [S.dma_src("%s%d" % (name, k)) for k in range(n)]

    def next(self):
        k = self.r.i % len(self.s)
        t, b = self.r.next()
        return t, b, self.s[k]


def build():
    nc = bass.Bass("TRN2", target_bir_lowering=False)
    ein = lambda n, s, d=F32: nc.dram_tensor(n, list(s), d, kind="ExternalInput").ap()
    scr = lambda n, s, d=BF16: nc.dram_tensor(n, list(s), d).ap()
    xT = ein("xT", [16, 128, 16, TB])
    xres = ein("xres", [NB, D])
    w_in_l = ein("w_in_l", [16, 128, 2, 16, 128])
    w_in_r = ein("w_in_r", [8, 6, 128, 16, 256])
    w3 = ein("w3", [16, 128, 80, 128])
    wo = ein("wo", [4, 128, 16, 512])
    wge = ein("wge", [65, 128, 16, 512])
    wue = ein("wue", [65, 128, 16, 512])
    wde = ein("wde", [65, 128, 4, 2048])
    wr = ein("wr", [128, 16, 64])
    lrup = ein("lrup", [128, 16, 8])
    wai = ein("wai", [128, 16, 2, 128])
    bg = ein("bg", [128, 2, 16])
    cosT = ein("cosT", [16, 128, TB])
    sinT = ein("sinT", [16, 128, TB])
    flags = ein("flags", [128, 2, 16])
    dmaskT = ein("dmaskT", [128, 8, 128])
    xizeta = ein("xizeta", [128, 2, 8])
    gain_bc = ein("gain_bc", [128, 4096])
    lnp = ein("lnp", [128, 4, D])
    rb_bc = ein("rb_bc", [128, 64])
    ident = ein("ident", [128, 128])
    ustrict = ein("ustrict", [128, 2, 128])
    iota512 = ein("iota512", [128, 512])
    iota64 = ein("iota64", [128, 2, 64])
    tokcol = ein("tokcol", [128, 16, 2])
    out = nc.dram_tensor("out", [NB, D], F32, kind="ExternalOutput").ap()
    b_in_l = scr("b_in_l", [16, 128, 2, 16, 128])
    b_in_r = scr("b_in_r", [8, 6, 128, 16, 256])
    b_w3 = scr("b_w3", [16, 128, 80, 128])
    b_wo = scr("b_wo", [4, 128, 16, 512])
    b_wge = scr("b_wge", [1, 128, 16, 512])
    b_wue = scr("b_wue", [1, 128, 16, 512])
    b_wde = scr("b_wde", [1, 128, 4, 2048])
    _dbg = os.environ.get("MK_DBG", "0") == "1"
    dscr = (lambda n, s, d=BF16: nc.dram_tensor(n, list(s), d, kind="ExternalOutput").ap()) if _dbg else scr
    Hs = dscr("Hs", [NB, D], F32)
    UA = dscr("UA", [4, 128, 16, TB])
    Hb = scr("Hb", [NB + 128, D])
    YS = scr("YS", [NB, D], F32)
    Yx = scr("Yx", [64 * 512, D])
    UB = dscr("UB", [4, 128, 32, TB])

    with ExitStack() as top:
        S = Sched(nc, top)
        dr_w = Buf("dram_w")
        dr_H = Buf("dram_H")
        dr_out = Buf("dram_out")
        PS = Ring(nc, top, "ps", 6, [128, 512], F32, psum=True)
        PT = Ring(nc, top, "pt", 2, [128, 8, 128], BF16, psum=True)

        dout = S.dma_src("dout")

        with ExitStack() as st:
            stg = DRing(S, nc, st, "cst", 3, [128, 4096], F32)
            cvb = Ring(nc, st, "cvb", 3, [128, 4096], BF16)
            cvs = [S.dma_src("cvo%d" % k) for k in range(3)]
            cnt = [0]

            def convert(src2d, dst2d, ncols):
                for c0 in range(0, ncols, 4096):
                    cw = min(4096, ncols - c0)
                    t, tb, ts = stg.next()
                    S.dma("sp", lambda e, t=t, c0=c0, cw=cw: e.dma_start(out=t[:, 0:cw], in_=src2d[:, c0:c0 + cw]), ts, writes=[tb])
                    k = cnt[0] % 3
                    o, ob = cvb.next()
                    eng = ("pool", "act", "dve")[cnt[0] % 3]
                    cnt[0] += 1
                    if eng == "act":
                        S.op("act", lambda e, o=o, t=t, cw=cw: e.copy(out=o[:, 0:cw], in_=t[:, 0:cw]), reads=[tb], writes=[ob])
                    else:
                        S.op(eng, lambda e, o=o, t=t, cw=cw: e.tensor_copy(out=o[:, 0:cw], in_=t[:, 0:cw]), reads=[tb], writes=[ob])
                    S.dma("act" if k == 1 else "sp", lambda e, o=o, c0=c0, cw=cw: e.dma_start(out=dst2d[:, c0:c0 + cw], in_=o[:, 0:cw]), cvs[k], reads=[ob], writes=[])

            for g in range(16 if "0" in _PH else 0):
                convert(w_in_l[g].rearrange("p a k c -> p (a k c)"), b_in_l[g].rearrange("p a k c -> p (a k c)"), 4096)
            for h in range(8 if "0" in _PH else 0):
                for j in range(6):
                    convert(w_in_r[h, j].rearrange("p k c -> p (k c)"), b_in_r[h, j].rearrange("p k c -> p (k c)"), 4096)
            for g in range(16 if "0" in _PH else 0):
                convert(w3[g].rearrange("p k c -> p (k c)"), b_w3[g].rearrange("p k c -> p (k c)"), 10240)
            for g in range(4 if "0" in _PH else 0):
                convert(wo[g].rearrange("p k c -> p (k c)"), b_wo[g].rearrange("p k c -> p (k c)"), 8192)
            if "0" in _PH:
                convert(wge[64].rearrange("p k c -> p (k c)"), b_wge[0].rearrange("p k c -> p (k c)"), 8192)
                convert(wue[64].rearrange("p k c -> p (k c)"), b_wue[0].rearrange("p k c -> p (k c)"), 8192)
                convert(wde[64].rearrange("p k c -> p (k c)"), b_wde[0].rearrange("p k c -> p (k c)"), 8192)
            S.barrier()

        with ExitStack() as st:
            sb = lambda n, s, d=F32: st.enter_context(nc.sbuf_tensor(n, list(s), d))
            cq = S.dma_src("cq")
            cb_ = Buf("consts")
            lp = sb("lp", [128, 16, 8])
            wab = sb("wab", [128, 16, 2, 128], BF16)
            bgt = sb("bgt", [128, 2, 16])
            flg = sb("flg", [128, 2, 16])
            dmk = sb("dmk", [128, 8, 128])
            xz = sb("xz", [128, 2, 8])
            idf = sb("idf", [128, 128])
            idb = sb("idb", [128, 128], BF16)
            lrc = sb("lrc", [128, 2, 16])
            ltmp = sb("ltmp", [128, 16])
            hst = sb("hst", [128, 16])
            carry = sb("carry", [128, 16, 4])
            Sst = sb("Sst", [128, 8, 2, 512])
            for t_, a_ in ((lp, lrup), (bgt, bg), (flg, flags), (dmk, dmaskT), (xz, xizeta), (idf, ident)):
                S.dma("sp", lambda e, t_=t_, a_=a_: e.dma_start(out=t_[:], in_=a_), cq, writes=[cb_])
            S.op("dve", lambda e: e.tensor_copy(out=idb[:], in_=idf[:]), reads=[cb_], writes=[cb_])
            S.op("act", lambda e: e.activation(out=ltmp[:], in_=lp[:, :, 7], func=AF.Exp, scale=-1.0), reads=[cb_], writes=[cb_])
            S.op("act", lambda e: e.activation(out=ltmp[:], in_=ltmp[:], func=AF.Ln, bias=1.0), reads=[cb_], writes=[cb_])
            S.op("dve", lambda e: e.tensor_scalar(out=lrc[:, 0, :], in0=ltmp[:], scalar1=-8.0, scalar2=None, op0=ALU.mult), reads=[cb_], writes=[cb_])
            S.op("dve", lambda e: e.tensor_scalar(out=lrc[:, 1, :], in0=ltmp[:], scalar1=-16.0, scalar2=None, op0=ALU.mult), reads=[cb_], writes=[cb_])
            S.op("pool", lambda e: e.memset(hst[:], 0.0), writes=[cb_])
            S.op("pool", lambda e: e.memset(carry[:], 0.0), writes=[cb_])
            S.op("pool", lambda e: e.memset(Sst[:], 0.0), writes=[cb_])
            hst_b = [Buf("hst%d" % h) for h in range(16)]
            car_b = [Buf("car%d" % h) for h in range(16)]
            S_b = [Buf("S%d" % h) for h in range(8)]
            for bl in hst_b + car_b + S_b:
                bl.w = cb_.w

            xstg = DRing(S, nc, st, "xstg", 2, [128, 4, TB], F32)
            for hf in range(2):
                t, tb, ts = xstg.next()
                S.dma("sp", lambda e, t=t, hf=hf: e.dma_start(out=t[:].rearrange("p a b -> p (a b)"), in_=wai[:, hf * 8:(hf + 1) * 8].rearrange("p h a o -> p (h a o)")), ts, writes=[tb])
                S.op("dve", lambda e, t=t, hf=hf: e.tensor_copy(out=wab[:, hf * 8:(hf + 1) * 8].rearrange("p h a o -> p (h a o)"), in_=t[:].rearrange("p a b -> p (a b)")), reads=[tb], writes=[cb_])
            xTb = Ring(nc, st, "xTb", 1, [128, 16, TB], BF16)
            cs = DRing(S, nc, st, "cs", 1, [128, 2, TB], F32)
            wl = DRing(S, nc, st, "wl", 2, [128, 2, 16, 128], BF16)
            wq = DRing(S, nc, st, "wq", 5, [128, 16, 256], BF16)
            L = {n: Ring(nc, st, "l_" + n, 1, [128, TB], F32) for n in ("xa", "r", "i", "a", "m", "h", "gy")}
            Lxs = Ring(nc, st, "l_xs", 1, [128, TB + 4], F32)
            Lxab = Ring(nc, st, "l_xab", 2, [128, TB], BF16)
            Lin = Ring(nc, st, "l_in", 2, [128, 1], F32)
            Rt = Ring(nc, st, "r_t", 4, [128, TB], F32)
            Rq = Ring(nc, st, "r_q", 2, [128, 2, TB], BF16)
            Rk = Ring(nc, st, "r_k", 2, [128, 2, TB], BF16)
            Rkz = Ring(nc, st, "r_kz", 2, [128, 256], BF16)
            Rv = Ring(nc, st, "r_v", 2, [128, 512], BF16)
            Rs = Ring(nc, st, "r_s", 2, [128, 128], BF16)
            Ro = Ring(nc, st, "r_o", 2, [128, 512], F32)
            Rsg = Ring(nc, st, "r_sg", 1, [128, 512], F32)
            Rub = Ring(nc, st, "r_ub", 2, [128, 512], BF16)
            Rst = Ring(nc, st, "r_st", 2, [128, 8], F32)
            Sb = Ring(nc, st, "Sb", 2, [128, 2, 512], BF16)
            gn = DRing(S, nc, st, "gn", 2, [128, 512], F32)
            uar = Ring(nc, st, "uar", 2, [128, TB], BF16)
            ubr = Ring(nc, st, "ubr", 2, [128, 4, TB], BF16)
            uas = [S.dma_src("uas%d" % k) for k in range(2)]
            ubs = [S.dma_src("ubs%d" % k) for k in range(2)]
            uacnt = [0, 0]

            print("phase1 sbuf remaining", nc.sbuf_bytes_remaining, flush=True)

            def mmgroup(ps, psb, pairs, reads):
                n = len(pairs)
                S.group("pe", [
                    (lambda e, l=l, r=r, i=i: e.matmul(ps, lhsT=l, rhs=r, start=(i == 0), stop=(i == n - 1)))
                    for i, (l, r) in enumerate(pairs)], reads=reads, writes=[psb])

            for blk in (_BLKS if "1" in _PH else []):
                main = blk >= 12
                xb, xbb = xTb.next()
                for hf in range(4):
                    t, tb, ts = xstg.next()
                    S.dma("sp", lambda e, t=t, hf=hf, blk=blk: e.dma_start(out=t[:], in_=xT[blk, :, hf * 4:(hf + 1) * 4, :]), ts, writes=[tb])
                    S.op("pool", lambda e, t=t, xb=xb, hf=hf: e.tensor_copy(out=xb[:, hf * 4:(hf + 1) * 4, :], in_=t[:]), reads=[tb], writes=[xbb])
                ct, ctb, cts = cs.next()
                S.dma("act", lambda e, ct=ct, blk=blk: e.dma_start(out=ct[:, 0, :], in_=cosT[blk]), cts, writes=[ctb])
                S.dma("act", lambda e, ct=ct, blk=blk: e.dma_start(out=ct[:, 1, :], in_=sinT[blk]), cts, writes=[ctb])

                for h in range(_LH):
                    w, wb_, ws = wl.next()
                    if main:
                        S.dma("sp", lambda e, w=w, h=h: e.dma_start(out=w[:], in_=b_in_l[h]), ws, reads=[dr_w], writes=[wb_])
                    else:
                        S.dma("sp", lambda e, w=w, h=h: e.dma_start(out=w[:, 0], in_=b_in_l[h, :, 0]), ws, reads=[dr_w], writes=[wb_])
                    px, pxb = PS.next()
                    mmgroup(px[:], pxb, [(w[:, 0, kc, :], xb[:, kc, :]) for kc in range(16)], [wb_, xbb])
                    xs, xsb = Lxs.next()
                    S.op("act", lambda e, xs=xs, px=px: e.copy(out=xs[:, 4:TB + 4], in_=px[:]), reads=[pxb], writes=[xsb])
                    S.op("pool", lambda e, xs=xs, h=h: e.tensor_copy(out=xs[:, 0:4], in_=carry[:, h, :]), reads=[car_b[h]], writes=[xsb])
                    S.op("pool", lambda e, xs=xs, h=h: e.tensor_copy(out=carry[:, h, :], in_=xs[:, TB:TB + 4]), reads=[xsb], writes=[car_b[h]])
                    xa, xab_ = L["xa"].next()
                    S.op("dve", lambda e, xa=xa, xs=xs, h=h: e.tensor_scalar(out=xa[:], in0=xs[:, 4:TB + 4], scalar1=lp[:, h, 3:4], scalar2=lp[:, h, 4:5], op0=ALU.mult, op1=ALU.add), reads=[xsb], writes=[xab_])
                    for j in range(3):
                        S.op("dve", lambda e, xa=xa, xs=xs, h=h, j=j: e.scalar_tensor_tensor(out=xa[:], in0=xs[:, 1 + j:TB + 1 + j], scalar=lp[:, h, j:j + 1], in1=xa[:], op0=ALU.mult, op1=ALU.add), reads=[xsb, xab_], writes=[xab_])
                    xq, xqb = Lxab.next()
                    S.op("act", lambda e, xq=xq, xa=xa: e.copy(out=xq[:], in_=xa[:]), reads=[xab_], writes=[xqb])
                    pr, prb = PS.next()
                    S.op("pe", lambda e, pr=pr, xq=xq, h=h: e.matmul(pr[:], lhsT=wab[:, h, 0, :], rhs=xq[:], start=True, stop=True), reads=[xqb], writes=[prb])
                    pi, pib = PS.next()
                    S.op("pe", lambda e, pi=pi, xq=xq, h=h: e.matmul(pi[:], lhsT=wab[:, h, 1, :], rhs=xq[:], start=True, stop=True), reads=[xqb], writes=[pib])
                    r, rb = L["r"].next()
                    S.op("act", lambda e, r=r, pr=pr, h=h: e.activation(out=r[:], in_=pr[:], func=AF.Sigmoid, bias=lp[:, h, 5:6]), reads=[prb], writes=[rb])
                    ig, igb = L["i"].next()
                    S.op("act", lambda e, ig=ig, pi=pi, h=h: e.activation(out=ig[:], in_=pi[:], func=AF.Sigmoid, bias=lp[:, h, 6:7]), reads=[pib], writes=[igb])
                    a, ab = L["a"].next()
                    S.op("act", lambda e, a=a, r=r, h=h: e.activation(out=a[:], in_=r[:], func=AF.Exp, scale=lrc[:, 0, h:h + 1]), reads=[rb], writes=[ab])
                    m, mb = L["m"].next()
                    S.op("act", lambda e, m=m, r=r, h=h: e.activation(out=m[:], in_=r[:], func=AF.Exp, scale=lrc[:, 1, h:h + 1]), reads=[rb], writes=[mb])
                    S.op("act", lambda e, m=m: e.activation(out=m[:], in_=m[:], func=AF.Sqrt, scale=-1.0, bias=1.0), reads=[mb], writes=[mb])
                    S.op("dve", lambda e, m=m, blk=blk: e.tensor_scalar(out=m[:, 0:1], in0=m[:, 0:1], scalar1=flg[:, 1, blk:blk + 1], scalar2=flg[:, 0, blk:blk + 1], op0=ALU.mult, op1=ALU.add), reads=[mb], writes=[mb])
                    S.op("dve", lambda e, m=m, ig=ig: e.tensor_tensor(out=m[:], in0=m[:], in1=ig[:], op=ALU.mult), reads=[mb, igb], writes=[mb])
                    S.op("dve", lambda e, m=m, xa=xa: e.tensor_tensor(out=m[:], in0=m[:], in1=xa[:], op=ALU.mult), reads=[mb, xab_], writes=[mb])
                    ini, inib = Lin.next()
                    S.op("dve", lambda e, ini=ini, h=h, blk=blk: e.tensor_tensor(out=ini[:], in0=hst[:, h:h + 1], in1=flg[:, 1, blk:blk + 1], op=ALU.mult), reads=[hst_b[h]], writes=[inib])
                    hh, hb = L["h"].next()
                    S.op("dve", lambda e, hh=hh, a=a, m=m, ini=ini: e.tensor_tensor_scan(out=hh[:], data0=a[:], data1=m[:], initial=ini[:], op0=ALU.mult, op1=ALU.add), reads=[ab, mb, inib], writes=[hb])
                    S.op("pool", lambda e, hh=hh, h=h: e.tensor_copy(out=hst[:, h:h + 1], in_=hh[:, TB - 1:TB]), reads=[hb], writes=[hst_b[h]])
                    if main:
                        py, pyb = PS.next()
                        mmgroup(py[:], pyb, [(w[:, 1, kc, :], xb[:, kc, :]) for kc in range(16)], [wb_, xbb])
                        gy, gyb = L["gy"].next()
                        S.op("act", lambda e, gy=gy, py=py: e.activation(out=gy[:], in_=py[:], func=AF.Gelu_apprx_tanh), reads=[pyb], writes=[gyb])
                        k_ = uacnt[0] % 2
                        uacnt[0] += 1
                        ut, utb = uar.next()
                        S.op("dve", lambda e, gy=gy, hh=hh, ut=ut: e.tensor_tensor(out=ut[:], in0=hh[:], in1=gy[:], op=ALU.mult), reads=[hb, gyb], writes=[utb])
                        S.dma("act", lambda e, ut=ut, h=h, blk=blk: e.dma_start(out=UA[blk - 12, :, h, :], in_=ut[:]), uas[k_], reads=[utb], writes=[])

                for h in range(_RH):
                    gc = GAMMA[h] ** 128
                    wts = {}
                    for j, nm in enumerate(("q", "k", "v0", "v1", "g0", "g1")):
                        if not main and nm in ("q", "g0", "g1"):
                            continue
                        w, wb_, ws = wq.next() if nm in ("q", "k") else (None, None, None)
                        if nm in ("q", "k"):
                            S.dma("sp", lambda e, w=w, h=h, j=j: e.dma_start(out=w[:], in_=b_in_r[h, j]), ws, reads=[dr_w], writes=[wb_])
                            wts[nm] = (w, wb_)
                    def proj_rope(nm, ring):
                        w, wb_ = wts[nm]
                        p0, p0b = PS.next()
                        mmgroup(p0[:], p0b, [(w[:, kc, 0:128], xb[:, kc, :]) for kc in range(16)], [wb_, xbb])
                        p1, p1b = PS.next()
                        mmgroup(p1[:], p1b, [(w[:, kc, 128:256], xb[:, kc, :]) for kc in range(16)], [wb_, xbb])
                        o, ob = ring.next()
                        t1, t1b = Rt.next()
                        t2, t2b = Rt.next()
                        S.op("dve", lambda e: e.tensor_tensor(out=t1[:], in0=p0[:], in1=ct[:, 0, :], op=ALU.mult), reads=[p0b, ctb], writes=[t1b])
                        S.op("dve", lambda e: e.tensor_tensor(out=t2[:], in0=p1[:], in1=ct[:, 1, :], op=ALU.mult), reads=[p1b, ctb], writes=[t2b])
                        S.op("pool", lambda e: e.tensor_tensor(out=o[:, 0, :], in0=t1[:], in1=t2[:], op=ALU.subtract), reads=[t1b, t2b], writes=[ob])
                        t3, t3b = Rt.next()
                        t4, t4b = Rt.next()
                        S.op("dve", lambda e: e.tensor_tensor(out=t3[:], in0=p0[:], in1=ct[:, 1, :], op=ALU.mult), reads=[p0b, ctb], writes=[t3b])
                        S.op("dve", lambda e: e.tensor_tensor(out=t4[:], in0=p1[:], in1=ct[:, 0, :], op=ALU.mult), reads=[p1b, ctb], writes=[t4b])
                        S.op("pool", lambda e: e.tensor_tensor(out=o[:, 1, :], in0=t3[:], in1=t4[:], op=ALU.add), reads=[t3b, t4b], writes=[ob])
                        return o, ob

                    kr, krb = proj_rope("k", Rk)
                    if main:
                        qr, qrb = proj_rope("q", Rq)
                    wv = []
                    for j in (2, 3):
                        w, wb_, ws = wq.next()
                        S.dma("sp", lambda e, w=w, h=h, j=j: e.dma_start(out=w[:], in_=b_in_r[h, j]), ws, reads=[dr_w], writes=[wb_])
                        wv.append((w, wb_))
                    wg_ = []
                    if main:
                        gt, gtb, gts = gn.next()
                        S.dma("act", lambda e, gt=gt, h=h: e.dma_start(out=gt[:], in_=gain_bc[:, h * 512:(h + 1) * 512]), gts, writes=[gtb])
                    if main:
                        k_ = uacnt[1] % 2
                        uacnt[1] += 1
                        ubt, ubtb = ubr.next()
                    for c in range(4):
                        tok = slice(c * 128, (c + 1) * 128)
                        pv, pvb = PS.next()
                        for hf in range(2):
                            n = 16
                            S.group("pe", [
                                (lambda e, kc=kc, hf=hf: e.matmul(pv[:, hf * 256:(hf + 1) * 256], lhsT=xb[:, kc, tok], rhs=wv[hf][0][:, kc, :], start=(kc == 0), stop=(kc == 15)))
                                for kc in range(16)], reads=[xbb, wv[hf][1]], writes=[pvb])
                        vb, vbb = Rv.next()
                        S.op("act", lambda e, vb=vb, pv=pv: e.copy(out=vb[:], in_=pv[:]), reads=[pvb], writes=[vbb])
                        kz, kzb = Rkz.next()
                        pt, ptb_ = PT.next()
                        S.group("pe", [(lambda e, pt=pt, j=j: e.transpose(pt[:, j, :], kr[:, j, tok], idb[:])) for j in range(2)], reads=[krb], writes=[ptb_])
                        S.op("dve", lambda e, pt=pt, kz=kz: e.tensor_scalar(out=kz[:].rearrange("p (a b) -> p a b", b=128), in0=pt[:, 0:2, :], scalar1=xz[:, 1, h:h + 1], scalar2=None, op0=ALU.mult), reads=[ptb_], writes=[kzb])
                        if main:
                            psc, pscb = PS.next()
                            mmgroup(psc[:, 0:128], pscb, [(kr[:, j, tok], qr[:, j, tok]) for j in range(2)], [krb, qrb])
                            sm, smb = Rs.next()
                            S.op("dve", lambda e, sm=sm, psc=psc: e.tensor_tensor(out=sm[:], in0=psc[:, 0:128], in1=dmk[:, h, :], op=ALU.mult), reads=[pscb], writes=[smb])
                            po, pob = PS.next()
                            S.op("pe", lambda e, po=po, sm=sm, vb=vb: e.matmul(po[:], lhsT=sm[:], rhs=vb[:], start=True, stop=True), reads=[smb, vbb], writes=[pob])
                            sbf, sbfb = Sb.next()
                            S.op("pool", lambda e, sbf=sbf: e.tensor_copy(out=sbf[:], in_=Sst[:, h]), reads=[S_b[h]], writes=[sbfb])
                            pc, pcb = PS.next()
                            mmgroup(pc[:], pcb, [(qr[:, j, tok], sbf[:, j, :]) for j in range(2)], [qrb, sbfb])
                            o1, o1b = Ro.next()
                            S.op("act", lambda e, o1=o1, po=po: e.copy(out=o1[:], in_=po[:]), reads=[pob], writes=[o1b])
                            o2, o2b = Ro.next()
                            S.op("dve", lambda e, o2=o2, pc=pc, o1=o1: e.scalar_tensor_tensor(out=o2[:], in0=pc[:], scalar=xz[:, 0, h:h + 1], in1=o1[:], op0=ALU.mult, op1=ALU.add), reads=[pcb, o1b], writes=[o2b])
                            stt, sttb = Rst.next()
                            S.op("dve", lambda e, stt=stt, o2=o2: e.bn_stats(out=stt[:, 0:6], in_=o2[:]), reads=[o2b], writes=[sttb])
                            S.op("dve", lambda e, stt=stt: e.bn_aggr(out=stt[:, 6:8], in_=stt[:, 0:6]), reads=[sttb], writes=[sttb])
                            S.op("dve", lambda e, stt=stt: e.tensor_scalar(out=stt[:, 7:8], in0=stt[:, 7:8], scalar1=LN_EPS, scalar2=None, op0=ALU.add), reads=[sttb], writes=[sttb])
                            S.op("act", lambda e, stt=stt: e.activation(out=stt[:, 7:8], in_=stt[:, 7:8], func=AF.Sqrt), reads=[sttb], writes=[sttb])
                            S.op("dve", lambda e, stt=stt: e.reciprocal(out=stt[:, 7:8], in_=stt[:, 7:8]), reads=[sttb], writes=[sttb])
                            S.op("dve", lambda e, stt=stt, o2=o2: e.tensor_scalar(out=o2[:], in0=o2[:], scalar1=stt[:, 6:7], scalar2=stt[:, 7:8], op0=ALU.subtract, op1=ALU.mult), reads=[sttb, o2b], writes=[o2b])
                            S.op("pool", lambda e, o2=o2, gt=gt: e.tensor_tensor(out=o2[:], in0=o2[:], in1=gt[:], op=ALU.mult), reads=[o2b, gtb], writes=[o2b])
                            if c == 0:
                                for j in (4, 5):
                                    w, wb_, ws = wq.next()
                                    S.dma("sp", lambda e, w=w, h=h, j=j: e.dma_start(out=w[:], in_=b_in_r[h, j]), ws, reads=[dr_w], writes=[wb_])
                                    wg_.append((w, wb_))
                            pg, pgb = PS.next()
                            for hf in range(2):
                                S.group("pe", [
                                    (lambda e, kc=kc, hf=hf: e.matmul(pg[:, hf * 256:(hf + 1) * 256], lhsT=xb[:, kc, tok], rhs=wg_[hf][0][:, kc, :], start=(kc == 0), stop=(kc == 15)))
                                    for kc in range(16)], reads=[xbb, wg_[hf][1]], writes=[pgb])
                            sg, sgb = Rsg.next()
                            S.op("act", lambda e, sg=sg, pg=pg: e.activation(out=sg[:], in_=pg[:], func=AF.Silu), reads=[pgb], writes=[sgb])
                            ub, ubb = Rub.next()
                            S.op("dve", lambda e, ub=ub, o2=o2, sg=sg: e.tensor_tensor(out=ub[:], in0=o2[:], in1=sg[:], op=ALU.mult), reads=[o2b, sgb], writes=[ubb])
                            pt, ptb_ = PT.next()
                            S.group("pe", [(lambda e, pt=pt, ub=ub, q4=q4: e.transpose(pt[:, q4, :], ub[:, q4 * 128:(q4 + 1) * 128], idb[:])) for q4 in range(4)], reads=[ubb], writes=[ptb_])
                            S.op("act", lambda e, pt=pt: e.copy(out=ubt[:, :, tok], in_=pt[:, 0:4, :]), reads=[ptb_], writes=[ubtb])
                        for j in range(2):
                            pd, pdb = PS.next()
                            S.op("pe", lambda e, pd=pd, kz=kz, vb=vb, j=j: e.matmul(pd[:], lhsT=kz[:, j * 128:(j + 1) * 128], rhs=vb[:], start=True, stop=True), reads=[kzb, vbb], writes=[pdb])
                            S.op("dve", lambda e, pd=pd, j=j: e.scalar_tensor_tensor(out=Sst[:, h, j, :], in0=Sst[:, h, j, :], scalar=gc, in1=pd[:], op0=ALU.mult, op1=ALU.add), reads=[pdb, S_b[h]], writes=[S_b[h]])

                    if main:
                        S.dma("act", lambda e, ubt=ubt, h=h, blk=blk: e.dma_start(out=UB[blk - 12, :, h * 4:(h + 1) * 4, :], in_=ubt[:]), ubs[k_], reads=[ubtb], writes=[])
            S.barrier()

        with ExitStack() as st:
            sb = lambda n, s, d=F32: st.enter_context(nc.sbuf_tensor(n, list(s), d))
            cq = S.dma_src("cq1b")
            bgt = sb("bgt2", [128, 2, 16])
            cb_ = Buf("consts1b")
            S.dma("sp", lambda e: e.dma_start(out=bgt[:], in_=bg), cq, writes=[cb_])
            xstg = DRing(S, nc, st, "xstg2", 2, [128, 2, TB], F32)
            xTb = Ring(nc, st, "xTb2", 1, [128, 16, TB], BF16)
            uaT = sb("uaT", [128, 16, TB], BF16)
            ubT = sb("ubT", [128, 32, TB], BF16)
            mxT = sb("mxT", [128, 16, TB], BF16)
            ua_b, ub_b, mx_b = Buf("uaT"), Buf("ubT"), Buf("mxT")
            uq = S.dma_src("uq")
            uq2 = S.dma_src("uq2")
            w3r = DRing(S, nc, st, "w3r", 5, [128, 20, 128], BF16)
            wor = DRing(S, nc, st, "wor", 3, [128, 8, 512], BF16)
            xrr = DRing(S, nc, st, "xrr", 1, [128, 4, D], F32)
            lnt = sb("lnt", [128, 2, D])
            lnb = cb_
            S.dma("act", lambda e: e.dma_start(out=lnt[:], in_=lnp[:, 0:2, :]), cq, writes=[lnb])
            G1 = Ring(nc, st, "g1", 1, [128, TB], F32)
            G2 = Ring(nc, st, "g2", 1, [128, TB], F32)
            Zst = Ring(nc, st, "zst", 2, [128, 32], F32)
            hsrc = S.dma_src("hs")

            def mmgroup(ps, psb, pairs, reads):
                n = len(pairs)
                S.group("pe", [
                    (lambda e, l=l, r=r, i=i: e.matmul(ps, lhsT=l, rhs=r, start=(i == 0), stop=(i == n - 1)))
                    for i, (l, r) in enumerate(pairs)], reads=reads, writes=[psb])

            for bi in range(4 if "1b" in _PH else 0):
                xb, xbb = xTb.next()
                for hf in range(8):
                    t, tb, ts = xstg.next()
                    S.dma("sp", lambda e, t=t, hf=hf, bi=bi: e.dma_start(out=t[:], in_=xT[12 + bi, :, hf * 2:(hf + 1) * 2, :]), ts, writes=[tb])
                    S.op("pool", lambda e, t=t, xb=xb, hf=hf: e.tensor_copy(out=xb[:, hf * 2:(hf + 1) * 2, :], in_=t[:]), reads=[tb], writes=[xbb])
                S.dma("act", lambda e, bi=bi: e.dma_start(out=uaT[:], in_=UA[bi]), uq, writes=[ua_b])
                S.dma("act", lambda e, bi=bi: e.dma_start(out=ubT[:], in_=UB[bi]), uq2, writes=[ub_b])
                for nt in range(16):
                    pcs = []
                    for q_ in range(4):
                        w, wb_, ws = w3r.next()
                        S.dma("sp", lambda e, w=w, nt=nt, q_=q_: e.dma_start(out=w[:], in_=b_w3[nt, :, q_ * 20:(q_ + 1) * 20, :]), ws, writes=[wb_])
                        pcs.append((w, wb_))
                    wsl = lambda kc: pcs[kc // 20][0][:, kc % 20, :]
                    wbs = lambda lo, hi: [pcs[q_][1] for q_ in range(lo // 20, (hi - 1) // 20 + 1)]
                    pa, pab = PS.next()
                    mmgroup(pa[:], pab, [(wsl(kc), uaT[:, kc, :]) for kc in range(16)], wbs(0, 16) + [ua_b])
                    pb, pbb = PS.next()
                    mmgroup(pb[:], pbb, [(wsl(16 + kc), ubT[:, kc, :]) for kc in range(32)], wbs(16, 48) + [ub_b])
                    pga, pgab = PS.next()
                    mmgroup(pga[:], pgab, [(wsl(48 + kc), xb[:, kc, :]) for kc in range(16)], wbs(48, 64) + [xbb])
                    pgb_, pgbb = PS.next()
                    mmgroup(pgb_[:], pgbb, [(wsl(64 + kc), xb[:, kc, :]) for kc in range(16)], wbs(64, 80) + [xbb])
                    s1, s1b = G1.next()
                    S.op("act", lambda e, s1=s1, pga=pga, nt=nt: e.activation(out=s1[:], in_=pga[:], func=AF.Sigmoid, bias=bgt[:, 0, nt:nt + 1]), reads=[pgab], writes=[s1b])
                    s2, s2b = G2.next()
                    S.op("act", lambda e, s2=s2, pgb_=pgb_, nt=nt: e.activation(out=s2[:], in_=pgb_[:], func=AF.Sigmoid, bias=bgt[:, 1, nt:nt + 1]), reads=[pgbb], writes=[s2b])
                    S.op("dve", lambda e, s1=s1, pa=pa: e.tensor_tensor(out=s1[:], in0=s1[:], in1=pa[:], op=ALU.mult), reads=[s1b, pab], writes=[s1b])
                    S.op("dve", lambda e, s2=s2, pb=pb: e.tensor_tensor(out=s2[:], in0=s2[:], in1=pb[:], op=ALU.mult), reads=[s2b, pbb], writes=[s2b])
                    S.op("pool", lambda e, s1=s1, s2=s2, nt=nt: e.tensor_tensor(out=mxT[:, nt, :], in0=s1[:], in1=s2[:], op=ALU.add), reads=[s1b, s2b], writes=[mx_b])
                xr, xrb, xrs = xrr.next()
                S.dma("act", lambda e, xr=xr, bi=bi: e.dma_start(out=xr[:], in_=xres[bi * TB:(bi + 1) * TB, :].rearrange("(t p) d -> p t d", p=128)), xrs, reads=[dr_H], writes=[xrb])
                for nb in range(4):
                    pcs = []
                    for q_ in range(2):
                        w, wb_, ws = wor.next()
                        S.dma("sp", lambda e, w=w, nb=nb, q_=q_: e.dma_start(out=w[:], in_=b_wo[nb, :, q_ * 8:(q_ + 1) * 8, :]), ws, writes=[wb_])
                        pcs.append((w, wb_))
                    for tt in range(4):
                        pz, pzb = PS.next()
                        mmgroup(pz[:], pzb, [(mxT[:, kc, tt * 128:(tt + 1) * 128], pcs[kc // 8][0][:, kc % 8, :]) for kc in range(16)], [pcs[0][1], pcs[1][1], mx_b])
                        S.op("dve", lambda e, xr=xr, pz=pz, tt=tt, nb=nb: e.scalar_tensor_tensor(out=xr[:, tt, nb * 512:(nb + 1) * 512], in0=xr[:, tt, nb * 512:(nb + 1) * 512], scalar=DN_ALPHA, in1=pz[:], op0=ALU.mult, op1=ALU.add), reads=[pzb, xrb], writes=[xrb])
                for tt in range(4):
                    zs, zsb = Zst.next()
                    for q4 in range(4):
                        S.op("dve", lambda e, zs=zs, xr=xr, tt=tt, q4=q4: e.bn_stats(out=zs[:, q4 * 6:(q4 + 1) * 6], in_=xr[:, tt, q4 * 512:(q4 + 1) * 512]), reads=[xrb], writes=[zsb])
                    S.op("dve", lambda e, zs=zs: e.bn_aggr(out=zs[:, 24:26], in_=zs[:, 0:24]), reads=[zsb], writes=[zsb])
                    S.op("dve", lambda e, zs=zs: e.tensor_scalar(out=zs[:, 25:26], in0=zs[:, 25:26], scalar1=LN_EPS, scalar2=None, op0=ALU.add), reads=[zsb], writes=[zsb])
                    S.op("act", lambda e, zs=zs: e.activation(out=zs[:, 25:26], in_=zs[:, 25:26], func=AF.Sqrt), reads=[zsb], writes=[zsb])
                    S.op("dve", lambda e, zs=zs: e.reciprocal(out=zs[:, 25:26], in_=zs[:, 25:26]), reads=[zsb], writes=[zsb])
                    S.op("dve", lambda e, zs=zs, xr=xr, tt=tt: e.tensor_scalar(out=xr[:, tt, :], in0=xr[:, tt, :], scalar1=zs[:, 24:25], scalar2=zs[:, 25:26], op0=ALU.subtract, op1=ALU.mult), reads=[zsb, xrb], writes=[xrb])
                    S.op("pool", lambda e, xr=xr, tt=tt: e.tensor_tensor(out=xr[:, tt, :], in0=xr[:, tt, :], in1=lnt[:, 0, :], op=ALU.mult), reads=[xrb, lnb], writes=[xrb])
                    S.op("pool", lambda e, xr=xr, tt=tt: e.tensor_tensor(out=xr[:, tt, :], in0=xr[:, tt, :], in1=lnt[:, 1, :], op=ALU.add), reads=[xrb, lnb], writes=[xrb])
                S.dma("sp", lambda e, xr=xr, bi=bi: e.dma_start(out=Hs[bi * TB:(bi + 1) * TB, :].rearrange("(t p) d -> p t d", p=128), in_=xr[:]), hsrc, reads=[xrb], writes=[dr_H])
            S.barrier()

        CAP = 512
        with ExitStack() as st2:
            sbp = lambda n, s, d=F32: st2.enter_context(nc.sbuf_tensor(n, list(s), d))
            cq2 = S.dma_src("cq2")
            c2b = Buf("consts2")
            idf2 = sbp("idf2", [128, 128])
            idb2 = sbp("idb2", [128, 128], BF16)
            ust = sbp("ust", [128, 2, 128])
            io512 = sbp("io512", [128, 512])
            io64 = sbp("io64", [128, 2, 64])
            tokc = sbp("tokc", [128, 16, 2])
            wall = sbp("wall", [128, 16, 64])
            selall = sbp("selall", [128, 16, 64])
            posall = sbp("posall", [128, 16, 64])
            eidf = sbp("eidf", [128, 16, 8])
            carry2 = sbp("carry2", [128, 64])
            rt_b = Buf("routing")
            for t_, a_ in ((idf2, ident), (ust, ustrict), (io512, iota512), (io64, iota64), (tokc, tokcol)):
                S.dma("sp", lambda e, t_=t_, a_=a_: e.dma_start(out=t_[:], in_=a_), cq2, writes=[c2b])
            S.op("dve", lambda e: e.tensor_copy(out=idb2[:], in_=idf2[:]), reads=[c2b], writes=[c2b])
            S.op("pool", lambda e: e.memset(carry2[:], 0.0), writes=[rt_b])

            def mmgroup(ps, psb, pairs, reads):
                n = len(pairs)
                S.group("pe", [
                    (lambda e, l=l, r=r, i=i: e.matmul(ps, lhsT=l, rhs=r, start=(i == 0), stop=(i == n - 1)))
                    for i, (l, r) in enumerate(pairs)], reads=reads, writes=[psb])

            def ffn(xT_, xTb_, wg, wgb, wu, wub, wd, wdb, h1, h1b, sgr, emit_y):
                for ft in range(4):
                    pg, pgb = PS.next()
                    mmgroup(pg[:], pgb, [(wg[:, kc, ft * 128:(ft + 1) * 128], xT_[:, kc, :]) for kc in range(16)], [wgb, xTb_])
                    pu, pub = PS.next()
                    mmgroup(pu[:], pub, [(wu[:, kc, ft * 128:(ft + 1) * 128], xT_[:, kc, :]) for kc in range(16)], [wub, xTb_])
                    sg, sgb = sgr.next()
                    S.op("act", lambda e, sg=sg, pg=pg: e.activation(out=sg[:], in_=pg[:], func=AF.Silu), reads=[pgb], writes=[sgb])
                    S.op("dve", lambda e, h1=h1, sg=sg, pu=pu, ft=ft: e.tensor_tensor(out=h1[:, ft, :], in0=sg[:], in1=pu[:], op=ALU.mult), reads=[sgb, pub], writes=[h1b])
                for tt in range(4):
                    for nb in range(4):
                        py, pyb = PS.next()
                        mmgroup(py[:], pyb, [(h1[:, ft, tt * 128:(tt + 1) * 128], wd[:, ft, nb * 512:(nb + 1) * 512]) for ft in range(4)], [h1b, wdb])
                        emit_y(tt, nb, py, pyb)

            with ExitStack() as st:
                sb = lambda n, s, d=F32: st.enter_context(nc.sbuf_tensor(n, list(s), d))
                wrt = sb("wrt", [128, 16, 64])
                rbt = sb("rbt", [128, 64])
                cq2b = S.dma_src("cq2b")
                cq2c = S.dma_src("cq2c")
                swq = S.dma_src("swq")
                r2b = Buf("rconsts")
                for t_, a_ in ((wrt, wr), (rbt, rb_bc)):
                    S.dma("sp", lambda e, t_=t_, a_=a_: e.dma_start(out=t_[:], in_=a_), cq2b, writes=[r2b])
                zr = sb("zr", [128, D], BF16)
                zrb = Buf("zr")
                S.op("pool", lambda e: e.memset(zr[:], 0.0), writes=[zrb])
                S.dma("sp", lambda e: e.dma_start(out=Hb[NB:NB + 128, :], in_=zr[:]), cq2c, reads=[zrb], writes=[])
                hT = Ring(nc, st, "hT32", 1, [128, 16, 128], F32)
                hld = DRing(S, nc, st, "hld", 1, [128, 4, D], F32)
                hbr = Ring(nc, st, "hbr", 2, [128, D], BF16)
                hbs = [S.dma_src("hbs%d" % k) for k in range(2)]
                R1 = Ring(nc, st, "rt1", 2, [128, 64], F32)
                R2 = Ring(nc, st, "rt2", 2, [128, 64], F32)
                R3 = Ring(nc, st, "rt3", 2, [128, 64], F32)
                R8 = Ring(nc, st, "rt8", 2, [128, 4, 8], F32)
                RI = Ring(nc, st, "rti", 2, [128, 8], mybir.dt.uint32)
                hTb = sb("hTb", [128, 16, TB], BF16)
                hTb_b = Buf("hTb")
                h1T = Ring(nc, st, "h1T", 1, [128, 4, TB], BF16)
                Rsg2 = Ring(nc, st, "sg2", 2, [128, TB], F32)
                swg = sb("swg", [128, 16, 512], BF16)
                swu = sb("swu", [128, 16, 512], BF16)
                swd = sb("swd", [128, 4, D], BF16)
                swb = Buf("sw")
                S.dma("sp", lambda e: e.dma_start(out=swg[:], in_=b_wge[0]), swq, writes=[swb])
                S.dma("act", lambda e: e.dma_start(out=swu[:], in_=b_wue[0]), swq, writes=[swb])
                S.dma("sp", lambda e: e.dma_start(out=swd[:], in_=b_wde[0]), swq, writes=[swb])
                ysr = Ring(nc, st, "ysr", 2, [128, D], F32)
                yss = [S.dma_src("yss%d" % k) for k in range(2)]
                hcnt = [0, 0]
                for bi in range(4 if "2" in _PH else 0):
                    hx, hxb, hxs = hld.next()
                    S.dma("sp", lambda e, hx=hx, bi=bi: e.dma_start(out=hx[:], in_=Hs[bi * TB:(bi + 1) * TB, :].rearrange("(t p) d -> p t d", p=128)), hxs, writes=[hxb])
                    for tt in range(4):
                        tg = bi * 4 + tt
                        k_ = hcnt[0] % 2
                        hcnt[0] += 1
                        hb_, hbb = hbr.next()
                        S.op("pool", lambda e, hb_=hb_, hx=hx, tt=tt: e.tensor_copy(out=hb_[:], in_=hx[:, tt, :]), reads=[hxb], writes=[hbb])
                        S.dma("act", lambda e, hb_=hb_, tg=tg: e.dma_start(out=Hb[tg * 128:(tg + 1) * 128, :], in_=hb_[:]), hbs[k_], reads=[hbb], writes=[])
                        h32, h32b = hT.next()
                        for k4 in range(4):
                            pp, ppb = PS.next()
                            S.group("pe", [(lambda e, pp=pp, hx=hx, tt=tt, kc=k4 * 4 + q4, q4=q4: e.transpose(pp[:, q4 * 128:(q4 + 1) * 128], hx[:, tt, kc * 128:(kc + 1) * 128], idf2[:])) for q4 in range(4)], reads=[hxb, c2b], writes=[ppb])
                            S.op("act", lambda e, pp=pp, h32=h32, k4=k4: e.copy(out=h32[:, k4 * 4:(k4 + 1) * 4, :], in_=pp[:].rearrange("p (a b) -> p a b", b=128)), reads=[ppb], writes=[h32b])
                            S.op("pool", lambda e, h32=h32, k4=k4, tt=tt: e.tensor_copy(out=hTb[:, k4 * 4:(k4 + 1) * 4, tt * 128:(tt + 1) * 128], in_=h32[:, k4 * 4:(k4 + 1) * 4, :]), reads=[h32b], writes=[hTb_b])
                        pl, plb = PS.next()
                        mmgroup(pl[:, 0:64], plb, [(h32[:, kc, :], wrt[:, kc, :]) for kc in range(16)], [h32b, r2b])
                        sc, scb = R1.next()
                        S.op("act", lambda e, sc=sc, pl=pl: e.activation(out=sc[:], in_=pl[:, 0:64], func=AF.Sigmoid), reads=[plb], writes=[scb])
                        bs, bsb = R2.next()
                        S.op("dve", lambda e, bs=bs, sc=sc: e.tensor_tensor(out=bs[:], in0=sc[:], in1=rbt[:], op=ALU.add), reads=[scb, r2b], writes=[bsb])
                        t8, t8b = R3.next()
                        for g in range(8):
                            S.op("dve", lambda e, t8=t8, bs=bs, g=g: e.max(out=t8[:, g * 8:(g + 1) * 8], in_=bs[:, g * 8:(g + 1) * 8]), reads=[bsb], writes=[t8b])
                        sm8, sm8b = R8.next()
                        t8v = t8[:].rearrange("p (g k) -> p g k", k=8)
                        S.op("dve", lambda e, sm8=sm8, t8v=t8v: e.tensor_tensor(out=sm8[:, 0, :], in0=t8v[:, :, 0], in1=t8v[:, :, 1], op=ALU.add), reads=[t8b], writes=[sm8b])
                        S.op("dve", lambda e, sm8=sm8: e.max(out=sm8[:, 1, :], in_=sm8[:, 0, :]), reads=[sm8b], writes=[sm8b])
                        S.op("dve", lambda e, sm8=sm8: e.tensor_scalar(out=sm8[:, 2, :], in0=sm8[:, 0, :], scalar1=sm8[:, 1, 3:4], scalar2=None, op0=ALU.is_ge), reads=[sm8b], writes=[sm8b])
                        S.op("dve", lambda e, sm8=sm8: e.tensor_scalar(out=sm8[:, 2, :], in0=sm8[:, 2, :], scalar1=-1.0, scalar2=1e9, op0=ALU.add, op1=ALU.mult), reads=[sm8b], writes=[sm8b])
                        bm, bmb = R3.next()
                        S.op("dve", lambda e, bm=bm, bs=bs, sm8=sm8: e.tensor_tensor(out=bm[:].rearrange("p (g k) -> p g k", k=8), in0=bs[:].rearrange("p (g k) -> p g k", k=8), in1=sm8[:, 2, :].unsqueeze(2).to_broadcast([128, 8, 8]), op=ALU.add), reads=[bsb, sm8b], writes=[bmb])
                        S.op("dve", lambda e, sm8=sm8, bm=bm: e.max(out=sm8[:, 3, :], in_=bm[:]), reads=[bmb], writes=[sm8b])
                        ei, eib = RI.next()
                        S.op("dve", lambda e, ei=ei, sm8=sm8, bm=bm: e.max_index(out=ei[:], in_max=sm8[:, 3, :], in_values=bm[:]), reads=[bmb, sm8b], writes=[eib])
                        S.op("dve", lambda e, ei=ei, tg=tg: e.tensor_copy(out=eidf[:, tg, :], in_=ei[:]), reads=[eib], writes=[rt_b])
                        S.op("dve", lambda e, sm8=sm8, bm=bm, tg=tg: e.tensor_scalar(out=selall[:, tg, :], in0=bm[:], scalar1=sm8[:, 3, 7:8], scalar2=None, op0=ALU.is_ge), reads=[bmb, sm8b], writes=[rt_b])
                        S.op("dve", lambda e, bm=bm, sc=sc, tg=tg: e.tensor_tensor(out=bm[:], in0=selall[:, tg, :], in1=sc[:], op=ALU.mult), reads=[rt_b, scb, bmb], writes=[bmb])
                        S.op("dve", lambda e, sm8=sm8, bm=bm: e.reduce_sum(out=sm8[:, 1, 0:1], in_=bm[:], axis=AX.X), reads=[bmb], writes=[sm8b])
                        S.op("dve", lambda e, sm8=sm8: e.reciprocal(out=sm8[:, 1, 1:2], in_=sm8[:, 1, 0:1]), reads=[sm8b], writes=[sm8b])
                        S.op("dve", lambda e, sm8=sm8, bm=bm, tg=tg: e.tensor_scalar(out=wall[:, tg, :], in0=bm[:], scalar1=sm8[:, 1, 1:2], scalar2=2.5, op0=ALU.mult, op1=ALU.mult), reads=[bmb, sm8b], writes=[rt_b])
                        pq, pqb = PS.next()
                        S.op("pe", lambda e, pq=pq, tg=tg: e.matmul(pq[:, 0:64], lhsT=ust[:, 0, :], rhs=selall[:, tg, :], start=True, stop=True), reads=[rt_b, c2b], writes=[pqb])
                        S.op("dve", lambda e, pq=pq, tg=tg: e.tensor_tensor(out=posall[:, tg, :], in0=pq[:, 0:64], in1=carry2[:], op=ALU.add), reads=[pqb, rt_b], writes=[rt_b])
                        S.op("dve", lambda e, tg=tg: e.tensor_scalar(out=posall[:, tg, :], in0=posall[:, tg, :], scalar1=float(CAP - 1), scalar2=1.0, op0=ALU.min, op1=ALU.add), reads=[rt_b], writes=[rt_b])
                        S.op("dve", lambda e, tg=tg: e.tensor_tensor(out=posall[:, tg, :], in0=posall[:, tg, :], in1=selall[:, tg, :], op=ALU.mult), reads=[rt_b], writes=[rt_b])
                        S.op("dve", lambda e, tg=tg: e.tensor_scalar(out=posall[:, tg, :], in0=posall[:, tg, :], scalar1=-1.0, scalar2=None, op0=ALU.add), reads=[rt_b], writes=[rt_b])
                        pq2, pq2b = PS.next()
                        S.op("pe", lambda e, pq2=pq2, tg=tg: e.matmul(pq2[:, 0:64], lhsT=ust[:, 1, :], rhs=selall[:, tg, :], start=True, stop=True), reads=[rt_b, c2b], writes=[pq2b])
                        S.op("dve", lambda e, pq2=pq2: e.tensor_tensor(out=carry2[:], in0=pq2[:, 0:64], in1=carry2[:], op=ALU.add), reads=[pq2b, rt_b], writes=[rt_b])
                    h1, h1b = h1T.next()
                    ys_cur = {}

                    def emit_sh(tt, nb, py, pyb, bi=bi, ys_cur=ys_cur):
                        if nb == 0:
                            k_ = hcnt[1] % 2
                            hcnt[1] += 1
                            ys_cur["t"] = ysr.next() + (k_,)
                        yt, ytb, k_ = ys_cur["t"]
                        S.op("act", lambda e, yt=yt, py=py, nb=nb: e.copy(out=yt[:, nb * 512:(nb + 1) * 512], in_=py[:]), reads=[pyb], writes=[ytb])
                        if nb == 3:
                            tg = bi * 4 + tt
                            S.dma("act", lambda e, yt=yt, tg=tg: e.dma_start(out=YS[tg * 128:(tg + 1) * 128, :], in_=yt[:]), yss[k_], reads=[ytb], writes=[])

                    ffn(hTb, hTb_b, swg, swb, swu, swb, swd, swb, h1, h1b, Rsg2, emit_sh)
                S.barrier()

            with ExitStack() as st:
                wst = DRing(S, nc, st, "wst", 2, [128, 4096], F32)
                wgr = Ring(nc, st, "wgr", 3, [128, 16, 512], BF16)
                wdr2 = Ring(nc, st, "wdr2", 2, [128, 4, D], BF16)
                xgr = DRing(S, nc, st, "xgr", 2, [128, 4, D], BF16)
                xTr = Ring(nc, st, "xTr", 1, [128, 16, CAP], BF16)
                h1r = Ring(nc, st, "h1r", 1, [128, 4, CAP], BF16)
                sgr = Ring(nc, st, "sgr", 2, [128, CAP], F32)
                ysb = Ring(nc, st, "ysb", 1, [128, 4, D], BF16)
                ysd = S.dma_src("ysd")
                Qr = Ring(nc, st, "Qr", 2, [128, CAP], F32)
                ixf = Ring(nc, st, "ixf", 2, [128, 4, 2], F32)
                ixu = Ring(nc, st, "ixu", 2, [128, 4], mybir.dt.uint32)
                ccnt = [0]

                def load_w(e_):
                    out = []
                    for (src, nk, ring) in ((wge, 16, wgr), (wue, 16, wgr), (wde, 4, wdr2)):
                        wt, wtb = ring.next()
                        flat = wt[:].rearrange("p a b -> p (a b)")
                        srcf = src[e_].rearrange("p a b -> p (a b)")
                        for hf in range(2):
                            t, tb, ts = wst.next()
                            S.dma("sp", lambda e, t=t, srcf=srcf, hf=hf: e.dma_start(out=t[:], in_=srcf[:, hf * 4096:(hf + 1) * 4096]), ts, writes=[tb])
                            eng = ("pool", "act")[ccnt[0] % 2]
                            ccnt[0] += 1
                            if eng == "act":
                                S.op("act", lambda e, t=t, flat=flat, hf=hf: e.copy(out=flat[:, hf * 4096:(hf + 1) * 4096], in_=t[:]), reads=[tb], writes=[wtb])
                            else:
                                S.op("pool", lambda e, t=t, flat=flat, hf=hf: e.tensor_copy(out=flat[:, hf * 4096:(hf + 1) * 4096], in_=t[:]), reads=[tb], writes=[wtb])
                        out += [wt, wtb]
                    return out

                def lists_and_gather(e_):
                    banks = [PS.next() for _ in range(4)]
                    for tg in range(16):
                        q, qb = Qr.next()
                        S.op("dve", lambda e, q=q, tg=tg, e_=e_: e.tensor_scalar(out=q[:], in0=io512[:], scalar1=posall[:, tg, e_:e_ + 1], scalar2=None, op0=ALU.is_equal), reads=[rt_b, c2b], writes=[qb])
                        for rg in range(4):
                            pb_, pbb_ = banks[rg]
                            S.op("pe", lambda e, pb_=pb_, q=q, rg=rg, tg=tg: e.matmul(pb_[:, 0:2], lhsT=q[:, rg * 128:(rg + 1) * 128], rhs=tokc[:, tg, :], start=(tg == 0), stop=(tg == 15)), reads=[qb, c2b], writes=[pbb_])
                    xf, xfb = ixf.next()
                    xu, xub = ixu.next()
                    for rg in range(4):
                        pb_, pbb_ = banks[rg]
                        S.op("dve", lambda e, xf=xf, pb_=pb_, rg=rg: e.tensor_copy(out=xf[:, rg, :], in_=pb_[:, 0:2]), reads=[pbb_], writes=[xfb])
                    S.op("dve", lambda e, xf=xf: e.tensor_scalar(out=xf[:, :, 1], in0=xf[:, :, 1], scalar1=-float(NB), scalar2=float(NB), op0=ALU.mult, op1=ALU.add), reads=[xfb], writes=[xfb])
                    S.op("dve", lambda e, xf=xf: e.tensor_tensor(out=xf[:, :, 0], in0=xf[:, :, 0], in1=xf[:, :, 1], op=ALU.add), reads=[xfb], writes=[xfb])
                    S.op("dve", lambda e, xf=xf: e.tensor_scalar(out=xf[:, :, 0], in0=xf[:, :, 0], scalar1=float(NB), scalar2=0.0, op0=ALU.min, op1=ALU.max), reads=[xfb], writes=[xfb])
                    S.op("dve", lambda e, xf=xf, xu=xu: e.tensor_copy(out=xu[:], in_=xf[:, :, 0]), reads=[xfb], writes=[xub])
                    xg, xgb, xgs = xgr.next()
                    for rg in range(4):
                        S.dma("pool", lambda e, xg=xg, xu=xu, rg=rg: e.indirect_dma_start(out=xg[:, rg, :], out_offset=None, in_=Hb, in_offset=bass.IndirectOffsetOnAxis(ap=xu[:, rg:rg + 1], axis=0)), xgs, reads=[xub], writes=[xgb])
                    return xg, xgb

                nexp = min(N_EXP, 64) if "2" in _PH else 0
                pend = lists_and_gather(0) if nexp else None
                for ex in range(nexp):
                    wg, wgb, wu, wub, wd, wdb = load_w(ex)
                    xg, xgb = pend
                    if ex + 1 < nexp:
                        pend = lists_and_gather(ex + 1)
                    xT_, xTb_ = xTr.next()
                    for rg in range(4):
                        for k8 in range(2):
                            pt, ptb_ = PT.next()
                            S.group("pe", [(lambda e, pt=pt, xg=xg, rg=rg, kc=k8 * 8 + j, j=j: e.transpose(pt[:, j, :], xg[:, rg, kc * 128:(kc + 1) * 128], idb2[:])) for j in range(8)], reads=[xgb, c2b], writes=[ptb_])
                            eng = "act" if (rg * 2 + k8) % 2 == 0 else "dve"
                            if eng == "act":
                                S.op("act", lambda e, pt=pt, xT_=xT_, rg=rg, k8=k8: e.copy(out=xT_[:, k8 * 8:(k8 + 1) * 8, rg * 128:(rg + 1) * 128], in_=pt[:]), reads=[ptb_], writes=[xTb_])
                            else:
                                S.op("dve", lambda e, pt=pt, xT_=xT_, rg=rg, k8=k8: e.tensor_copy(out=xT_[:, k8 * 8:(k8 + 1) * 8, rg * 128:(rg + 1) * 128], in_=pt[:]), reads=[ptb_], writes=[xTb_])
                    h1, h1b = h1r.next()
                    yt, ytb = ysb.next()

                    def emit_y(tt, nb, py, pyb, yt=yt, ytb=ytb):
                        if (tt * 4 + nb) % 2 == 0:
                            S.op("act", lambda e, yt=yt, py=py, tt=tt, nb=nb: e.copy(out=yt[:, tt, nb * 512:(nb + 1) * 512], in_=py[:]), reads=[pyb], writes=[ytb])
                        else:
                            S.op("dve", lambda e, yt=yt, py=py, tt=tt, nb=nb: e.tensor_copy(out=yt[:, tt, nb * 512:(nb + 1) * 512], in_=py[:]), reads=[pyb], writes=[ytb])

                    ffn(xT_, xTb_, wg, wgb, wu, wub, wd, wdb, h1, h1b, sgr, emit_y)
                    S.dma("sp", lambda e, yt=yt, ex=ex: e.dma_start(out=Yx[ex * CAP:(ex + 1) * CAP, :].rearrange("(g p) n -> p g n", p=128), in_=yt[:]), ysd, reads=[ytb], writes=[])
                S.barrier()

            with ExitStack() as st:
                sb = lambda n, s, d=F32: st.enter_context(nc.sbuf_tensor(n, list(s), d))
                ln2 = sb("ln2", [128, 2, D])
                cq2d = S.dma_src("cq2d")
                l2b = Buf("ln2")
                S.dma("sp", lambda e: e.dma_start(out=ln2[:], in_=lnp[:, 2:4, :]), cq2d, writes=[l2b])
                hr = DRing(S, nc, st, "hr3", 2, [128, D], F32)
                yr = DRing(S, nc, st, "yr3", 2, [128, D], F32)
                gk = DRing(S, nc, st, "gk3", 4, [128, D], BF16)
                acc = Ring(nc, st, "acc3", 2, [128, D], F32)
                oh = Ring(nc, st, "oh3", 2, [128, 64], F32)
                sw = Ring(nc, st, "sw3", 2, [128, 2, 8], F32)
                su = Ring(nc, st, "su3", 2, [128, 8], mybir.dt.uint32)
                Zs2 = Ring(nc, st, "zs2", 2, [128, 32], F32)
                osr = [S.dma_src("os%d" % k) for k in range(2)]
                for tg in range(16 if "2" in _PH else 0):
                    ht, htb, hts = hr.next()
                    S.dma("sp", lambda e, ht=ht, tg=tg: e.dma_start(out=ht[:], in_=Hs[tg * 128:(tg + 1) * 128, :]), hts, writes=[htb])
                    yt, ytb, yts = yr.next()
                    S.dma("act", lambda e, yt=yt, tg=tg: e.dma_start(out=yt[:], in_=YS[tg * 128:(tg + 1) * 128, :]), yts, writes=[ytb])
                    S.op("dve", lambda e, tg=tg: e.tensor_tensor(out=posall[:, tg, :], in0=posall[:, tg, :], in1=io64[:, 1, :], op=ALU.add), reads=[rt_b, c2b], writes=[rt_b])
                    s_, s_b = sw.next()
                    for k in range(8):
                        o_, o_b = oh.next()
                        S.op("dve", lambda e, o_=o_, tg=tg, k=k: e.tensor_scalar(out=o_[:], in0=io64[:, 0, :], scalar1=eidf[:, tg, k:k + 1], scalar2=None, op0=ALU.is_equal), reads=[rt_b, c2b], writes=[o_b])
                        o2_, o2_b = oh.next()
                        S.op("dve", lambda e, o_=o_, o2_=o2_, tg=tg: e.tensor_tensor(out=o2_[:], in0=o_[:], in1=posall[:, tg, :], op=ALU.mult), reads=[o_b, rt_b], writes=[o2_b])
                        S.op("dve", lambda e, o2_=o2_, s_=s_, k=k: e.reduce_sum(out=s_[:, 0, k:k + 1], in_=o2_[:], axis=AX.X), reads=[o2_b], writes=[s_b])
                        S.op("dve", lambda e, o_=o_, tg=tg: e.tensor_tensor(out=o_[:], in0=o_[:], in1=wall[:, tg, :], op=ALU.mult), reads=[o_b, rt_b], writes=[o_b])
                        S.op("dve", lambda e, o_=o_, s_=s_, k=k: e.reduce_sum(out=s_[:, 1, k:k + 1], in_=o_[:], axis=AX.X), reads=[o_b], writes=[s_b])
                    u_, u_b = su.next()
                    S.op("dve", lambda e, u_=u_, s_=s_: e.tensor_copy(out=u_[:], in_=s_[:, 0, :]), reads=[s_b], writes=[u_b])
                    a_, a_b = acc.next()
                    S.op("dve", lambda e, a_=a_, ht=ht, yt=yt: e.scalar_tensor_tensor(out=a_[:], in0=ht[:], scalar=DN_ALPHA, in1=yt[:], op0=ALU.mult, op1=ALU.add), reads=[htb, ytb], writes=[a_b])
                    for k in range(8):
                        g_, g_b, g_s = gk.next()
                        S.dma("pool", lambda e, g_=g_, u_=u_, k=k: e.indirect_dma_start(out=g_[:], out_offset=None, in_=Yx, in_offset=bass.IndirectOffsetOnAxis(ap=u_[:, k:k + 1], axis=0)), g_s, reads=[u_b], writes=[g_b])
                        S.op("dve", lambda e, a_=a_, g_=g_, s_=s_, k=k: e.scalar_tensor_tensor(out=a_[:], in0=g_[:], scalar=s_[:, 1, k:k + 1], in1=a_[:], op0=ALU.mult, op1=ALU.add), reads=[g_b, s_b, a_b], writes=[a_b])
                    zs, zsb = Zs2.next()
                    for q4 in range(4):
                        S.op("dve", lambda e, zs=zs, a_=a_, q4=q4: e.bn_stats(out=zs[:, q4 * 6:(q4 + 1) * 6], in_=a_[:, q4 * 512:(q4 + 1) * 512]), reads=[a_b], writes=[zsb])
                    S.op("dve", lambda e, zs=zs: e.bn_aggr(out=zs[:, 24:26], in_=zs[:, 0:24]), reads=[zsb], writes=[zsb])
                    S.op("dve", lambda e, zs=zs: e.tensor_scalar(out=zs[:, 25:26], in0=zs[:, 25:26], scalar1=LN_EPS, scalar2=None, op0=ALU.add), reads=[zsb], writes=[zsb])
                    S.op("act", lambda e, zs=zs: e.activation(out=zs[:, 25:26], in_=zs[:, 25:26], func=AF.Sqrt), reads=[zsb], writes=[zsb])
                    S.op("dve", lambda e, zs=zs: e.reciprocal(out=zs[:, 25:26], in_=zs[:, 25:26]), reads=[zsb], writes=[zsb])
                    S.op("dve", lambda e, zs=zs, a_=a_: e.tensor_scalar(out=a_[:], in0=a_[:], scalar1=zs[:, 24:25], scalar2=zs[:, 25:26], op0=ALU.subtract, op1=ALU.mult), reads=[zsb, a_b], writes=[a_b])
                    S.op("pool", lambda e, a_=a_: e.tensor_tensor(out=a_[:], in0=a_[:], in1=ln2[:, 0, :], op=ALU.mult), reads=[a_b, l2b], writes=[a_b])
                    S.op("pool", lambda e, a_=a_: e.tensor_tensor(out=a_[:], in0=a_[:], in1=ln2[:, 1, :], op=ALU.add), reads=[a_b, l2b], writes=[a_b])
                    S.dma("sp", lambda e, a_=a_, tg=tg: e.dma_start(out=out[tg * 128:(tg + 1) * 128, :], in_=a_[:]), osr[tg % 2], reads=[a_b], writes=[])
                S.barrier()
        S.finish()
        print("instructions:", S.n_inst, flush=True)
    return nc


def _consts(p):
    half = 128
    freq = (10000.0 ** (-np.arange(half, dtype=np.float64) / half))
    cosT = np.zeros((16, 128, TB), np.float32)
    sinT = np.zeros((16, 128, TB), np.float32)
    flags = np.zeros((128, 2, 16), np.float32)
    flags[:, 1, :] = 1.0
    for blk in range(16):
        q = p - 3 + blk // 4
        if q < 0:
            continue
        pos = q * NB + (blk % 4) * TB + np.arange(TB, dtype=np.float64)
        ang = (pos[None, :].astype(np.float32) * freq[:, None].astype(np.float32)).astype(np.float32)
        cosT[blk] = np.cos(ang)
        sinT[blk] = np.sin(ang)
        if q == 0 and blk % 4 == 0:
            flags[:, 0, blk] = 1.0
            flags[:, 1, blk] = 0.0
    return cosT, sinT, flags


def kernel(x, w_in, conv_w, conv_b, lru_wa, lru_ba, lru_wi, lru_bi, lru_lambda, ret_gn_gain,
           w_lru_out, w_ret_out, b_gate, w_o, ln1_g, ln1_b, w_router, router_bias,
           w_gate_e, w_up_e, w_down_e, w_gate_s, w_up_s, w_down_s, ln2_g, ln2_b):
    import time
    _t0 = time.time()
    f = lambda a: np.ascontiguousarray(np.asarray(a, dtype=np.float32))
    x = f(x)
    w_in = f(w_in)[0]
    kp = lambda w: w.reshape(16, 128, -1).transpose(1, 0, 2)
    wx, wy = w_in[:, 0:2048], w_in[:, 2048:4096]
    wq_, wk_ = w_in[:, 4096:6144], w_in[:, 6144:8192]
    wv_, wg_ = w_in[:, 8192:12288], w_in[:, 12288:16384]
    wga, wgb = w_in[:, 16384:18432], w_in[:, 18432:20480]
    w_in_l = np.stack([np.stack([kp(wx[:, h * 128:(h + 1) * 128]), kp(wy[:, h * 128:(h + 1) * 128])], 1) for h in range(16)])
    w_in_r = np.stack([np.stack([
        kp(wq_[:, h * 256:(h + 1) * 256]), kp(wk_[:, h * 256:(h + 1) * 256]),
        kp(wv_[:, h * 512:h * 512 + 256]), kp(wv_[:, h * 512 + 256:(h + 1) * 512]),
        kp(wg_[:, h * 512:h * 512 + 256]), kp(wg_[:, h * 512 + 256:(h + 1) * 512])]) for h in range(8)])
    wlo, wro, wo_ = f(w_lru_out)[0], f(w_ret_out)[0], f(w_o)[0]
    kp2 = lambda w, n: w.reshape(n, 128, -1).transpose(1, 0, 2)
    w3 = np.stack([np.concatenate([
        kp2(wlo[:, nt * 128:(nt + 1) * 128], 16), kp2(wro[:, nt * 128:(nt + 1) * 128], 32),
        kp2(wga[:, nt * 128:(nt + 1) * 128], 16), kp2(wgb[:, nt * 128:(nt + 1) * 128], 16)], 1) for nt in range(16)])
    wo_t = np.stack([kp(wo_[:, nb * 512:(nb + 1) * 512]) for nb in range(4)])
    wge = np.concatenate([f(w_gate_e)[0], f(w_gate_s)], 0).reshape(65, 16, 128, 512).transpose(0, 2, 1, 3)
    wue = np.concatenate([f(w_up_e)[0], f(w_up_s)], 0).reshape(65, 16, 128, 512).transpose(0, 2, 1, 3)
    wde = np.concatenate([f(w_down_e)[0], f(w_down_s)], 0).reshape(65, 4, 128, 2048).transpose(0, 2, 1, 3)
    wr_t = kp(f(w_router)[0])
    chp = lambda v: f(v).reshape(16, 128).T
    cw = f(conv_w)[0]
    lrup = np.stack([chp(cw[0]), chp(cw[1]), chp(cw[2]), chp(cw[3]), chp(conv_b), chp(lru_ba), chp(lru_bi), chp(lru_lambda)], 2)
    wai = np.stack([f(lru_wa)[0].transpose(1, 0, 2), f(lru_wi)[0].transpose(1, 0, 2)], 2)
    bgv = f(b_gate)[0]
    bg = np.stack([bgv[:2048].reshape(16, 128).T, bgv[2048:].reshape(16, 128).T], 1)
    bc = lambda v, n: np.ascontiguousarray(np.broadcast_to(f(v).reshape(1, -1), (128, n)))
    lnp = np.stack([bc(ln1_g, D), bc(ln1_b, D), bc(ln2_g, D), bc(ln2_b, D)], 1)
    idx = np.arange(128, dtype=np.float64)
    dmaskT = np.zeros((128, 8, 128), np.float32)
    xizeta = np.zeros((128, 2, 8), np.float32)
    for h in range(8):
        lg = np.log1p(-np.exp2(-5.0 - h))
        diff = idx[None, :] - idx[:, None]
        dmaskT[:, h, :] = np.where(diff >= 0, np.exp(np.maximum(diff, 0) * lg), 0.0) / 16.0
        xizeta[:, 0, h] = np.exp((idx + 1.0) * lg)
        xizeta[:, 1, h] = np.exp((127.0 - idx) * lg) / 16.0
    ustrict = np.zeros((128, 2, 128), np.float32)
    ustrict[:, 0, :] = (np.arange(128)[:, None] < np.arange(128)[None, :]).astype(np.float32)
    ustrict[:, 1, :] = 1.0
    iota512 = np.broadcast_to(np.arange(512, dtype=np.float32)[None, :], (128, 512))
    iota64 = np.stack([np.broadcast_to(np.arange(64, dtype=np.float32)[None, :], (128, 64)),
                       np.broadcast_to(512.0 * np.arange(64, dtype=np.float32)[None, :], (128, 64))], 1)
    tokcol = np.zeros((128, 16, 2), np.float32)
    tokcol[:, :, 0] = np.arange(16)[None, :] * 128 + np.arange(128)[:, None]
    tokcol[:, :, 1] = 1.0
    shared = dict(ustrict=ustrict, iota512=iota512, iota64=iota64, tokcol=tokcol, w_in_l=w_in_l, w_in_r=w_in_r, w3=w3, wo=wo_t, wge=wge, wue=wue, wde=wde, wr=wr_t,
                  lrup=lrup, wai=wai, bg=bg, dmaskT=dmaskT, xizeta=xizeta, gain_bc=bc(ret_gn_gain, 4096),
                  lnp=lnp, rb_bc=bc(router_bias, 64), ident=np.eye(128, dtype=np.float32))
    shared = {k: np.ascontiguousarray(v, dtype=np.float32) for k, v in shared.items()}
    in_maps = []
    for c in range(8):
        b, p = divmod(c, 4)
        cosT, sinT, flags = _consts(p)
        xT = np.zeros((16, 128, 16, TB), np.float32)
        for blk in range(16):
            q = p - 3 + blk // 4
            if q < 0:
                continue
            t0 = q * NB + (blk % 4) * TB
            xT[blk] = x[b, t0:t0 + TB, :].reshape(TB, 16, 128).transpose(2, 1, 0)
        m = dict(shared)
        m.update(xT=xT, xres=np.ascontiguousarray(x[b, p * NB:(p + 1) * NB, :]), cosT=cosT, sinT=sinT, flags=flags)
        in_maps.append(m)
    print("[kernel] host layout %.1fs" % (time.time() - _t0), flush=True)
    nc = build()
    print("[kernel] build %.1fs" % (time.time() - _t0), flush=True)
    res = run_bass_kernel_spmd(nc, in_maps, core_ids=list(range(8)))
    print("[kernel] run done %.1fs" % (time.time() - _t0), flush=True)
    if os.environ.get("MK_DBG", "0") == "1":
        _LAST.clear()
        _LAST.append(res.results)
    outp = np.zeros((2, SEQ, D), np.float32)
    for c in range(8):
        b, p = divmod(c, 4)
        outp[b, p * NB:(p + 1) * NB, :] = res.results[c]["out"]
    return outp
```

```python
import os
import types
import numpy as np
from contextlib import ExitStack
import concourse.bass as bass
import concourse.mybir as mybir
from concourse.bass_utils import run_bass_kernel_spmd

F32 = mybir.dt.float32
BF16 = mybir.dt.bfloat16
AF = mybir.ActivationFunctionType
ALU = mybir.AluOpType
AX = mybir.AxisListType

D = 2048
SEQ = 8192
NB = 2048
TB = 512
NE = 64
DN_ALPHA = 2.0 ** 0.25
LN_EPS = 1e-5
GAMMA = [1.0 - 2.0 ** (-5.0 - h) for h in range(8)]
_LAST = []
N_EXP = int(os.environ.get("MK_NEXP", "65"))
_PH = os.environ.get("MK_PH", "0,1,1b,2").split(",")
_BLKS = [int(v) for v in os.environ.get("MK_BLKS", ",".join(str(i) for i in range(16))).split(",")]
_LH = int(os.environ.get("MK_LH", "16"))
_RH = int(os.environ.get("MK_RH", "8"))


def freeze(fn):
    if fn.__closure__ is None:
        return fn
    cells = []
    for c in fn.__closure__:
        try:
            cells.append(types.CellType(c.cell_contents))
        except ValueError:
            cells.append(c)
    return types.FunctionType(fn.__code__, fn.__globals__, fn.__name__, fn.__defaults__, tuple(cells))


class Src:
    def __init__(self, sem, step, name):
        self.sem, self.step, self.count, self.name = sem, step, 0, name


class Buf:
    __slots__ = ("w", "r", "name")

    def __init__(self, name=""):
        self.w, self.r, self.name = None, {}, name


class Sched:
    ENGS = ("pe", "act", "dve", "pool", "sp")

    def __init__(self, nc, stack):
        self.nc, self.stack = nc, stack
        self.streams = {e: [] for e in self.ENGS}
        self.src = {}
        for e in self.ENGS:
            self.src[e] = Src(stack.enter_context(nc.semaphore("s_" + e)), 1, e)
        self.seen = {e: {} for e in self.ENGS}
        self.dma_srcs = []
        self.n_inst = 0

    def dma_src(self, name):
        s = Src(self.stack.enter_context(self.nc.semaphore("d_" + name)), 16, name)
        self.dma_srcs.append(s)
        return s

    def _waits(self, eng, reads, writes):
        need = {}
        me = self.src[eng]

        def add(ev):
            if ev is None:
                return
            s, c = ev
            if s is me and eng == "pe":
                return
            if need.get(s, 0) < c:
                need[s] = c

        for b in reads:
            add(b.w)
        for b in writes:
            add(b.w)
            for s, c in b.r.items():
                if s is not me:
                    add((s, c))
        out = []
        seen = self.seen[eng]
        for s, c in need.items():
            if seen.get(s, 0) < c:
                seen[s] = c
                out.append((s, c))
        return out

    def op(self, eng, fn, reads=(), writes=()):
        self.group(eng, [fn], reads, writes)

    def group(self, eng, fns, reads=(), writes=()):
        waits = self._waits(eng, reads, writes)
        me = self.src[eng]
        me.count += 1
        cnt = me.count
        st = self.streams[eng]
        for i, fn in enumerate(fns):
            last = i == len(fns) - 1
            st.append((waits if i == 0 else (), freeze(fn), me if last else None, 1))
        for b in reads:
            b.r[me] = cnt
        for b in writes:
            b.w = (me, cnt)
            b.r = {}
        self.n_inst += len(fns)

    def dma(self, q, fn, dsrc, reads=(), writes=()):
        waits = self._waits(q, reads, writes)
        dsrc.count += 16
        cnt = dsrc.count
        self.streams[q].append((waits, freeze(fn), dsrc, 16))
        for b in reads:
            b.r[dsrc] = cnt
        for b in writes:
            b.w = (dsrc, cnt)
            b.r = {}
        self.n_inst += 1

    def barrier(self):
        allsrc = [self.src[e] for e in self.ENGS] + self.dma_srcs
        for e in self.ENGS:
            waits = []
            for s in allsrc:
                if s is self.src[e] or s.count == 0:
                    continue
                if self.seen[e].get(s, 0) < s.count:
                    self.seen[e][s] = s.count
                    waits.append((s, s.count))
            if waits:
                self.streams[e].append((waits, None, None, 0))

    def finish(self):
        nc = self.nc
        eh = {"pe": "tensor", "act": "scalar", "dve": "vector", "pool": "gpsimd", "sp": "sync"}
        self.barrier()
        with nc.Block() as block:
            for e in self.ENGS:
                stream = self.streams[e]

                def body(engine, stream=stream):
                    for waits, fn, src, inc in stream:
                        for s, c in waits:
                            engine.wait_ge(s.sem, c)
                        if fn is not None:
                            ins = fn(engine)
                            if src is not None:
                                ins.then_inc(src.sem, inc)

                getattr(block, eh[e])(body)


class Ring:
    def __init__(self, nc, st, name, n, shape, dt, psum=False):
        self.t, self.b, self.i = [], [], 0
        for k in range(n):
            nm = "%s%d" % (name, k)
            if psum:
                self.t.append(st.enter_context(nc.psum_tensor(nm, shape, dt)))
            else:
                self.t.append(st.enter_context(nc.sbuf_tensor(nm, shape, dt)))
            self.b.append(Buf(nm))

    def next(self):
        k = self.i % len(self.t)
        self.i += 1
        return self.t[k], self.b[k]


class DRing:
    def __init__(self, S, nc, st, name, n, shape, dt):
        self.r = Ring(nc, st, name, n, shape, dt)
        self.s = [S.dma_src("%s%d" % (name, k)) for k in range(n)]

    def next(self):
        k = self.r.i % len(self.s)
        t, b = self.r.next()
        return t, b, self.s[k]


def build():
    nc = bass.Bass("TRN2", target_bir_lowering=False)
    ein = lambda n, s, d=F32: nc.dram_tensor(n, list(s), d, kind="ExternalInput").ap()
    scr = lambda n, s, d=BF16: nc.dram_tensor(n, list(s), d).ap()
    xT = ein("xT", [16, 128, 16, TB])
    xres = ein("xres", [NB, D])
    w_in_l = ein("w_in_l", [16, 128, 2, 16, 128])
    w_in_r = ein("w_in_r", [8, 6, 128, 16, 256])
    w3 = ein("w3", [16, 128, 80, 128])
    wo = ein("wo", [4, 128, 16, 512])
    wge = ein("wge", [65, 128, 16, 512])
    wue = ein("wue", [65, 128, 16, 512])
    wde = ein("wde", [65, 128, 4, 2048])
    wr = ein("wr", [128, 16, 64])
    lrup = ein("lrup", [128, 16, 8])
    wai = ein("wai", [128, 16, 2, 128])
    bg = ein("bg", [128, 2, 16])
    cosT = ein("cosT", [16, 128, TB])
    sinT = ein("sinT", [16, 128, TB])
    flags = ein("flags", [128, 2, 16])
    dmaskT = ein("dmaskT", [128, 8, 128])
    xizeta = ein("xizeta", [128, 2, 8])
    gain_bc = ein("gain_bc", [128, 4096])
    lnp = ein("lnp", [128, 4, D])
    rb_bc = ein("rb_bc", [128, 64])
    ident = ein("ident", [128, 128])
    ustrict = ein("ustrict", [128, 2, 128])
    iota512 = ein("iota512", [128, 512])
    iota64 = ein("iota64", [128, 2, 64])
    tokcol = ein("tokcol", [128, 16, 2])
    out = nc.dram_tensor("out", [NB, D], F32, kind="ExternalOutput").ap()
    b_in_l = scr("b_in_l", [16, 128, 2, 16, 128])
    b_in_r = scr("b_in_r", [8, 6, 128, 16, 256])
    b_w3 = scr("b_w3", [16, 128, 80, 128])
    b_wo = scr("b_wo", [4, 128, 16, 512])
    b_wge = scr("b_wge", [1, 128, 16, 512])
    b_wue = scr("b_wue", [1, 128, 16, 512])
    b_wde = scr("b_wde", [1, 128, 4, 2048])
    _dbg = os.environ.get("MK_DBG", "0") == "1"
    dscr = (lambda n, s, d=BF16: nc.dram_tensor(n, list(s), d, kind="ExternalOutput").ap()) if _dbg else scr
    Hs = dscr("Hs", [NB, D], F32)
    UA = dscr("UA", [4, 128, 16, TB])
    Hb = scr("Hb", [NB + 128, D])
    YS = scr("YS", [NB, D], F32)
    Yx = scr("Yx", [64 * 512, D])
    UB = dscr("UB", [4, 128, 32, TB])

    with ExitStack() as top:
        S = Sched(nc, top)
        dr_w = Buf("dram_w")
        dr_H = Buf("dram_H")
        dr_out = Buf("dram_out")
        PS = Ring(nc, top, "ps", 6, [128, 512], F32, psum=True)
        PT = Ring(nc, top, "pt", 2, [128, 8, 128], BF16, psum=True)

        dout = S.dma_src("dout")

        with ExitStack() as st:
            stg = DRing(S, nc, st, "cst", 3, [128, 4096], F32)
            cvb = Ring(nc, st, "cvb", 3, [128, 4096], BF16)
            cvs = [S.dma_src("cvo%d" % k) for k in range(3)]
            cnt = [0]

            def convert(src2d, dst2d, ncols):
                for c0 in range(0, ncols, 4096):
                    cw = min(4096, ncols - c0)
                    t, tb, ts = stg.next()
                    S.dma("sp", lambda e, t=t, c0=c0, cw=cw: e.dma_start(out=t[:, 0:cw], in_=src2d[:, c0:c0 + cw]), ts, writes=[tb])
                    k = cnt[0] % 3
                    o, ob = cvb.next()
                    eng = ("pool", "act", "dve")[cnt[0] % 3]
                    cnt[0] += 1
                    if eng == "act":
                        S.op("act", lambda e, o=o, t=t, cw=cw: e.copy(out=o[:, 0:cw], in_=t[:, 0:cw]), reads=[tb], writes=[ob])
                    else:
                        S.op(eng, lambda e, o=o, t=t, cw=cw: e.tensor_copy(out=o[:, 0:cw], in_=t[:, 0:cw]), reads=[tb], writes=[ob])
                    S.dma("act" if k == 1 else "sp", lambda e, o=o, c0=c0, cw=cw: e.dma_start(out=dst2d[:, c0:c0 + cw], in_=o[:, 0:cw]), cvs[k], reads=[ob], writes=[])

            for g in range(16 if "0" in _PH else 0):
                convert(w_in_l[g].rearrange("p a k c -> p (a k c)"), b_in_l[g].rearrange("p a k c -> p (a k c)"), 4096)
            for h in range(8 if "0" in _PH else 0):
                for j in range(6):
                    convert(w_in_r[h, j].rearrange("p k c -> p (k c)"), b_in_r[h, j].rearrange("p k c -> p (k c)"), 4096)
            for g in range(16 if "0" in _PH else 0):
                convert(w3[g].rearrange("p k c -> p (k c)"), b_w3[g].rearrange("p k c -> p (k c)"), 10240)
            for g in range(4 if "0" in _PH else 0):
                convert(wo[g].rearrange("p k c -> p (k c)"), b_wo[g].rearrange("p k c -> p (k c)"), 8192)
            if "0" in _PH:
                convert(wge[64].rearrange("p k c -> p (k c)"), b_wge[0].rearrange("p k c -> p (k c)"), 8192)
                convert(wue[64].rearrange("p k c -> p (k c)"), b_wue[0].rearrange("p k c -> p (k c)"), 8192)
                convert(wde[64].rearrange("p k c -> p (k c)"), b_wde[0].rearrange("p k c -> p (k c)"), 8192)
            S.barrier()

        with ExitStack() as st:
            sb = lambda n, s, d=F32: st.enter_context(nc.sbuf_tensor(n, list(s), d))
            cq = S.dma_src("cq")
            cb_ = Buf("consts")
            lp = sb("lp", [128, 16, 8])
            wab = sb("wab", [128, 16, 2, 128], BF16)
            bgt = sb("bgt", [128, 2, 16])
            flg = sb("flg", [128, 2, 16])
            dmk = sb("dmk", [128, 8, 128])
            xz = sb("xz", [128, 2, 8])
            idf = sb("idf", [128, 128])
            idb = sb("idb", [128, 128], BF16)
            lrc = sb("lrc", [128, 2, 16])
            ltmp = sb("ltmp", [128, 16])
            hst = sb("hst", [128, 16])
            carry = sb("carry", [128, 16, 4])
            Sst = sb("Sst", [128, 8, 2, 512])
            for t_, a_ in ((lp, lrup), (bgt, bg), (flg, flags), (dmk, dmaskT), (xz, xizeta), (idf, ident)):
                S.dma("sp", lambda e, t_=t_, a_=a_: e.dma_start(out=t_[:], in_=a_), cq, writes=[cb_])
            S.op("dve", lambda e: e.tensor_copy(out=idb[:], in_=idf[:]), reads=[cb_], writes=[cb_])
            S.op("act", lambda e: e.activation(out=ltmp[:], in_=lp[:, :, 7], func=AF.Exp, scale=-1.0), reads=[cb_], writes=[cb_])
            S.op("act", lambda e: e.activation(out=ltmp[:], in_=ltmp[:], func=AF.Ln, bias=1.0), reads=[cb_], writes=[cb_])
            S.op("dve", lambda e: e.tensor_scalar(out=lrc[:, 0, :], in0=ltmp[:], scalar1=-8.0, scalar2=None, op0=ALU.mult), reads=[cb_], writes=[cb_])
            S.op("dve", lambda e: e.tensor_scalar(out=lrc[:, 1, :], in0=ltmp[:], scalar1=-16.0, scalar2=None, op0=ALU.mult), reads=[cb_], writes=[cb_])
            S.op("pool", lambda e: e.memset(hst[:], 0.0), writes=[cb_])
            S.op("pool", lambda e: e.memset(carry[:], 0.0), writes=[cb_])
            S.op("pool", lambda e: e.memset(Sst[:], 0.0), writes=[cb_])
            hst_b = [Buf("hst%d" % h) for h in range(16)]
            car_b = [Buf("car%d" % h) for h in range(16)]
            S_b = [Buf("S%d" % h) for h in range(8)]
            for bl in hst_b + car_b + S_b:
                bl.w = cb_.w

            xstg = DRing(S, nc, st, "xstg", 2, [128, 2, TB], F32)
            for hf in range(4):
                t, tb, ts = xstg.next()
                S.dma("sp", lambda e, t=t, hf=hf: e.dma_start(out=t[:].rearrange("p a b -> p (a b)"), in_=wai[:, hf * 4:(hf + 1) * 4].rearrange("p h a o -> p (h a o)")), ts, writes=[tb])
                S.op("dve", lambda e, t=t, hf=hf: e.tensor_copy(out=wab[:, hf * 4:(hf + 1) * 4].rearrange("p h a o -> p (h a o)"), in_=t[:].rearrange("p a b -> p (a b)")), reads=[tb], writes=[cb_])
            xTb = Ring(nc, st, "xTb", 1, [128, 16, TB], BF16)
            cs = DRing(S, nc, st, "cs", 1, [128, 2, TB], F32)
            wl = DRing(S, nc, st, "wl", 2, [128, 2, 16, 128], BF16)
            wq = DRing(S, nc, st, "wq", 5, [128, 16, 256], BF16)
            L = {n: Ring(nc, st, "l_" + n, 2, [128, TB], F32) for n in ("xa", "r", "i", "a", "m", "h", "gy")}
            Lxs = Ring(nc, st, "l_xs", 1, [128, TB + 4], F32)
            Lxab = Ring(nc, st, "l_xab", 2, [128, TB], BF16)
            Lin = Ring(nc, st, "l_in", 2, [128, 1], F32)
            Rt = Ring(nc, st, "r_t", 4, [128, TB], F32)
            Rq = Ring(nc, st, "r_q", 2, [128, 2, TB], BF16)
            Rk = Ring(nc, st, "r_k", 2, [128, 2, TB], BF16)
            Rkz = Ring(nc, st, "r_kz", 2, [128, 256], BF16)
            Rv = Ring(nc, st, "r_v", 2, [128, 512], BF16)
            Rs = Ring(nc, st, "r_s", 2, [128, 128], BF16)
            Ro = Ring(nc, st, "r_o", 2, [128, 512], F32)
            Rsg = Ring(nc, st, "r_sg", 1, [128, 512], F32)
            Rub = Ring(nc, st, "r_ub", 2, [128, 512], BF16)
            Rst = Ring(nc, st, "r_st", 2, [128, 8], F32)
            Sb = Ring(nc, st, "Sb", 2, [128, 2, 512], BF16)
            gn = DRing(S, nc, st, "gn", 1, [128, 512], F32)
            uar = Ring(nc, st, "uar", 2, [128, TB], BF16)
            ubr = Ring(nc, st, "ubr", 1, [128, 4, TB], BF16)
            uas = [S.dma_src("uas%d" % k) for k in range(2)]
            ubs = [S.dma_src("ubs%d" % k) for k in range(2)]
            uacnt = [0, 0]

            print("phase1 sbuf remaining", nc.sbuf_bytes_remaining, flush=True)

            def mmgroup(ps, psb, pairs, reads):
                n = len(pairs)
                S.group("pe", [
                    (lambda e, l=l, r=r, i=i: e.matmul(ps, lhsT=l, rhs=r, start=(i == 0), stop=(i == n - 1)))
                    for i, (l, r) in enumerate(pairs)], reads=reads, writes=[psb])

            for blk in (_BLKS if "1" in _PH else []):
                main = blk >= 12
                xb, xbb = xTb.next()
                for hf in range(8):
                    t, tb, ts = xstg.next()
                    S.dma("sp", lambda e, t=t, hf=hf, blk=blk: e.dma_start(out=t[:], in_=xT[blk, :, hf * 2:(hf + 1) * 2, :]), ts, writes=[tb])
                    S.op("pool", lambda e, t=t, xb=xb, hf=hf: e.tensor_copy(out=xb[:, hf * 2:(hf + 1) * 2, :], in_=t[:]), reads=[tb], writes=[xbb])
                ct, ctb, cts = cs.next()
                S.dma("act", lambda e, ct=ct, blk=blk: e.dma_start(out=ct[:, 0, :], in_=cosT[blk]), cts, writes=[ctb])
                S.dma("act", lambda e, ct=ct, blk=blk: e.dma_start(out=ct[:, 1, :], in_=sinT[blk]), cts, writes=[ctb])

                for h in range(_LH):
                    w, wb_, ws = wl.next()
                    if main:
                        S.dma("sp", lambda e, w=w, h=h: e.dma_start(out=w[:], in_=b_in_l[h]), ws, reads=[dr_w], writes=[wb_])
                    else:
                        S.dma("sp", lambda e, w=w, h=h: e.dma_start(out=w[:, 0], in_=b_in_l[h, :, 0]), ws, reads=[dr_w], writes=[wb_])
                    px, pxb = PS.next()
                    mmgroup(px[:], pxb, [(w[:, 0, kc, :], xb[:, kc, :]) for kc in range(16)], [wb_, xbb])
                    xs, xsb = Lxs.next()
                    S.op("act", lambda e, xs=xs, px=px: e.copy(out=xs[:, 4:TB + 4], in_=px[:]), reads=[pxb], writes=[xsb])
                    S.op("pool", lambda e, xs=xs, h=h: e.tensor_copy(out=xs[:, 0:4], in_=carry[:, h, :]), reads=[car_b[h]], writes=[xsb])
                    S.op("pool", lambda e, xs=xs, h=h: e.tensor_copy(out=carry[:, h, :], in_=xs[:, TB:TB + 4]), reads=[xsb], writes=[car_b[h]])
                    xa, xab_ = L["xa"].next()
                    S.op("dve", lambda e, xa=xa, xs=xs, h=h: e.tensor_scalar(out=xa[:], in0=xs[:, 4:TB + 4], scalar1=lp[:, h, 3:4], scalar2=lp[:, h, 4:5], op0=ALU.mult, op1=ALU.add), reads=[xsb], writes=[xab_])
                    for j in range(3):
                        S.op("dve", lambda e, xa=xa, xs=xs, h=h, j=j: e.scalar_tensor_tensor(out=xa[:], in0=xs[:, 1 + j:TB + 1 + j], scalar=lp[:, h, j:j + 1], in1=xa[:], op0=ALU.mult, op1=ALU.add), reads=[xsb, xab_], writes=[xab_])
                    xq, xqb = Lxab.next()
                    S.op("act", lambda e, xq=xq, xa=xa: e.copy(out=xq[:], in_=xa[:]), reads=[xab_], writes=[xqb])
                    pr, prb = PS.next()
                    S.op("pe", lambda e, pr=pr, xq=xq, h=h: e.matmul(pr[:], lhsT=wab[:, h, 0, :], rhs=xq[:], start=True, stop=True), reads=[xqb], writes=[prb])
                    pi, pib = PS.next()
                    S.op("pe", lambda e, pi=pi, xq=xq, h=h: e.matmul(pi[:], lhsT=wab[:, h, 1, :], rhs=xq[:], start=True, stop=True), reads=[xqb], writes=[pib])
                    r, rb = L["r"].next()
                    S.op("act", lambda e, r=r, pr=pr, h=h: e.activation(out=r[:], in_=pr[:], func=AF.Sigmoid, bias=lp[:, h, 5:6]), reads=[prb], writes=[rb])
                    ig, igb = L["i"].next()
                    S.op("act", lambda e, ig=ig, pi=pi, h=h: e.activation(out=ig[:], in_=pi[:], func=AF.Sigmoid, bias=lp[:, h, 6:7]), reads=[pib], writes=[igb])
                    a, ab = L["a"].next()
                    S.op("act", lambda e, a=a, r=r, h=h: e.activation(out=a[:], in_=r[:], func=AF.Exp, scale=lrc[:, 0, h:h + 1]), reads=[rb], writes=[ab])
                    m, mb = L["m"].next()
                    S.op("act", lambda e, m=m, r=r, h=h: e.activation(out=m[:], in_=r[:], func=AF.Exp, scale=lrc[:, 1, h:h + 1]), reads=[rb], writes=[mb])
                    S.op("act", lambda e, m=m: e.activation(out=m[:], in_=m[:], func=AF.Sqrt, scale=-1.0, bias=1.0), reads=[mb], writes=[mb])
                    S.op("dve", lambda e, m=m, blk=blk: e.tensor_scalar(out=m[:, 0:1], in0=m[:, 0:1], scalar1=flg[:, 1, blk:blk + 1], scalar2=flg[:, 0, blk:blk + 1], op0=ALU.mult, op1=ALU.add), reads=[mb], writes=[mb])
                    S.op("dve", lambda e, m=m, ig=ig: e.tensor_tensor(out=m[:], in0=m[:], in1=ig[:], op=ALU.mult), reads=[mb, igb], writes=[mb])
                    S.op("dve", lambda e, m=m, xa=xa: e.tensor_tensor(out=m[:], in0=m[:], in1=xa[:], op=ALU.mult), reads=[mb, xab_], writes=[mb])
                    ini, inib = Lin.next()
                    S.op("dve", lambda e, ini=ini, h=h, blk=blk: e.tensor_tensor(out=ini[:], in0=hst[:, h:h + 1], in1=flg[:, 1, blk:blk + 1], op=ALU.mult), reads=[hst_b[h]], writes=[inib])
                    hh, hb = L["h"].next()
                    S.op("dve", lambda e, hh=hh, a=a, m=m, ini=ini: e.tensor_tensor_scan(out=hh[:], data0=a[:], data1=m[:], initial=ini[:], op0=ALU.mult, op1=ALU.add), reads=[ab, mb, inib], writes=[hb])
                    S.op("pool", lambda e, hh=hh, h=h: e.tensor_copy(out=hst[:, h:h + 1], in_=hh[:, TB - 1:TB]), reads=[hb], writes=[hst_b[h]])
                    if main:
                        py, pyb = PS.next()
                        mmgroup(py[:], pyb, [(w[:, 1, kc, :], xb[:, kc, :]) for kc in range(16)], [wb_, xbb])
                        gy, gyb = L["gy"].next()
                        S.op("act", lambda e, gy=gy, py=py: e.activation(out=gy[:], in_=py[:], func=AF.Gelu_apprx_tanh), reads=[pyb], writes=[gyb])
                        k_ = uacnt[0] % 2
                        uacnt[0] += 1
                        ut, utb = uar.next()
                        S.op("dve", lambda e, gy=gy, hh=hh, ut=ut: e.tensor_tensor(out=ut[:], in0=hh[:], in1=gy[:], op=ALU.mult), reads=[hb, gyb], writes=[utb])
                        S.dma("act", lambda e, ut=ut, h=h, blk=blk: e.dma_start(out=UA[blk - 12, :, h, :], in_=ut[:]), uas[k_], reads=[utb], writes=[])

                for h in range(_RH):
                    gc = GAMMA[h] ** 128
                    wts = {}
                    for j, nm in enumerate(("q", "k", "v0", "v1", "g0", "g1")):
                        if not main and nm in ("q", "g0", "g1"):
                            continue
                        w, wb_, ws = wq.next() if nm in ("q", "k") else (None, None, None)
                        if nm in ("q", "k"):
                            S.dma("sp", lambda e, w=w, h=h, j=j: e.dma_start(out=w[:], in_=b_in_r[h, j]), ws, reads=[dr_w], writes=[wb_])
                            wts[nm] = (w, wb_)
                    def proj_rope(nm, ring):
                        w, wb_ = wts[nm]
                        p0, p0b = PS.next()
                        mmgroup(p0[:], p0b, [(w[:, kc, 0:128], xb[:, kc, :]) for kc in range(16)], [wb_, xbb])
                        p1, p1b = PS.next()
                        mmgroup(p1[:], p1b, [(w[:, kc, 128:256], xb[:, kc, :]) for kc in range(16)], [wb_, xbb])
                        o, ob = ring.next()
                        t1, t1b = Rt.next()
                        t2, t2b = Rt.next()
                        S.op("dve", lambda e: e.tensor_tensor(out=t1[:], in0=p0[:], in1=ct[:, 0, :], op=ALU.mult), reads=[p0b, ctb], writes=[t1b])
                        S.op("dve", lambda e: e.tensor_tensor(out=t2[:], in0=p1[:], in1=ct[:, 1, :], op=ALU.mult), reads=[p1b, ctb], writes=[t2b])
                        S.op("pool", lambda e: e.tensor_tensor(out=o[:, 0, :], in0=t1[:], in1=t2[:], op=ALU.subtract), reads=[t1b, t2b], writes=[ob])
                        t3, t3b = Rt.next()
                        t4, t4b = Rt.next()
                        S.op("dve", lambda e: e.tensor_tensor(out=t3[:], in0=p0[:], in1=ct[:, 1, :], op=ALU.mult), reads=[p0b, ctb], writes=[t3b])
                        S.op("dve", lambda e: e.tensor_tensor(out=t4[:], in0=p1[:], in1=ct[:, 0, :], op=ALU.mult), reads=[p1b, ctb], writes=[t4b])
                        S.op("pool", lambda e: e.tensor_tensor(out=o[:, 1, :], in0=t3[:], in1=t4[:], op=ALU.add), reads=[t3b, t4b], writes=[ob])
                        return o, ob

                    kr, krb = proj_rope("k", Rk)
                    if main:
                        qr, qrb = proj_rope("q", Rq)
                    wv = []
                    for j in (2, 3):
                        w, wb_, ws = wq.next()
                        S.dma("sp", lambda e, w=w, h=h, j=j: e.dma_start(out=w[:], in_=b_in_r[h, j]), ws, reads=[dr_w], writes=[wb_])
                        wv.append((w, wb_))
                    wg_ = []
                    if main:
                        gt, gtb, gts = gn.next()
                        S.dma("act", lambda e, gt=gt, h=h: e.dma_start(out=gt[:], in_=gain_bc[:, h * 512:(h + 1) * 512]), gts, writes=[gtb])
                    if main:
                        k_ = uacnt[1] % 2
                        uacnt[1] += 1
                        ubt, ubtb = ubr.next()
                    for c in range(4):
                        tok = slice(c * 128, (c + 1) * 128)
                        pv, pvb = PS.next()
                        for hf in range(2):
                            n = 16
                            S.group("pe", [
                                (lambda e, kc=kc, hf=hf: e.matmul(pv[:, hf * 256:(hf + 1) * 256], lhsT=xb[:, kc, tok], rhs=wv[hf][0][:, kc, :], start=(kc == 0), stop=(kc == 15)))
                                for kc in range(16)], reads=[xbb, wv[hf][1]], writes=[pvb])
                        vb, vbb = Rv.next()
                        S.op("act", lambda e, vb=vb, pv=pv: e.copy(out=vb[:], in_=pv[:]), reads=[pvb], writes=[vbb])
                        kz, kzb = Rkz.next()
                        pt, ptb_ = PT.next()
                        S.group("pe", [(lambda e, pt=pt, j=j: e.transpose(pt[:, j, :], kr[:, j, tok], idb[:])) for j in range(2)], reads=[krb], writes=[ptb_])
                        S.op("dve", lambda e, pt=pt, kz=kz: e.tensor_scalar(out=kz[:].rearrange("p (a b) -> p a b", b=128), in0=pt[:, 0:2, :], scalar1=xz[:, 1, h:h + 1], scalar2=None, op0=ALU.mult), reads=[ptb_], writes=[kzb])
                        if main:
                            psc, pscb = PS.next()
                            mmgroup(psc[:, 0:128], pscb, [(kr[:, j, tok], qr[:, j, tok]) for j in range(2)], [krb, qrb])
                            sm, smb = Rs.next()
                            S.op("dve", lambda e, sm=sm, psc=psc: e.tensor_tensor(out=sm[:], in0=psc[:, 0:128], in1=dmk[:, h, :], op=ALU.mult), reads=[pscb], writes=[smb])
                            po, pob = PS.next()
                            S.op("pe", lambda e, po=po, sm=sm, vb=vb: e.matmul(po[:], lhsT=sm[:], rhs=vb[:], start=True, stop=True), reads=[smb, vbb], writes=[pob])
                            sbf, sbfb = Sb.next()
                            S.op("pool", lambda e, sbf=sbf: e.tensor_copy(out=sbf[:], in_=Sst[:, h]), reads=[S_b[h]], writes=[sbfb])
                            pc, pcb = PS.next()
                            mmgroup(pc[:], pcb, [(qr[:, j, tok], sbf[:, j, :]) for j in range(2)], [qrb, sbfb])
                            o1, o1b = Ro.next()
                            S.op("act", lambda e, o1=o1, po=po: e.copy(out=o1[:], in_=po[:]), reads=[pob], writes=[o1b])
                            o2, o2b = Ro.next()
                            S.op("dve", lambda e, o2=o2, pc=pc, o1=o1: e.scalar_tensor_tensor(out=o2[:], in0=pc[:], scalar=xz[:, 0, h:h + 1], in1=o1[:], op0=ALU.mult, op1=ALU.add), reads=[pcb, o1b], writes=[o2b])
                            stt, sttb = Rst.next()
                            S.op("dve", lambda e, stt=stt, o2=o2: e.bn_stats(out=stt[:, 0:6], in_=o2[:]), reads=[o2b], writes=[sttb])
                            S.op("dve", lambda e, stt=stt: e.bn_aggr(out=stt[:, 6:8], in_=stt[:, 0:6]), reads=[sttb], writes=[sttb])
                            S.op("dve", lambda e, stt=stt: e.tensor_scalar(out=stt[:, 7:8], in0=stt[:, 7:8], scalar1=LN_EPS, scalar2=None, op0=ALU.add), reads=[sttb], writes=[sttb])
                            S.op("act", lambda e, stt=stt: e.activation(out=stt[:, 7:8], in_=stt[:, 7:8], func=AF.Sqrt), reads=[sttb], writes=[sttb])
                            S.op("dve", lambda e, stt=stt: e.reciprocal(out=stt[:, 7:8], in_=stt[:, 7:8]), reads=[sttb], writes=[sttb])
                            S.op("dve", lambda e, stt=stt, o2=o2: e.tensor_scalar(out=o2[:], in0=o2[:], scalar1=stt[:, 6:7], scalar2=stt[:, 7:8], op0=ALU.subtract, op1=ALU.mult), reads=[sttb, o2b], writes=[o2b])
                            S.op("pool", lambda e, o2=o2, gt=gt: e.tensor_tensor(out=o2[:], in0=o2[:], in1=gt[:], op=ALU.mult), reads=[o2b, gtb], writes=[o2b])
                            if c == 0:
                                for j in (4, 5):
                                    w, wb_, ws = wq.next()
                                    S.dma("sp", lambda e, w=w, h=h, j=j: e.dma_start(out=w[:], in_=b_in_r[h, j]), ws, reads=[dr_w], writes=[wb_])
                                    wg_.append((w, wb_))
                            pg, pgb = PS.next()
                            for hf in range(2):
                                S.group("pe", [
                                    (lambda e, kc=kc, hf=hf: e.matmul(pg[:, hf * 256:(hf + 1) * 256], lhsT=xb[:, kc, tok], rhs=wg_[hf][0][:, kc, :], start=(kc == 0), stop=(kc == 15)))
                                    for kc in range(16)], reads=[xbb, wg_[hf][1]], writes=[pgb])
                            sg, sgb = Rsg.next()
                            S.op("act", lambda e, sg=sg, pg=pg: e.activation(out=sg[:], in_=pg[:], func=AF.Silu), reads=[pgb], writes=[sgb])
                            ub, ubb = Rub.next()
                            S.op("dve", lambda e, ub=ub, o2=o2, sg=sg: e.tensor_tensor(out=ub[:], in0=o2[:], in1=sg[:], op=ALU.mult), reads=[o2b, sgb], writes=[ubb])
                            pt, ptb_ = PT.next()
                            S.group("pe", [(lambda e, pt=pt, ub=ub, q4=q4: e.transpose(pt[:, q4, :], ub[:, q4 * 128:(q4 + 1) * 128], idb[:])) for q4 in range(4)], reads=[ubb], writes=[ptb_])
                            S.op("act", lambda e, pt=pt: e.copy(out=ubt[:, :, tok], in_=pt[:, 0:4, :]), reads=[ptb_], writes=[ubtb])
                        for j in range(2):
                            pd, pdb = PS.next()
                            S.op("pe", lambda e, pd=pd, kz=kz, vb=vb, j=j: e.matmul(pd[:], lhsT=kz[:, j * 128:(j + 1) * 128], rhs=vb[:], start=True, stop=True), reads=[kzb, vbb], writes=[pdb])
                            S.op("dve", lambda e, pd=pd, j=j: e.scalar_tensor_tensor(out=Sst[:, h, j, :], in0=Sst[:, h, j, :], scalar=gc, in1=pd[:], op0=ALU.mult, op1=ALU.add), reads=[pdb, S_b[h]], writes=[S_b[h]])

                    if main:
                        S.dma("act", lambda e, ubt=ubt, h=h, blk=blk: e.dma_start(out=UB[blk - 12, :, h * 4:(h + 1) * 4, :], in_=ubt[:]), ubs[k_], reads=[ubtb], writes=[])
            S.barrier()

        with ExitStack() as st:
            sb = lambda n, s, d=F32: st.enter_context(nc.sbuf_tensor(n, list(s), d))
            cq = S.dma_src("cq1b")
            bgt = sb("bgt2", [128, 2, 16])
            cb_ = Buf("consts1b")
            S.dma("sp", lambda e: e.dma_start(out=bgt[:], in_=bg), cq, writes=[cb_])
            xstg = DRing(S, nc, st, "xstg2", 2, [128, 2, TB], F32)
            xTb = Ring(nc, st, "xTb2", 1, [128, 16, TB], BF16)
            uaT = sb("uaT", [128, 16, TB], BF16)
            ubT = sb("ubT", [128, 32, TB], BF16)
            mxT = sb("mxT", [128, 16, TB], BF16)
            ua_b, ub_b, mx_b = Buf("uaT"), Buf("ubT"), Buf("mxT")
            uq = S.dma_src("uq")
            uq2 = S.dma_src("uq2")
            w3r = DRing(S, nc, st, "w3r", 5, [128, 20, 128], BF16)
            wor = DRing(S, nc, st, "wor", 3, [128, 8, 512], BF16)
            xrr = DRing(S, nc, st, "xrr", 1, [128, 4, D], F32)
            lnt = sb("lnt", [128, 2, D])
            lnb = cb_
            S.dma("act", lambda e: e.dma_start(out=lnt[:], in_=lnp[:, 0:2, :]), cq, writes=[lnb])
            G1 = Ring(nc, st, "g1", 1, [128, TB], F32)
            G2 = Ring(nc, st, "g2", 1, [128, TB], F32)
            Zst = Ring(nc, st, "zst", 2, [128, 32], F32)
            hsrc = S.dma_src("hs")

            def mmgroup(ps, psb, pairs, reads):
                n = len(pairs)
                S.group("pe", [
                    (lambda e, l=l, r=r, i=i: e.matmul(ps, lhsT=l, rhs=r, start=(i == 0), stop=(i == n - 1)))
                    for i, (l, r) in enumerate(pairs)], reads=reads, writes=[psb])

            for bi in range(4 if "1b" in _PH else 0):
                xb, xbb = xTb.next()
                for hf in range(8):
                    t, tb, ts = xstg.next()
                    S.dma("sp", lambda e, t=t, hf=hf, bi=bi: e.dma_start(out=t[:], in_=xT[12 + bi, :, hf * 2:(hf + 1) * 2, :]), ts, writes=[tb])
                    S.op("pool", lambda e, t=t, xb=xb, hf=hf: e.tensor_copy(out=xb[:, hf * 2:(hf + 1) * 2, :], in_=t[:]), reads=[tb], writes=[xbb])
                S.dma("act", lambda e, bi=bi: e.dma_start(out=uaT[:], in_=UA[bi]), uq, writes=[ua_b])
                S.dma("act", lambda e, bi=bi: e.dma_start(out=ubT[:], in_=UB[bi]), uq2, writes=[ub_b])
                for nt in range(16):
                    pcs = []
                    for q_ in range(4):
                        w, wb_, ws = w3r.next()
                        S.dma("sp", lambda e, w=w, nt=nt, q_=q_: e.dma_start(out=w[:], in_=b_w3[nt, :, q_ * 20:(q_ + 1) * 20, :]), ws, writes=[wb_])
                        pcs.append((w, wb_))
                    wsl = lambda kc: pcs[kc // 20][0][:, kc % 20, :]
                    wbs = lambda lo, hi: [pcs[q_][1] for q_ in range(lo // 20, (hi - 1) // 20 + 1)]
                    pa, pab = PS.next()
                    mmgroup(pa[:], pab, [(wsl(kc), uaT[:, kc, :]) for kc in range(16)], wbs(0, 16) + [ua_b])
                    pb, pbb = PS.next()
                    mmgroup(pb[:], pbb, [(wsl(16 + kc), ubT[:, kc, :]) for kc in range(32)], wbs(16, 48) + [ub_b])
                    pga, pgab = PS.next()
                    mmgroup(pga[:], pgab, [(wsl(48 + kc), xb[:, kc, :]) for kc in range(16)], wbs(48, 64) + [xbb])
                    pgb_, pgbb = PS.next()
                    mmgroup(pgb_[:], pgbb, [(wsl(64 + kc), xb[:, kc, :]) for kc in range(16)], wbs(64, 80) + [xbb])
                    s1, s1b = G1.next()
                    S.op("act", lambda e, s1=s1, pga=pga, nt=nt: e.activation(out=s1[:], in_=pga[:], func=AF.Sigmoid, bias=bgt[:, 0, nt:nt + 1]), reads=[pgab], writes=[s1b])
                    s2, s2b = G2.next()
                    S.op("act", lambda e, s2=s2, pgb_=pgb_, nt=nt: e.activation(out=s2[:], in_=pgb_[:], func=AF.Sigmoid, bias=bgt[:, 1, nt:nt + 1]), reads=[pgbb], writes=[s2b])
                    S.op("dve", lambda e, s1=s1, pa=pa: e.tensor_tensor(out=s1[:], in0=s1[:], in1=pa[:], op=ALU.mult), reads=[s1b, pab], writes=[s1b])
                    S.op("dve", lambda e, s2=s2, pb=pb: e.tensor_tensor(out=s2[:], in0=s2[:], in1=pb[:], op=ALU.mult), reads=[s2b, pbb], writes=[s2b])
                    S.op("pool", lambda e, s1=s1, s2=s2, nt=nt: e.tensor_tensor(out=mxT[:, nt, :], in0=s1[:], in1=s2[:], op=ALU.add), reads=[s1b, s2b], writes=[mx_b])
                xr, xrb, xrs = xrr.next()
                S.dma("act", lambda e, xr=xr, bi=bi: e.dma_start(out=xr[:], in_=xres[bi * TB:(bi + 1) * TB, :].rearrange("(t p) d -> p t d", p=128)), xrs, reads=[dr_H], writes=[xrb])
                for nb in range(4):
                    pcs = []
                    for q_ in range(2):
                        w, wb_, ws = wor.next()
                        S.dma("sp", lambda e, w=w, nb=nb, q_=q_: e.dma_start(out=w[:], in_=b_wo[nb, :, q_ * 8:(q_ + 1) * 8, :]), ws, writes=[wb_])
                        pcs.append((w, wb_))
                    for tt in range(4):
                        pz, pzb = PS.next()
                        mmgroup(pz[:], pzb, [(mxT[:, kc, tt * 128:(tt + 1) * 128], pcs[kc // 8][0][:, kc % 8, :]) for kc in range(16)], [pcs[0][1], pcs[1][1], mx_b])
                        S.op("dve", lambda e, xr=xr, pz=pz, tt=tt, nb=nb: e.scalar_tensor_tensor(out=xr[:, tt, nb * 512:(nb + 1) * 512], in0=xr[:, tt, nb * 512:(nb + 1) * 512], scalar=DN_ALPHA, in1=pz[:], op0=ALU.mult, op1=ALU.add), reads=[pzb, xrb], writes=[xrb])
                for tt in range(4):
                    zs, zsb = Zst.next()
                    for q4 in range(4):
                        S.op("dve", lambda e, zs=zs, xr=xr, tt=tt, q4=q4: e.bn_stats(out=zs[:, q4 * 6:(q4 + 1) * 6], in_=xr[:, tt, q4 * 512:(q4 + 1) * 512]), reads=[xrb], writes=[zsb])
                    S.op("dve", lambda e, zs=zs: e.bn_aggr(out=zs[:, 24:26], in_=zs[:, 0:24]), reads=[zsb], writes=[zsb])
                    S.op("dve", lambda e, zs=zs: e.tensor_scalar(out=zs[:, 25:26], in0=zs[:, 25:26], scalar1=LN_EPS, scalar2=None, op0=ALU.add), reads=[zsb], writes=[zsb])
                    S.op("act", lambda e, zs=zs: e.activation(out=zs[:, 25:26], in_=zs[:, 25:26], func=AF.Sqrt), reads=[zsb], writes=[zsb])
                    S.op("dve", lambda e, zs=zs: e.reciprocal(out=zs[:, 25:26], in_=zs[:, 25:26]), reads=[zsb], writes=[zsb])
                    S.op("dve", lambda e, zs=zs, xr=xr, tt=tt: e.tensor_scalar(out=xr[:, tt, :], in0=xr[:, tt, :], scalar1=zs[:, 24:25], scalar2=zs[:, 25:26], op0=ALU.subtract, op1=ALU.mult), reads=[zsb, xrb], writes=[xrb])
                    S.op("pool", lambda e, xr=xr, tt=tt: e.tensor_tensor(out=xr[:, tt, :], in0=xr[:, tt, :], in1=lnt[:, 0, :], op=ALU.mult), reads=[xrb, lnb], writes=[xrb])
                    S.op("pool", lambda e, xr=xr, tt=tt: e.tensor_tensor(out=xr[:, tt, :], in0=xr[:, tt, :], in1=lnt[:, 1, :], op=ALU.add), reads=[xrb, lnb], writes=[xrb])
                S.dma("sp", lambda e, xr=xr, bi=bi: e.dma_start(out=Hs[bi * TB:(bi + 1) * TB, :].rearrange("(t p) d -> p t d", p=128), in_=xr[:]), hsrc, reads=[xrb], writes=[dr_H])
            S.barrier()

        CAP = 512
        with ExitStack() as st2:
            sbp = lambda n, s, d=F32: st2.enter_context(nc.sbuf_tensor(n, list(s), d))
            cq2 = S.dma_src("cq2")
            c2b = Buf("consts2")
            idf2 = sbp("idf2", [128, 128])
            idb2 = sbp("idb2", [128, 128], BF16)
            ust = sbp("ust", [128, 2, 128])
            io512 = sbp("io512", [128, 512])
            io64 = sbp("io64", [128, 2, 64])
            tokc = sbp("tokc", [128, 16, 2])
            wall = sbp("wall", [128, 16, 64])
            selall = sbp("selall", [128, 16, 64])
            posall = sbp("posall", [128, 16, 64])
            eidf = sbp("eidf", [128, 16, 8])
            carry2 = sbp("carry2", [128, 64])
            rt_b = Buf("routing")
            for t_, a_ in ((idf2, ident), (ust, ustrict), (io512, iota512), (io64, iota64), (tokc, tokcol)):
                S.dma("sp", lambda e, t_=t_, a_=a_: e.dma_start(out=t_[:], in_=a_), cq2, writes=[c2b])
            S.op("dve", lambda e: e.tensor_copy(out=idb2[:], in_=idf2[:]), reads=[c2b], writes=[c2b])
            S.op("pool", lambda e: e.memset(carry2[:], 0.0), writes=[rt_b])

            def mmgroup(ps, psb, pairs, reads):
                n = len(pairs)
                S.group("pe", [
                    (lambda e, l=l, r=r, i=i: e.matmul(ps, lhsT=l, rhs=r, start=(i == 0), stop=(i == n - 1)))
                    for i, (l, r) in enumerate(pairs)], reads=reads, writes=[psb])

            def ffn(xT_, xTb_, wg, wgb, wu, wub, wd, wdb, h1, h1b, sgr, emit_y):
                for ft in range(4):
                    pg, pgb = PS.next()
                    mmgroup(pg[:], pgb, [(wg[:, kc, ft * 128:(ft + 1) * 128], xT_[:, kc, :]) for kc in range(16)], [wgb, xTb_])
                    pu, pub = PS.next()
                    mmgroup(pu[:], pub, [(wu[:, kc, ft * 128:(ft + 1) * 128], xT_[:, kc, :]) for kc in range(16)], [wub, xTb_])
                    sg, sgb = sgr.next()
                    S.op("act", lambda e, sg=sg, pg=pg: e.activation(out=sg[:], in_=pg[:], func=AF.Silu), reads=[pgb], writes=[sgb])
                    S.op("dve", lambda e, h1=h1, sg=sg, pu=pu, ft=ft: e.tensor_tensor(out=h1[:, ft, :], in0=sg[:], in1=pu[:], op=ALU.mult), reads=[sgb, pub], writes=[h1b])
                for tt in range(4):
                    for nb in range(4):
                        py, pyb = PS.next()
                        mmgroup(py[:], pyb, [(h1[:, ft, tt * 128:(tt + 1) * 128], wd[:, ft, nb * 512:(nb + 1) * 512]) for ft in range(4)], [h1b, wdb])
                        emit_y(tt, nb, py, pyb)

            with ExitStack() as st:
                sb = lambda n, s, d=F32: st.enter_context(nc.sbuf_tensor(n, list(s), d))
                wrt = sb("wrt", [128, 16, 64])
                rbt = sb("rbt", [128, 64])
                cq2b = S.dma_src("cq2b")
                cq2c = S.dma_src("cq2c")
                swq = S.dma_src("swq")
                r2b = Buf("rconsts")
                for t_, a_ in ((wrt, wr), (rbt, rb_bc)):
                    S.dma("sp", lambda e, t_=t_, a_=a_: e.dma_start(out=t_[:], in_=a_), cq2b, writes=[r2b])
                zr = sb("zr", [128, D], BF16)
                zrb = Buf("zr")
                S.op("pool", lambda e: e.memset(zr[:], 0.0), writes=[zrb])
                S.dma("sp", lambda e: e.dma_start(out=Hb[NB:NB + 128, :], in_=zr[:]), cq2c, reads=[zrb], writes=[])
                hT = Ring(nc, st, "hT32", 1, [128, 16, 128], F32)
                hld = DRing(S, nc, st, "hld", 1, [128, 4, D], F32)
                hbr = Ring(nc, st, "hbr", 2, [128, D], BF16)
                hbs = [S.dma_src("hbs%d" % k) for k in range(2)]
                R1 = Ring(nc, st, "rt1", 2, [128, 64], F32)
                R2 = Ring(nc, st, "rt2", 2, [128, 64], F32)
                R3 = Ring(nc, st, "rt3", 2, [128, 64], F32)
                R8 = Ring(nc, st, "rt8", 2, [128, 4, 8], F32)
                RI = Ring(nc, st, "rti", 2, [128, 8], mybir.dt.uint32)
                hTb = sb("hTb", [128, 16, TB], BF16)
                hTb_b = Buf("hTb")
                h1T = Ring(nc, st, "h1T", 1, [128, 4, TB], BF16)
                Rsg2 = Ring(nc, st, "sg2", 2, [128, TB], F32)
                swg = sb("swg", [128, 16, 512], BF16)
                swu = sb("swu", [128, 16, 512], BF16)
                swd = sb("swd", [128, 4, D], BF16)
                swb = Buf("sw")
                S.dma("sp", lambda e: e.dma_start(out=swg[:], in_=b_wge[0]), swq, writes=[swb])
                S.dma("act", lambda e: e.dma_start(out=swu[:], in_=b_wue[0]), swq, writes=[swb])
                S.dma("sp", lambda e: e.dma_start(out=swd[:], in_=b_wde[0]), swq, writes=[swb])
                ysr = Ring(nc, st, "ysr", 2, [128, D], F32)
                yss = [S.dma_src("yss%d" % k) for k in range(2)]
                hcnt = [0, 0]
                for bi in range(4 if "2" in _PH else 0):
                    hx, hxb, hxs = hld.next()
                    S.dma("sp", lambda e, hx=hx, bi=bi: e.dma_start(out=hx[:], in_=Hs[bi * TB:(bi + 1) * TB, :].rearrange("(t p) d -> p t d", p=128)), hxs, writes=[hxb])
                    for tt in range(4):
                        tg = bi * 4 + tt
                        k_ = hcnt[0] % 2
                        hcnt[0] += 1
                        hb_, hbb = hbr.next()
                        S.op("pool", lambda e, hb_=hb_, hx=hx, tt=tt: e.tensor_copy(out=hb_[:], in_=hx[:, tt, :]), reads=[hxb], writes=[hbb])
                        S.dma("act", lambda e, hb_=hb_, tg=tg: e.dma_start(out=Hb[tg * 128:(tg + 1) * 128, :], in_=hb_[:]), hbs[k_], reads=[hbb], writes=[])
                        h32, h32b = hT.next()
                        for k4 in range(4):
                            pp, ppb = PS.next()
                            S.group("pe", [(lambda e, pp=pp, hx=hx, tt=tt, kc=k4 * 4 + q4, q4=q4: e.transpose(pp[:, q4 * 128:(q4 + 1) * 128], hx[:, tt, kc * 128:(kc + 1) * 128], idf2[:])) for q4 in range(4)], reads=[hxb, c2b], writes=[ppb])
                            S.op("act", lambda e, pp=pp, h32=h32, k4=k4: e.copy(out=h32[:, k4 * 4:(k4 + 1) * 4, :], in_=pp[:].rearrange("p (a b) -> p a b", b=128)), reads=[ppb], writes=[h32b])
                            S.op("pool", lambda e, h32=h32, k4=k4, tt=tt: e.tensor_copy(out=hTb[:, k4 * 4:(k4 + 1) * 4, tt * 128:(tt + 1) * 128], in_=h32[:, k4 * 4:(k4 + 1) * 4, :]), reads=[h32b], writes=[hTb_b])
                        pl, plb = PS.next()
                        mmgroup(pl[:, 0:64], plb, [(h32[:, kc, :], wrt[:, kc, :]) for kc in range(16)], [h32b, r2b])
                        sc, scb = R1.next()
                        S.op("act", lambda e, sc=sc, pl=pl: e.activation(out=sc[:], in_=pl[:, 0:64], func=AF.Sigmoid), reads=[plb], writes=[scb])
                        bs, bsb = R2.next()
                        S.op("dve", lambda e, bs=bs, sc=sc: e.tensor_tensor(out=bs[:], in0=sc[:], in1=rbt[:], op=ALU.add), reads=[scb, r2b], writes=[bsb])
                        t8, t8b = R3.next()
                        for g in range(8):
                            S.op("dve", lambda e, t8=t8, bs=bs, g=g: e.max(out=t8[:, g * 8:(g + 1) * 8], in_=bs[:, g * 8:(g + 1) * 8]), reads=[bsb], writes=[t8b])
                        sm8, sm8b = R8.next()
                        t8v = t8[:].rearrange("p (g k) -> p g k", k=8)
                        S.op("dve", lambda e, sm8=sm8, t8v=t8v: e.tensor_tensor(out=sm8[:, 0, :], in0=t8v[:, :, 0], in1=t8v[:, :, 1], op=ALU.add), reads=[t8b], writes=[sm8b])
                        S.op("dve", lambda e, sm8=sm8: e.max(out=sm8[:, 1, :], in_=sm8[:, 0, :]), reads=[sm8b], writes=[sm8b])
                        S.op("dve", lambda e, sm8=sm8: e.tensor_scalar(out=sm8[:, 2, :], in0=sm8[:, 0, :], scalar1=sm8[:, 1, 3:4], scalar2=None, op0=ALU.is_ge), reads=[sm8b], writes=[sm8b])
                        S.op("dve", lambda e, sm8=sm8: e.tensor_scalar(out=sm8[:, 2, :], in0=sm8[:, 2, :], scalar1=-1.0, scalar2=1e9, op0=ALU.add, op1=ALU.mult), reads=[sm8b], writes=[sm8b])
                        bm, bmb = R3.next()
                        S.op("dve", lambda e, bm=bm, bs=bs, sm8=sm8: e.tensor_tensor(out=bm[:].rearrange("p (g k) -> p g k", k=8), in0=bs[:].rearrange("p (g k) -> p g k", k=8), in1=sm8[:, 2, :].unsqueeze(2).to_broadcast([128, 8, 8]), op=ALU.add), reads=[bsb, sm8b], writes=[bmb])
                        S.op("dve", lambda e, sm8=sm8, bm=bm: e.max(out=sm8[:, 3, :], in_=bm[:]), reads=[bmb], writes=[sm8b])
                        ei, eib = RI.next()
                        S.op("dve", lambda e, ei=ei, sm8=sm8, bm=bm: e.max_index(out=ei[:], in_max=sm8[:, 3, :], in_values=bm[:]), reads=[bmb, sm8b], writes=[eib])
                        S.op("dve", lambda e, ei=ei, tg=tg: e.tensor_copy(out=eidf[:, tg, :], in_=ei[:]), reads=[eib], writes=[rt_b])
                        S.op("dve", lambda e, sm8=sm8, bm=bm, tg=tg: e.tensor_scalar(out=selall[:, tg, :], in0=bm[:], scalar1=sm8[:, 3, 7:8], scalar2=None, op0=ALU.is_ge), reads=[bmb, sm8b], writes=[rt_b])
                        S.op("dve", lambda e, bm=bm, sc=sc, tg=tg: e.tensor_tensor(out=bm[:], in0=selall[:, tg, :], in1=sc[:], op=ALU.mult), reads=[rt_b, scb, bmb], writes=[bmb])
                        S.op("dve", lambda e, sm8=sm8, bm=bm: e.reduce_sum(out=sm8[:, 1, 0:1], in_=bm[:], axis=AX.X), reads=[bmb], writes=[sm8b])
                        S.op("dve", lambda e, sm8=sm8: e.reciprocal(out=sm8[:, 1, 1:2], in_=sm8[:, 1, 0:1]), reads=[sm8b], writes=[sm8b])
                        S.op("dve", lambda e, sm8=sm8, bm=bm, tg=tg: e.tensor_scalar(out=wall[:, tg, :], in0=bm[:], scalar1=sm8[:, 1, 1:2], scalar2=2.5, op0=ALU.mult, op1=ALU.mult), reads=[bmb, sm8b], writes=[rt_b])
                        pq, pqb = PS.next()
                        S.op("pe", lambda e, pq=pq, tg=tg: e.matmul(pq[:, 0:64], lhsT=ust[:, 0, :], rhs=selall[:, tg, :], start=True, stop=True), reads=[rt_b, c2b], writes=[pqb])
                        S.op("dve", lambda e, pq=pq, tg=tg: e.tensor_tensor(out=posall[:, tg, :], in0=pq[:, 0:64], in1=carry2[:], op=ALU.add), reads=[pqb, rt_b], writes=[rt_b])
                        S.op("dve", lambda e, tg=tg: e.tensor_scalar(out=posall[:, tg, :], in0=posall[:, tg, :], scalar1=float(CAP - 1), scalar2=1.0, op0=ALU.min, op1=ALU.add), reads=[rt_b], writes=[rt_b])
                        S.op("dve", lambda e, tg=tg: e.tensor_tensor(out=posall[:, tg, :], in0=posall[:, tg, :], in1=selall[:, tg, :], op=ALU.mult), reads=[rt_b], writes=[rt_b])
                        S.op("dve", lambda e, tg=tg: e.tensor_scalar(out=posall[:, tg, :], in0=posall[:, tg, :], scalar1=-1.0, scalar2=None, op0=ALU.add), reads=[rt_b], writes=[rt_b])
                        pq2, pq2b = PS.next()
                        S.op("pe", lambda e, pq2=pq2, tg=tg: e.matmul(pq2[:, 0:64], lhsT=ust[:, 1, :], rhs=selall[:, tg, :], start=True, stop=True), reads=[rt_b, c2b], writes=[pq2b])
                        S.op("dve", lambda e, pq2=pq2: e.tensor_tensor(out=carry2[:], in0=pq2[:, 0:64], in1=carry2[:], op=ALU.add), reads=[pq2b, rt_b], writes=[rt_b])
                    h1, h1b = h1T.next()
                    ys_cur = {}

                    def emit_sh(tt, nb, py, pyb, bi=bi, ys_cur=ys_cur):
                        if nb == 0:
                            k_ = hcnt[1] % 2
                            hcnt[1] += 1
                            ys_cur["t"] = ysr.next() + (k_,)
                        yt, ytb, k_ = ys_cur["t"]
                        S.op("act", lambda e, yt=yt, py=py, nb=nb: e.copy(out=yt[:, nb * 512:(nb + 1) * 512], in_=py[:]), reads=[pyb], writes=[ytb])
                        if nb == 3:
                            tg = bi * 4 + tt
                            S.dma("act", lambda e, yt=yt, tg=tg: e.dma_start(out=YS[tg * 128:(tg + 1) * 128, :], in_=yt[:]), yss[k_], reads=[ytb], writes=[])

                    ffn(hTb, hTb_b, swg, swb, swu, swb, swd, swb, h1, h1b, Rsg2, emit_sh)
                S.barrier()

            with ExitStack() as st:
                wst = DRing(S, nc, st, "wst", 2, [128, 4096], F32)
                wgr = Ring(nc, st, "wgr", 3, [128, 16, 512], BF16)
                wdr2 = Ring(nc, st, "wdr2", 2, [128, 4, D], BF16)
                xgr = DRing(S, nc, st, "xgr", 2, [128, 4, D], BF16)
                xTr = Ring(nc, st, "xTr", 1, [128, 16, CAP], BF16)
                h1r = Ring(nc, st, "h1r", 1, [128, 4, CAP], BF16)
                sgr = Ring(nc, st, "sgr", 2, [128, CAP], F32)
                ysb = Ring(nc, st, "ysb", 1, [128, 4, D], BF16)
                ysd = S.dma_src("ysd")
                Qr = Ring(nc, st, "Qr", 2, [128, CAP], F32)
                ixf = Ring(nc, st, "ixf", 2, [128, 4, 2], F32)
                ixu = Ring(nc, st, "ixu", 2, [128, 4], mybir.dt.uint32)
                ccnt = [0]

                def load_w(e_):
                    out = []
                    for (src, nk, ring) in ((wge, 16, wgr), (wue, 16, wgr), (wde, 4, wdr2)):
                        wt, wtb = ring.next()
                        flat = wt[:].rearrange("p a b -> p (a b)")
                        srcf = src[e_].rearrange("p a b -> p (a b)")
                        for hf in range(2):
                            t, tb, ts = wst.next()
                            S.dma("sp", lambda e, t=t, srcf=srcf, hf=hf: e.dma_start(out=t[:], in_=srcf[:, hf * 4096:(hf + 1) * 4096]), ts, writes=[tb])
                            eng = ("pool", "act")[ccnt[0] % 2]
                            ccnt[0] += 1
                            if eng == "act":
                                S.op("act", lambda e, t=t, flat=flat, hf=hf: e.copy(out=flat[:, hf * 4096:(hf + 1) * 4096], in_=t[:]), reads=[tb], writes=[wtb])
                            else:
                                S.op("pool", lambda e, t=t, flat=flat, hf=hf: e.tensor_copy(out=flat[:, hf * 4096:(hf + 1) * 4096], in_=t[:]), reads=[tb], writes=[wtb])
                        out += [wt, wtb]
                    return out

                def lists_and_gather(e_):
                    banks = [PS.next() for _ in range(4)]
                    for tg in range(16):
                        q, qb = Qr.next()
                        S.op("dve", lambda e, q=q, tg=tg, e_=e_: e.tensor_scalar(out=q[:], in0=io512[:], scalar1=posall[:, tg, e_:e_ + 1], scalar2=None, op0=ALU.is_equal), reads=[rt_b, c2b], writes=[qb])
                        for rg in range(4):
                            pb_, pbb_ = banks[rg]
                            S.op("pe", lambda e, pb_=pb_, q=q, rg=rg, tg=tg: e.matmul(pb_[:, 0:2], lhsT=q[:, rg * 128:(rg + 1) * 128], rhs=tokc[:, tg, :], start=(tg == 0), stop=(tg == 15)), reads=[qb, c2b], writes=[pbb_])
                    xf, xfb = ixf.next()
                    xu, xub = ixu.next()
                    for rg in range(4):
                        pb_, pbb_ = banks[rg]
                        S.op("dve", lambda e, xf=xf, pb_=pb_, rg=rg: e.tensor_copy(out=xf[:, rg, :], in_=pb_[:, 0:2]), reads=[pbb_], writes=[xfb])
                    S.op("dve", lambda e, xf=xf: e.tensor_scalar(out=xf[:, :, 1], in0=xf[:, :, 1], scalar1=-float(NB), scalar2=float(NB), op0=ALU.mult, op1=ALU.add), reads=[xfb], writes=[xfb])
                    S.op("dve", lambda e, xf=xf: e.tensor_tensor(out=xf[:, :, 0], in0=xf[:, :, 0], in1=xf[:, :, 1], op=ALU.add), reads=[xfb], writes=[xfb])
                    S.op("dve", lambda e, xf=xf: e.tensor_scalar(out=xf[:, :, 0], in0=xf[:, :, 0], scalar1=float(NB), scalar2=0.0, op0=ALU.min, op1=ALU.max), reads=[xfb], writes=[xfb])
                    S.op("dve", lambda e, xf=xf, xu=xu: e.tensor_copy(out=xu[:], in_=xf[:, :, 0]), reads=[xfb], writes=[xub])
                    xg, xgb, xgs = xgr.next()
                    for rg in range(4):
                        S.dma("pool", lambda e, xg=xg, xu=xu, rg=rg: e.indirect_dma_start(out=xg[:, rg, :], out_offset=None, in_=Hb, in_offset=bass.IndirectOffsetOnAxis(ap=xu[:, rg:rg + 1], axis=0)), xgs, reads=[xub], writes=[xgb])
                    return xg, xgb

                nexp = min(N_EXP, 64) if "2" in _PH else 0
                pend = lists_and_gather(0) if nexp else None
                for ex in range(nexp):
                    wg, wgb, wu, wub, wd, wdb = load_w(ex)
                    xg, xgb = pend
                    if ex + 1 < nexp:
                        pend = lists_and_gather(ex + 1)
                    xT_, xTb_ = xTr.next()
                    for rg in range(4):
                        for k8 in range(2):
                            pt, ptb_ = PT.next()
                            S.group("pe", [(lambda e, pt=pt, xg=xg, rg=rg, kc=k8 * 8 + j, j=j: e.transpose(pt[:, j, :], xg[:, rg, kc * 128:(kc + 1) * 128], idb2[:])) for j in range(8)], reads=[xgb, c2b], writes=[ptb_])
                            eng = "act" if (rg * 2 + k8) % 2 == 0 else "dve"
                            if eng == "act":
                                S.op("act", lambda e, pt=pt, xT_=xT_, rg=rg, k8=k8: e.copy(out=xT_[:, k8 * 8:(k8 + 1) * 8, rg * 128:(rg + 1) * 128], in_=pt[:]), reads=[ptb_], writes=[xTb_])
                            else:
                                S.op("dve", lambda e, pt=pt, xT_=xT_, rg=rg, k8=k8: e.tensor_copy(out=xT_[:, k8 * 8:(k8 + 1) * 8, rg * 128:(rg + 1) * 128], in_=pt[:]), reads=[ptb_], writes=[xTb_])
                    h1, h1b = h1r.next()
                    yt, ytb = ysb.next()

                    def emit_y(tt, nb, py, pyb, yt=yt, ytb=ytb):
                        if (tt * 4 + nb) % 2 == 0:
                            S.op("act", lambda e, yt=yt, py=py, tt=tt, nb=nb: e.copy(out=yt[:, tt, nb * 512:(nb + 1) * 512], in_=py[:]), reads=[pyb], writes=[ytb])
                        else:
                            S.op("dve", lambda e, yt=yt, py=py, tt=tt, nb=nb: e.tensor_copy(out=yt[:, tt, nb * 512:(nb + 1) * 512], in_=py[:]), reads=[pyb], writes=[ytb])

                    ffn(xT_, xTb_, wg, wgb, wu, wub, wd, wdb, h1, h1b, sgr, emit_y)
                    S.dma("act", lambda e, yt=yt, ex=ex: e.dma_start(out=Yx[ex * CAP:(ex + 1) * CAP, :].rearrange("(g p) n -> p g n", p=128), in_=yt[:]), ysd, reads=[ytb], writes=[])
                S.barrier()

            with ExitStack() as st:
                sb = lambda n, s, d=F32: st.enter_context(nc.sbuf_tensor(n, list(s), d))
                ln2 = sb("ln2", [128, 2, D])
                cq2d = S.dma_src("cq2d")
                l2b = Buf("ln2")
                S.dma("sp", lambda e: e.dma_start(out=ln2[:], in_=lnp[:, 2:4, :]), cq2d, writes=[l2b])
                hr = DRing(S, nc, st, "hr3", 2, [128, D], F32)
                yr = DRing(S, nc, st, "yr3", 2, [128, D], F32)
                gk = DRing(S, nc, st, "gk3", 4, [128, D], BF16)
                acc = Ring(nc, st, "acc3", 2, [128, D], F32)
                oh = Ring(nc, st, "oh3", 2, [128, 64], F32)
                sw = Ring(nc, st, "sw3", 2, [128, 2, 8], F32)
                su = Ring(nc, st, "su3", 2, [128, 8], mybir.dt.uint32)
                Zs2 = Ring(nc, st, "zs2", 2, [128, 32], F32)
                osr = [S.dma_src("os%d" % k) for k in range(2)]
                for tg in range(16 if "2" in _PH else 0):
                    ht, htb, hts = hr.next()
                    S.dma("sp", lambda e, ht=ht, tg=tg: e.dma_start(out=ht[:], in_=Hs[tg * 128:(tg + 1) * 128, :]), hts, writes=[htb])
                    yt, ytb, yts = yr.next()
                    S.dma("act", lambda e, yt=yt, tg=tg: e.dma_start(out=yt[:], in_=YS[tg * 128:(tg + 1) * 128, :]), yts, writes=[ytb])
                    S.op("dve", lambda e, tg=tg: e.tensor_tensor(out=posall[:, tg, :], in0=posall[:, tg, :], in1=io64[:, 1, :], op=ALU.add), reads=[rt_b, c2b], writes=[rt_b])
                    s_, s_b = sw.next()
                    for k in range(8):
                        o_, o_b = oh.next()
                        S.op("dve", lambda e, o_=o_, tg=tg, k=k: e.tensor_scalar(out=o_[:], in0=io64[:, 0, :], scalar1=eidf[:, tg, k:k + 1], scalar2=None, op0=ALU.is_equal), reads=[rt_b, c2b], writes=[o_b])
                        o2_, o2_b = oh.next()
                        S.op("dve", lambda e, o_=o_, o2_=o2_, tg=tg: e.tensor_tensor(out=o2_[:], in0=o_[:], in1=posall[:, tg, :], op=ALU.mult), reads=[o_b, rt_b], writes=[o2_b])
                        S.op("dve", lambda e, o2_=o2_, s_=s_, k=k: e.reduce_sum(out=s_[:, 0, k:k + 1], in_=o2_[:], axis=AX.X), reads=[o2_b], writes=[s_b])
                        S.op("dve", lambda e, o_=o_, tg=tg: e.tensor_tensor(out=o_[:], in0=o_[:], in1=wall[:, tg, :], op=ALU.mult), reads=[o_b, rt_b], writes=[o_b])
                        S.op("dve", lambda e, o_=o_, s_=s_, k=k: e.reduce_sum(out=s_[:, 1, k:k + 1], in_=o_[:], axis=AX.X), reads=[o_b], writes=[s_b])
                    u_, u_b = su.next()
                    S.op("dve", lambda e, u_=u_, s_=s_: e.tensor_copy(out=u_[:], in_=s_[:, 0, :]), reads=[s_b], writes=[u_b])
                    a_, a_b = acc.next()
                    S.op("dve", lambda e, a_=a_, ht=ht, yt=yt: e.scalar_tensor_tensor(out=a_[:], in0=ht[:], scalar=DN_ALPHA, in1=yt[:], op0=ALU.mult, op1=ALU.add), reads=[htb, ytb], writes=[a_b])
                    for k in range(8):
                        g_, g_b, g_s = gk.next()
                        S.dma("pool", lambda e, g_=g_, u_=u_, k=k: e.indirect_dma_start(out=g_[:], out_offset=None, in_=Yx, in_offset=bass.IndirectOffsetOnAxis(ap=u_[:, k:k + 1], axis=0)), g_s, reads=[u_b], writes=[g_b])
                        S.op("dve", lambda e, a_=a_, g_=g_, s_=s_, k=k: e.scalar_tensor_tensor(out=a_[:], in0=g_[:], scalar=s_[:, 1, k:k + 1], in1=a_[:], op0=ALU.mult, op1=ALU.add), reads=[g_b, s_b, a_b], writes=[a_b])
                    zs, zsb = Zs2.next()
                    for q4 in range(4):
                        S.op("dve", lambda e, zs=zs, a_=a_, q4=q4: e.bn_stats(out=zs[:, q4 * 6:(q4 + 1) * 6], in_=a_[:, q4 * 512:(q4 + 1) * 512]), reads=[a_b], writes=[zsb])
                    S.op("dve", lambda e, zs=zs: e.bn_aggr(out=zs[:, 24:26], in_=zs[:, 0:24]), reads=[zsb], writes=[zsb])
                    S.op("dve", lambda e, zs=zs: e.tensor_scalar(out=zs[:, 25:26], in0=zs[:, 25:26], scalar1=LN_EPS, scalar2=None, op0=ALU.add), reads=[zsb], writes=[zsb])
                    S.op("act", lambda e, zs=zs: e.activation(out=zs[:, 25:26], in_=zs[:, 25:26], func=AF.Sqrt), reads=[zsb], writes=[zsb])
                    S.op("dve", lambda e, zs=zs: e.reciprocal(out=zs[:, 25:26], in_=zs[:, 25:26]), reads=[zsb], writes=[zsb])
                    S.op("dve", lambda e, zs=zs, a_=a_: e.tensor_scalar(out=a_[:], in0=a_[:], scalar1=zs[:, 24:25], scalar2=zs[:, 25:26], op0=ALU.subtract, op1=ALU.mult), reads=[zsb, a_b], writes=[a_b])
                    S.op("pool", lambda e, a_=a_: e.tensor_tensor(out=a_[:], in0=a_[:], in1=ln2[:, 0, :], op=ALU.mult), reads=[a_b, l2b], writes=[a_b])
                    S.op("pool", lambda e, a_=a_: e.tensor_tensor(out=a_[:], in0=a_[:], in1=ln2[:, 1, :], op=ALU.add), reads=[a_b, l2b], writes=[a_b])
                    S.dma("sp", lambda e, a_=a_, tg=tg: e.dma_start(out=out[tg * 128:(tg + 1) * 128, :], in_=a_[:]), osr[tg % 2], reads=[a_b], writes=[])
                S.barrier()
        S.finish()
        print("instructions:", S.n_inst, flush=True)
    return nc


def _consts(p):
    half = 128
    freq = (10000.0 ** (-np.arange(half, dtype=np.float64) / half))
    cosT = np.zeros((16, 128, TB), np.float32)
    sinT = np.zeros((16, 128, TB), np.float32)
    flags = np.zeros((128, 2, 16), np.float32)
    flags[:, 1, :] = 1.0
    for blk in range(16):
        q = p - 3 + blk // 4
        if q < 0:
            continue
        pos = q * NB + (blk % 4) * TB + np.arange(TB, dtype=np.float64)
        ang = (pos[None, :].astype(np.float32) * freq[:, None].astype(np.float32)).astype(np.float32)
        cosT[blk] = np.cos(ang)
        sinT[blk] = np.sin(ang)
        if q == 0 and blk % 4 == 0:
            flags[:, 0, blk] = 1.0
            flags[:, 1, blk] = 0.0
    return cosT, sinT, flags


def kernel(x, w_in, conv_w, conv_b, lru_wa, lru_ba, lru_wi, lru_bi, lru_lambda, ret_gn_gain,
           w_lru_out, w_ret_out, b_gate, w_o, ln1_g, ln1_b, w_router, router_bias,
           w_gate_e, w_up_e, w_down_e, w_gate_s, w_up_s, w_down_s, ln2_g, ln2_b):
    import time
    _t0 = time.time()
    f = lambda a: np.ascontiguousarray(np.asarray(a, dtype=np.float32))
    x = f(x)
    w_in = f(w_in)[0]
    kp = lambda w: w.reshape(16, 128, -1).transpose(1, 0, 2)
    wx, wy = w_in[:, 0:2048], w_in[:, 2048:4096]
    wq_, wk_ = w_in[:, 4096:6144], w_in[:, 6144:8192]
    wv_, wg_ = w_in[:, 8192:12288], w_in[:, 12288:16384]
    wga, wgb = w_in[:, 16384:18432], w_in[:, 18432:20480]
    w_in_l = np.stack([np.stack([kp(wx[:, h * 128:(h + 1) * 128]), kp(wy[:, h * 128:(h + 1) * 128])], 1) for h in range(16)])
    w_in_r = np.stack([np.stack([
        kp(wq_[:, h * 256:(h + 1) * 256]), kp(wk_[:, h * 256:(h + 1) * 256]),
        kp(wv_[:, h * 512:h * 512 + 256]), kp(wv_[:, h * 512 + 256:(h + 1) * 512]),
        kp(wg_[:, h * 512:h * 512 + 256]), kp(wg_[:, h * 512 + 256:(h + 1) * 512])]) for h in range(8)])
    wlo, wro, wo_ = f(w_lru_out)[0], f(w_ret_out)[0], f(w_o)[0]
    kp2 = lambda w, n: w.reshape(n, 128, -1).transpose(1, 0, 2)
    w3 = np.stack([np.concatenate([
        kp2(wlo[:, nt * 128:(nt + 1) * 128], 16), kp2(wro[:, nt * 128:(nt + 1) * 128], 32),
        kp2(wga[:, nt * 128:(nt + 1) * 128], 16), kp2(wgb[:, nt * 128:(nt + 1) * 128], 16)], 1) for nt in range(16)])
    wo_t = np.stack([kp(wo_[:, nb * 512:(nb + 1) * 512]) for nb in range(4)])
    wge = np.concatenate([f(w_gate_e)[0], f(w_gate_s)], 0).reshape(65, 16, 128, 512).transpose(0, 2, 1, 3)
    wue = np.concatenate([f(w_up_e)[0], f(w_up_s)], 0).reshape(65, 16, 128, 512).transpose(0, 2, 1, 3)
    wde = np.concatenate([f(w_down_e)[0], f(w_down_s)], 0).reshape(65, 4, 128, 2048).transpose(0, 2, 1, 3)
    wr_t = kp(f(w_router)[0])
    chp = lambda v: f(v).reshape(16, 128).T
    cw = f(conv_w)[0]
    lrup = np.stack([chp(cw[0]), chp(cw[1]), chp(cw[2]), chp(cw[3]), chp(conv_b), chp(lru_ba), chp(lru_bi), chp(lru_lambda)], 2)
    wai = np.stack([f(lru_wa)[0].transpose(1, 0, 2), f(lru_wi)[0].transpose(1, 0, 2)], 2)
    bgv = f(b_gate)[0]
    bg = np.stack([bgv[:2048].reshape(16, 128).T, bgv[2048:].reshape(16, 128).T], 1)
    bc = lambda v, n: np.ascontiguousarray(np.broadcast_to(f(v).reshape(1, -1), (128, n)))
    lnp = np.stack([bc(ln1_g, D), bc(ln1_b, D), bc(ln2_g, D), bc(ln2_b, D)], 1)
    idx = np.arange(128, dtype=np.float64)
    dmaskT = np.zeros((128, 8, 128), np.float32)
    xizeta = np.zeros((128, 2, 8), np.float32)
    for h in range(8):
        lg = np.log1p(-np.exp2(-5.0 - h))
        diff = idx[None, :] - idx[:, None]
        dmaskT[:, h, :] = np.where(diff >= 0, np.exp(np.maximum(diff, 0) * lg), 0.0) / 16.0
        xizeta[:, 0, h] = np.exp((idx + 1.0) * lg)
        xizeta[:, 1, h] = np.exp((127.0 - idx) * lg) / 16.0
    ustrict = np.zeros((128, 2, 128), np.float32)
    ustrict[:, 0, :] = (np.arange(128)[:, None] < np.arange(128)[None, :]).astype(np.float32)
    ustrict[:, 1, :] = 1.0
    iota512 = np.broadcast_to(np.arange(512, dtype=np.float32)[None, :], (128, 512))
    iota64 = np.stack([np.broadcast_to(np.arange(64, dtype=np.float32)[None, :], (128, 64)),
                       np.broadcast_to(512.0 * np.arange(64, dtype=np.float32)[None, :], (128, 64))], 1)
    tokcol = np.zeros((128, 16, 2), np.float32)
    tokcol[:, :, 0] = np.arange(16)[None, :] * 128 + np.arange(128)[:, None]
    tokcol[:, :, 1] = 1.0
    shared = dict(ustrict=ustrict, iota512=iota512, iota64=iota64, tokcol=tokcol, w_in_l=w_in_l, w_in_r=w_in_r, w3=w3, wo=wo_t, wge=wge, wue=wue, wde=wde, wr=wr_t,
                  lrup=lrup, wai=wai, bg=bg, dmaskT=dmaskT, xizeta=xizeta, gain_bc=bc(ret_gn_gain, 4096),
                  lnp=lnp, rb_bc=bc(router_bias, 64), ident=np.eye(128, dtype=np.float32))
    shared = {k: np.ascontiguousarray(v, dtype=np.float32) for k, v in shared.items()}
    in_maps = []
    for c in range(8):
        b, p = divmod(c, 4)
        cosT, sinT, flags = _consts(p)
        xT = np.zeros((16, 128, 16, TB), np.float32)
        for blk in range(16):
            q = p - 3 + blk // 4
            if q < 0:
                continue
            t0 = q * NB + (blk % 4) * TB
            xT[blk] = x[b, t0:t0 + TB, :].reshape(TB, 16, 128).transpose(2, 1, 0)
        m = dict(shared)
        m.update(xT=xT, xres=np.ascontiguousarray(x[b, p * NB:(p + 1) * NB, :]), cosT=cosT, sinT=sinT, flags=flags)
        in_maps.append(m)
    print("[kernel] host layout %.1fs" % (time.time() - _t0), flush=True)
    nc = build()
    print("[kernel] build %.1fs" % (time.time() - _t0), flush=True)
    res = run_bass_kernel_spmd(nc, in_maps, core_ids=list(range(8)))
    print("[kernel] run done %.1fs" % (time.time() - _t0), flush=True)
    if os.environ.get("MK_DBG", "0") == "1":
        _LAST.clear()
        _LAST.append(res.results)
    outp = np.zeros((2, SEQ, D), np.float32)
    for c in range(8):
        b, p = divmod(c, 4)
        outp[b, p * NB:(p + 1) * NB, :] = res.results[c]["out"]
    return outp
```

```python
import os
import types
import numpy as np
from contextlib import ExitStack
import concourse.bass as bass
import concourse.mybir as mybir
from concourse.bass_utils import run_bass_kernel_spmd

F32 = mybir.dt.float32
BF16 = mybir.dt.bfloat16
AF = mybir.ActivationFunctionType
ALU = mybir.AluOpType
AX = mybir.AxisListType

D = 2048
SEQ = 8192
NB = 2048
TB = 512
NE = 64
DN_ALPHA = 2.0 ** 0.25
LN_EPS = 1e-5
GAMMA = [1.0 - 2.0 ** (-5.0 - h) for h in range(8)]
_LAST = []
N_EXP = int(os.environ.get("MK_NEXP", "65"))
_PH = os.environ.get("MK_PH", "0,1,1b,2").split(",")
_BLKS = [int(v) for v in os.environ.get("MK_BLKS", ",".join(str(i) for i in range(16))).split(",")]
_LH = int(os.environ.get("MK_LH", "16"))
_RH = int(os.environ.get("MK_RH", "8"))


def freeze(fn):
    if fn.__closure__ is None:
        return fn
    cells = []
    for c in fn.__closure__:
        try:
            cells.append(types.CellType(c.cell_contents))
        except ValueError:
            cells.append(c)
    return types.FunctionType(fn.__code__, fn.__globals__, fn.__name__, fn.__defaults__, tuple(cells))


class Src:
    def __init__(self, sem, step, name):
        self.sem, self.step, self.count, self.name = sem, step, 0, name


class Buf:
    __slots__ = ("w", "r", "name")

    def __init__(self, name=""):
        self.w, self.r, self.name = None, {}, name


class Sched:
    ENGS = ("pe", "act", "dve", "pool", "sp")

    def __init__(self, nc, stack):
        self.nc, self.stack = nc, stack
        self.streams = {e: [] for e in self.ENGS}
        self.src = {}
        for e in self.ENGS:
            self.src[e] = Src(stack.enter_context(nc.semaphore("s_" + e)), 1, e)
        self.seen = {e: {} for e in self.ENGS}
        self.dma_srcs = []
        self.n_inst = 0

    def dma_src(self, name):
        s = Src(self.stack.enter_context(self.nc.semaphore("d_" + name)), 16, name)
        self.dma_srcs.append(s)
        return s

    def _waits(self, eng, reads, writes):
        need = {}
        me = self.src[eng]

        def add(ev):
            if ev is None:
                return
            s, c = ev
            if s is me and eng == "pe":
                return
            if need.get(s, 0) < c:
                need[s] = c

        for b in reads:
            add(b.w)
        for b in writes:
            add(b.w)
            for s, c in b.r.items():
                if s is not me:
                    add((s, c))
        out = []
        seen = self.seen[eng]
        for s, c in need.items():
            if seen.get(s, 0) < c:
                seen[s] = c
                out.append((s, c))
        return out

    def op(self, eng, fn, reads=(), writes=()):
        self.group(eng, [fn], reads, writes)

    def group(self, eng, fns, reads=(), writes=()):
        waits = self._waits(eng, reads, writes)
        me = self.src[eng]
        me.count += 1
        cnt = me.count
        st = self.streams[eng]
        for i, fn in enumerate(fns):
            last = i == len(fns) - 1
            st.append((waits if i == 0 else (), freeze(fn), me if last else None, 1))
        for b in reads:
            b.r[me] = cnt
        for b in writes:
            b.w = (me, cnt)
            b.r = {}
        self.n_inst += len(fns)

    def dma(self, q, fn, dsrc, reads=(), writes=()):
        waits = self._waits(q, reads, writes)
        dsrc.count += 16
        cnt = dsrc.count
        self.streams[q].append((waits, freeze(fn), dsrc, 16))
        for b in reads:
            b.r[dsrc] = cnt
        for b in writes:
            b.w = (dsrc, cnt)
            b.r = {}
        self.n_inst += 1

    def barrier(self):
        allsrc = [self.src[e] for e in self.ENGS] + self.dma_srcs
        for e in self.ENGS:
            waits = []
            for s in allsrc:
                if s is self.src[e] or s.count == 0:
                    continue
                if self.seen[e].get(s, 0) < s.count:
                    self.seen[e][s] = s.count
                    waits.append((s, s.count))
            if waits:
                self.streams[e].append((waits, None, None, 0))

    def finish(self):
        nc = self.nc
        eh = {"pe": "tensor", "act": "scalar", "dve": "vector", "pool": "gpsimd", "sp": "sync"}
        self.barrier()
        with nc.Block() as block:
            for e in self.ENGS:
                stream = self.streams[e]

                def body(engine, stream=stream):
                    for waits, fn, src, inc in stream:
                        for s, c in waits:
                            engine.wait_ge(s.sem, c)
                        if fn is not None:
                            ins = fn(engine)
                            if src is not None:
                                ins.then_inc(src.sem, inc)

                getattr(block, eh[e])(body)


class Ring:
    def __init__(self, nc, st, name, n, shape, dt, psum=False):
        self.t, self.b, self.i = [], [], 0
        for k in range(n):
            nm = "%s%d" % (name, k)
            if psum:
                self.t.append(st.enter_context(nc.psum_tensor(nm, shape, dt)))
            else:
                self.t.append(st.enter_context(nc.sbuf_tensor(nm, shape, dt)))
            self.b.append(Buf(nm))

    def next(self):
        k = self.i % len(self.t)
        self.i += 1
        return self.t[k], self.b[k]


class DRing:
    def __init__(self, S, nc, st, name, n, shape, dt):
        self.r = Ring(nc, st, name, n, shape, dt)
        self.s = [S.dma_src("%s%d" % (name, k)) for k in range(n)]

    def next(self):
        k = self.r.i % len(self.s)
        t, b = self.r.next()
        return t, b, self.s[k]


def build():
    nc = bass.Bass("TRN2", target_bir_lowering=False)
    ein = lambda n, s, d=F32: nc.dram_tensor(n, list(s), d, kind="ExternalInput").ap()
    scr = lambda n, s, d=BF16: nc.dram_tensor(n, list(s), d).ap()
    xT = ein("xT", [16, 128, 16, TB])
    xres = ein("xres", [NB, D])
    w_in_l = ein("w_in_l", [16, 128, 2, 16, 128])
    w_in_r = ein("w_in_r", [8, 6, 128, 16, 256])
    w3 = ein("w3", [16, 128, 80, 128])
    wo = ein("wo", [4, 128, 16, 512])
    wge = ein("wge", [65, 128, 16, 512])
    wue = ein("wue", [65, 128, 16, 512])
    wde = ein("wde", [65, 128, 4, 2048])
    wr = ein("wr", [128, 16, 64])
    lrup = ein("lrup", [128, 16, 8])
    wai = ein("wai", [128, 16, 2, 128])
    bg = ein("bg", [128, 2, 16])
    cosT = ein("cosT", [16, 128, TB])
    sinT = ein("sinT", [16, 128, TB])
    flags = ein("flags", [128, 2, 16])
    dmaskT = ein("dmaskT", [128, 8, 128])
    xizeta = ein("xizeta", [128, 2, 8])
    gain_bc = ein("gain_bc", [128, 4096])
    lnp = ein("lnp", [128, 4, D])
    rb_bc = ein("rb_bc", [128, 64])
    ident = ein("ident", [128, 128])
    ustrict = ein("ustrict", [128, 2, 128])
    iota512 = ein("iota512", [128, 512])
    iota64 = ein("iota64", [128, 2, 64])
    tokcol = ein("tokcol", [128, 16, 2])
    out = nc.dram_tensor("out", [NB, D], F32, kind="ExternalOutput").ap()
    b_in_l = scr("b_in_l", [16, 128, 2, 16, 128])
    b_in_r = scr("b_in_r", [8, 6, 128, 16, 256])
    b_w3 = scr("b_w3", [16, 128, 80, 128])
    b_wo = scr("b_wo", [4, 128, 16, 512])
    b_wge = scr("b_wge", [1, 128, 16, 512])
    b_wue = scr("b_wue", [1, 128, 16, 512])
    b_wde = scr("b_wde", [1, 128, 4, 2048])
    _dbg = os.environ.get("MK_DBG", "0") == "1"
    dscr = (lambda n, s, d=BF16: nc.dram_tensor(n, list(s), d, kind="ExternalOutput").ap()) if _dbg else scr
    Hs = dscr("Hs", [NB, D], F32)
    UA = dscr("UA", [4, 128, 16, TB])
    Hb = scr("Hb", [NB + 128, D])
    YS = scr("YS", [NB, D], F32)
    Yx = scr("Yx", [64 * 512, D])
    UB = dscr("UB", [4, 128, 32, TB])

    with ExitStack() as top:
        S = Sched(nc, top)
        dr_w = Buf("dram_w")
        dr_H = Buf("dram_H")
        dr_out = Buf("dram_out")
        PS = Ring(nc, top, "ps", 6, [128, 512], F32, psum=True)
        PT = Ring(nc, top, "pt", 2, [128, 8, 128], BF16, psum=True)

        dout = S.dma_src("dout")

        with ExitStack() as st:
            stg = DRing(S, nc, st, "cst", 3, [128, 4096], F32)
            cvb = Ring(nc, st, "cvb", 3, [128, 4096], BF16)
            cvs = [S.dma_src("cvo%d" % k) for k in range(3)]
            cnt = [0]

            def convert(src2d, dst2d, ncols):
                for c0 in range(0, ncols, 4096):
                    cw = min(4096, ncols - c0)
                    t, tb, ts = stg.next()
                    S.dma("sp", lambda e, t=t, c0=c0, cw=cw: e.dma_start(out=t[:, 0:cw], in_=src2d[:, c0:c0 + cw]), ts, writes=[tb])
                    k = cnt[0] % 3
                    o, ob = cvb.next()
                    eng = ("pool", "act", "dve")[cnt[0] % 3]
                    cnt[0] += 1
                    if eng == "act":
                        S.op("act", lambda e, o=o, t=t, cw=cw: e.copy(out=o[:, 0:cw], in_=t[:, 0:cw]), reads=[tb], writes=[ob])
                    else:
                        S.op(eng, lambda e, o=o, t=t, cw=cw: e.tensor_copy(out=o[:, 0:cw], in_=t[:, 0:cw]), reads=[tb], writes=[ob])
                    S.dma("act" if k == 1 else "sp", lambda e, o=o, c0=c0, cw=cw: e.dma_start(out=dst2d[:, c0:c0 + cw], in_=o[:, 0:cw]), cvs[k], reads=[ob], writes=[])

            for g in range(16 if "0" in _PH else 0):
                convert(w_in_l[g].rearrange("p a k c -> p (a k c)"), b_in_l[g].rearrange("p a k c -> p (a k c)"), 4096)
            for h in range(8 if "0" in _PH else 0):
                for j in range(6):
                    convert(w_in_r[h, j].rearrange("p k c -> p (k c)"), b_in_r[h, j].rearrange("p k c -> p (k c)"), 4096)
            for g in range(16 if "0" in _PH else 0):
                convert(w3[g].rearrange("p k c -> p (k c)"), b_w3[g].rearrange("p k c -> p (k c)"), 10240)
            for g in range(4 if "0" in _PH else 0):
                convert(wo[g].rearrange("p k c -> p (k c)"), b_wo[g].rearrange("p k c -> p (k c)"), 8192)
            if "0" in _PH:
                convert(wge[64].rearrange("p k c -> p (k c)"), b_wge[0].rearrange("p k c -> p (k c)"), 8192)
                convert(wue[64].rearrange("p k c -> p (k c)"), b_wue[0].rearrange("p k c -> p (k c)"), 8192)
                convert(wde[64].rearrange("p k c -> p (k c)"), b_wde[0].rearrange("p k c -> p (k c)"), 8192)
            S.barrier()

        with ExitStack() as st:
            sb = lambda n, s, d=F32: st.enter_context(nc.sbuf_tensor(n, list(s), d))
            cq = S.dma_src("cq")
            cb_ = Buf("consts")
            lp = sb("lp", [128, 16, 8])
            wab = sb("wab", [128, 16, 2, 128], BF16)
            bgt = sb("bgt", [128, 2, 16])
            flg = sb("flg", [128, 2, 16])
            dmk = sb("dmk", [128, 8, 128])
            xz = sb("xz", [128, 2, 8])
            idf = sb("idf", [128, 128])
            idb = sb("idb", [128, 128], BF16)
            lrc = sb("lrc", [128, 2, 16])
            ltmp = sb("ltmp", [128, 16])
            hst = sb("hst", [128, 16])
            carry = sb("carry", [128, 16, 4])
            Sst = sb("Sst", [128, 8, 2, 512])
            for t_, a_ in ((lp, lrup), (bgt, bg), (flg, flags), (dmk, dmaskT), (xz, xizeta), (idf, ident)):
                S.dma("sp", lambda e, t_=t_, a_=a_: e.dma_start(out=t_[:], in_=a_), cq, writes=[cb_])
            S.op("dve", lambda e: e.tensor_copy(out=idb[:], in_=idf[:]), reads=[cb_], writes=[cb_])
            S.op("act", lambda e: e.activation(out=ltmp[:], in_=lp[:, :, 7], func=AF.Exp, scale=-1.0), reads=[cb_], writes=[cb_])
            S.op("act", lambda e: e.activation(out=ltmp[:], in_=ltmp[:], func=AF.Ln, bias=1.0), reads=[cb_], writes=[cb_])
            S.op("dve", lambda e: e.tensor_scalar(out=lrc[:, 0, :], in0=ltmp[:], scalar1=-8.0, scalar2=None, op0=ALU.mult), reads=[cb_], writes=[cb_])
            S.op("dve", lambda e: e.tensor_scalar(out=lrc[:, 1, :], in0=ltmp[:], scalar1=-16.0, scalar2=None, op0=ALU.mult), reads=[cb_], writes=[cb_])
            S.op("pool", lambda e: e.memset(hst[:], 0.0), writes=[cb_])
            S.op("pool", lambda e: e.memset(carry[:], 0.0), writes=[cb_])
            S.op("pool", lambda e: e.memset(Sst[:], 0.0), writes=[cb_])
            hst_b = [Buf("hst%d" % h) for h in range(16)]
            car_b = [Buf("car%d" % h) for h in range(16)]
            S_b = [Buf("S%d" % h) for h in range(8)]
            for bl in hst_b + car_b + S_b:
                bl.w = cb_.w

            xstg = DRing(S, nc, st, "xstg", 2, [128, 2, TB], F32)
            for hf in range(4):
                t, tb, ts = xstg.next()
                S.dma("sp", lambda e, t=t, hf=hf: e.dma_start(out=t[:].rearrange("p a b -> p (a b)"), in_=wai[:, hf * 4:(hf + 1) * 4].rearrange("p h a o -> p (h a o)")), ts, writes=[tb])
                S.op("dve", lambda e, t=t, hf=hf: e.tensor_copy(out=wab[:, hf * 4:(hf + 1) * 4].rearrange("p h a o -> p (h a o)"), in_=t[:].rearrange("p a b -> p (a b)")), reads=[tb], writes=[cb_])
            xTb = Ring(nc, st, "xTb", 1, [128, 16, TB], BF16)
            cs = DRing(S, nc, st, "cs", 1, [128, 2, TB], F32)
            wl = DRing(S, nc, st, "wl", 2, [128, 2, 16, 128], BF16)
            wq = DRing(S, nc, st, "wq", 5, [128, 16, 256], BF16)
            L = {n: Ring(nc, st, "l_" + n, 2, [128, TB], F32) for n in ("xa", "r", "i", "a", "m", "h", "gy")}
            Lxs = Ring(nc, st, "l_xs", 2, [128, TB + 4], F32)
            Lxab = Ring(nc, st, "l_xab", 2, [128, TB], BF16)
            Lin = Ring(nc, st, "l_in", 2, [128, 1], F32)
            Rt = Ring(nc, st, "r_t", 4, [128, TB], F32)
            Rq = Ring(nc, st, "r_q", 2, [128, 2, TB], BF16)
            Rk = Ring(nc, st, "r_k", 2, [128, 2, TB], BF16)
            Rkz = Ring(nc, st, "r_kz", 2, [128, 256], BF16)
            Rv = Ring(nc, st, "r_v", 2, [128, 512], BF16)
            Rs = Ring(nc, st, "r_s", 2, [128, 128], BF16)
            Ro = Ring(nc, st, "r_o", 2, [128, 512], F32)
            Rsg = Ring(nc, st, "r_sg", 1, [128, 512], F32)
            Rub = Ring(nc, st, "r_ub", 2, [128, 512], BF16)
            Rst = Ring(nc, st, "r_st", 2, [128, 8], F32)
            Sb = Ring(nc, st, "Sb", 2, [128, 2, 512], BF16)
            gn = DRing(S, nc, st, "gn", 1, [128, 512], F32)
            uar = Ring(nc, st, "uar", 2, [128, TB], BF16)
            ubr = Ring(nc, st, "ubr", 1, [128, 4, TB], BF16)
            uas = [S.dma_src("uas%d" % k) for k in range(2)]
            ubs = [S.dma_src("ubs%d" % k) for k in range(2)]
            uacnt = [0, 0]

            print("phase1 sbuf remaining", nc.sbuf_bytes_remaining, flush=True)

            def mmgroup(ps, psb, pairs, reads):
                n = len(pairs)
                S.group("pe", [
                    (lambda e, l=l, r=r, i=i: e.matmul(ps, lhsT=l, rhs=r, start=(i == 0), stop=(i == n - 1)))
                    for i, (l, r) in enumerate(pairs)], reads=reads, writes=[psb])

            for blk in (_BLKS if "1" in _PH else []):
                main = blk >= 12
                xb, xbb = xTb.next()
                for hf in range(8):
                    t, tb, ts = xstg.next()
                    S.dma("sp", lambda e, t=t, hf=hf, blk=blk: e.dma_start(out=t[:], in_=xT[blk, :, hf * 2:(hf + 1) * 2, :]), ts, writes=[tb])
                    S.op("pool", lambda e, t=t, xb=xb, hf=hf: e.tensor_copy(out=xb[:, hf * 2:(hf + 1) * 2, :], in_=t[:]), reads=[tb], writes=[xbb])
                ct, ctb, cts = cs.next()
                S.dma("act", lambda e, ct=ct, blk=blk: e.dma_start(out=ct[:, 0, :], in_=cosT[blk]), cts, writes=[ctb])
                S.dma("act", lambda e, ct=ct, blk=blk: e.dma_start(out=ct[:, 1, :], in_=sinT[blk]), cts, writes=[ctb])

                def lru_head(h, blk=blk, main=main, xb=xb, xbb=xbb):
                    w, wb_, ws = wl.next()
                    if main:
                        S.dma("sp", lambda e, w=w, h=h: e.dma_start(out=w[:], in_=b_in_l[h]), ws, reads=[dr_w], writes=[wb_])
                        yield
                    else:
                        S.dma("sp", lambda e, w=w, h=h: e.dma_start(out=w[:, 0], in_=b_in_l[h, :, 0]), ws, reads=[dr_w], writes=[wb_])
                        yield
                    px, pxb = PS.next()
                    mmgroup(px[:], pxb, [(w[:, 0, kc, :], xb[:, kc, :]) for kc in range(16)], [wb_, xbb])
                    yield
                    xs, xsb = Lxs.next()
                    S.op("act", lambda e, xs=xs, px=px: e.copy(out=xs[:, 4:TB + 4], in_=px[:]), reads=[pxb], writes=[xsb])
                    yield
                    S.op("pool", lambda e, xs=xs, h=h: e.tensor_copy(out=xs[:, 0:4], in_=carry[:, h, :]), reads=[car_b[h]], writes=[xsb])
                    yield
                    S.op("pool", lambda e, xs=xs, h=h: e.tensor_copy(out=carry[:, h, :], in_=xs[:, TB:TB + 4]), reads=[xsb], writes=[car_b[h]])
                    yield
                    xa, xab_ = L["xa"].next()
                    S.op("dve", lambda e, xa=xa, xs=xs, h=h: e.tensor_scalar(out=xa[:], in0=xs[:, 4:TB + 4], scalar1=lp[:, h, 3:4], scalar2=lp[:, h, 4:5], op0=ALU.mult, op1=ALU.add), reads=[xsb], writes=[xab_])
                    yield
                    for j in range(3):
                        S.op("dve", lambda e, xa=xa, xs=xs, h=h, j=j: e.scalar_tensor_tensor(out=xa[:], in0=xs[:, 1 + j:TB + 1 + j], scalar=lp[:, h, j:j + 1], in1=xa[:], op0=ALU.mult, op1=ALU.add), reads=[xsb, xab_], writes=[xab_])
                        yield
                    xq, xqb = Lxab.next()
                    S.op("act", lambda e, xq=xq, xa=xa: e.copy(out=xq[:], in_=xa[:]), reads=[xab_], writes=[xqb])
                    yield
                    pr, prb = PS.next()
                    S.op("pe", lambda e, pr=pr, xq=xq, h=h: e.matmul(pr[:], lhsT=wab[:, h, 0, :], rhs=xq[:], start=True, stop=True), reads=[xqb], writes=[prb])
                    yield
                    pi, pib = PS.next()
                    S.op("pe", lambda e, pi=pi, xq=xq, h=h: e.matmul(pi[:], lhsT=wab[:, h, 1, :], rhs=xq[:], start=True, stop=True), reads=[xqb], writes=[pib])
                    yield
                    r, rb = L["r"].next()
                    S.op("act", lambda e, r=r, pr=pr, h=h: e.activation(out=r[:], in_=pr[:], func=AF.Sigmoid, bias=lp[:, h, 5:6]), reads=[prb], writes=[rb])
                    yield
                    ig, igb = L["i"].next()
                    S.op("act", lambda e, ig=ig, pi=pi, h=h: e.activation(out=ig[:], in_=pi[:], func=AF.Sigmoid, bias=lp[:, h, 6:7]), reads=[pib], writes=[igb])
                    yield
                    a, ab = L["a"].next()
                    S.op("act", lambda e, a=a, r=r, h=h: e.activation(out=a[:], in_=r[:], func=AF.Exp, scale=lrc[:, 0, h:h + 1]), reads=[rb], writes=[ab])
                    yield
                    m, mb = L["m"].next()
                    S.op("act", lambda e, m=m, r=r, h=h: e.activation(out=m[:], in_=r[:], func=AF.Exp, scale=lrc[:, 1, h:h + 1]), reads=[rb], writes=[mb])
                    yield
                    S.op("act", lambda e, m=m: e.activation(out=m[:], in_=m[:], func=AF.Sqrt, scale=-1.0, bias=1.0), reads=[mb], writes=[mb])
                    yield
                    S.op("dve", lambda e, m=m, blk=blk: e.tensor_scalar(out=m[:, 0:1], in0=m[:, 0:1], scalar1=flg[:, 1, blk:blk + 1], scalar2=flg[:, 0, blk:blk + 1], op0=ALU.mult, op1=ALU.add), reads=[mb], writes=[mb])
                    yield
                    S.op("dve", lambda e, m=m, ig=ig: e.tensor_tensor(out=m[:], in0=m[:], in1=ig[:], op=ALU.mult), reads=[mb, igb], writes=[mb])
                    yield
                    S.op("dve", lambda e, m=m, xa=xa: e.tensor_tensor(out=m[:], in0=m[:], in1=xa[:], op=ALU.mult), reads=[mb, xab_], writes=[mb])
                    yield
                    ini, inib = Lin.next()
                    S.op("dve", lambda e, ini=ini, h=h, blk=blk: e.tensor_tensor(out=ini[:], in0=hst[:, h:h + 1], in1=flg[:, 1, blk:blk + 1], op=ALU.mult), reads=[hst_b[h]], writes=[inib])
                    yield
                    hh, hb = L["h"].next()
                    S.op("dve", lambda e, hh=hh, a=a, m=m, ini=ini: e.tensor_tensor_scan(out=hh[:], data0=a[:], data1=m[:], initial=ini[:], op0=ALU.mult, op1=ALU.add), reads=[ab, mb, inib], writes=[hb])
                    yield
                    S.op("pool", lambda e, hh=hh, h=h: e.tensor_copy(out=hst[:, h:h + 1], in_=hh[:, TB - 1:TB]), reads=[hb], writes=[hst_b[h]])
                    yield
                    if main:
                        py, pyb = PS.next()
                        mmgroup(py[:], pyb, [(w[:, 1, kc, :], xb[:, kc, :]) for kc in range(16)], [wb_, xbb])
                        yield
                        gy, gyb = L["gy"].next()
                        S.op("act", lambda e, gy=gy, py=py: e.activation(out=gy[:], in_=py[:], func=AF.Gelu_apprx_tanh), reads=[pyb], writes=[gyb])
                        yield
                        k_ = uacnt[0] % 2
                        uacnt[0] += 1
                        ut, utb = uar.next()
                        S.op("dve", lambda e, gy=gy, hh=hh, ut=ut: e.tensor_tensor(out=ut[:], in0=hh[:], in1=gy[:], op=ALU.mult), reads=[hb, gyb], writes=[utb])
                        yield
                        S.dma("act", lambda e, ut=ut, h=h, blk=blk: e.dma_start(out=UA[blk - 12, :, h, :], in_=ut[:]), uas[k_], reads=[utb], writes=[])
                        yield

                for h0 in range(0, _LH, 2):
                    gens = [lru_head(h) for h in range(h0, min(h0 + 2, _LH))]
                    while gens:
                        for g_ in list(gens):
                            try:
                                next(g_)
                            except StopIteration:
                                gens.remove(g_)

                for h in range(_RH):
                    gc = GAMMA[h] ** 128
                    wts = {}
                    for j, nm in enumerate(("q", "k", "v0", "v1", "g0", "g1")):
                        if not main and nm in ("q", "g0", "g1"):
                            continue
                        w, wb_, ws = wq.next() if nm in ("q", "k") else (None, None, None)
                        if nm in ("q", "k"):
                            S.dma("sp", lambda e, w=w, h=h, j=j: e.dma_start(out=w[:], in_=b_in_r[h, j]), ws, reads=[dr_w], writes=[wb_])
                            wts[nm] = (w, wb_)
                    def proj_rope(nm, ring):
                        w, wb_ = wts[nm]
                        p0, p0b = PS.next()
                        mmgroup(p0[:], p0b, [(w[:, kc, 0:128], xb[:, kc, :]) for kc in range(16)], [wb_, xbb])
                        p1, p1b = PS.next()
                        mmgroup(p1[:], p1b, [(w[:, kc, 128:256], xb[:, kc, :]) for kc in range(16)], [wb_, xbb])
                        o, ob = ring.next()
                        t1, t1b = Rt.next()
                        t2, t2b = Rt.next()
                        S.op("dve", lambda e: e.tensor_tensor(out=t1[:], in0=p0[:], in1=ct[:, 0, :], op=ALU.mult), reads=[p0b, ctb], writes=[t1b])
                        S.op("dve", lambda e: e.tensor_tensor(out=t2[:], in0=p1[:], in1=ct[:, 1, :], op=ALU.mult), reads=[p1b, ctb], writes=[t2b])
                        S.op("pool", lambda e: e.tensor_tensor(out=o[:, 0, :], in0=t1[:], in1=t2[:], op=ALU.subtract), reads=[t1b, t2b], writes=[ob])
                        t3, t3b = Rt.next()
                        t4, t4b = Rt.next()
                        S.op("dve", lambda e: e.tensor_tensor(out=t3[:], in0=p0[:], in1=ct[:, 1, :], op=ALU.mult), reads=[p0b, ctb], writes=[t3b])
                        S.op("dve", lambda e: e.tensor_tensor(out=t4[:], in0=p1[:], in1=ct[:, 0, :], op=ALU.mult), reads=[p1b, ctb], writes=[t4b])
                        S.op("pool", lambda e: e.tensor_tensor(out=o[:, 1, :], in0=t3[:], in1=t4[:], op=ALU.add), reads=[t3b, t4b], writes=[ob])
                        return o, ob

                    kr, krb = proj_rope("k", Rk)
                    if main:
                        qr, qrb = proj_rope("q", Rq)
                    wv = []
                    for j in (2, 3):
                        w, wb_, ws = wq.next()
                        S.dma("sp", lambda e, w=w, h=h, j=j: e.dma_start(out=w[:], in_=b_in_r[h, j]), ws, reads=[dr_w], writes=[wb_])
                        wv.append((w, wb_))
                    wg_ = []
                    if main:
                        gt, gtb, gts = gn.next()
                        S.dma("act", lambda e, gt=gt, h=h: e.dma_start(out=gt[:], in_=gain_bc[:, h * 512:(h + 1) * 512]), gts, writes=[gtb])
                    if main:
                        k_ = uacnt[1] % 2
                        uacnt[1] += 1
                        ubt, ubtb = ubr.next()
                    def chunk(c):
                        tok = slice(c * 128, (c + 1) * 128)
                        pv, pvb = PS.next()
                        for hf in range(2):
                            n = 16
                            S.group("pe", [
                                (lambda e, kc=kc, hf=hf: e.matmul(pv[:, hf * 256:(hf + 1) * 256], lhsT=xb[:, kc, tok], rhs=wv[hf][0][:, kc, :], start=(kc == 0), stop=(kc == 15)))
                                for kc in range(16)], reads=[xbb, wv[hf][1]], writes=[pvb])
                            yield
                        vb, vbb = Rv.next()
                        S.op("act", lambda e, vb=vb, pv=pv: e.copy(out=vb[:], in_=pv[:]), reads=[pvb], writes=[vbb])
                        yield
                        kz, kzb = Rkz.next()
                        pt, ptb_ = PT.next()
                        S.group("pe", [(lambda e, pt=pt, j=j: e.transpose(pt[:, j, :], kr[:, j, tok], idb[:])) for j in range(2)], reads=[krb], writes=[ptb_])
                        yield
                        S.op("dve", lambda e, pt=pt, kz=kz: e.tensor_scalar(out=kz[:].rearrange("p (a b) -> p a b", b=128), in0=pt[:, 0:2, :], scalar1=xz[:, 1, h:h + 1], scalar2=None, op0=ALU.mult), reads=[ptb_], writes=[kzb])
                        yield
                        if main:
                            psc, pscb = PS.next()
                            mmgroup(psc[:, 0:128], pscb, [(kr[:, j, tok], qr[:, j, tok]) for j in range(2)], [krb, qrb])
                            yield
                            sm, smb = Rs.next()
                            S.op("dve", lambda e, sm=sm, psc=psc: e.tensor_tensor(out=sm[:], in0=psc[:, 0:128], in1=dmk[:, h, :], op=ALU.mult), reads=[pscb], writes=[smb])
                            yield
                            po, pob = PS.next()
                            S.op("pe", lambda e, po=po, sm=sm, vb=vb: e.matmul(po[:], lhsT=sm[:], rhs=vb[:], start=True, stop=True), reads=[smb, vbb], writes=[pob])
                            yield
                            sbf, sbfb = Sb.next()
                            S.op("pool", lambda e, sbf=sbf: e.tensor_copy(out=sbf[:], in_=Sst[:, h]), reads=[S_b[h]], writes=[sbfb])
                            yield
                            pc, pcb = PS.next()
                            mmgroup(pc[:], pcb, [(qr[:, j, tok], sbf[:, j, :]) for j in range(2)], [qrb, sbfb])
                            yield
                            o1, o1b = Ro.next()
                            S.op("act", lambda e, o1=o1, po=po: e.copy(out=o1[:], in_=po[:]), reads=[pob], writes=[o1b])
                            yield
                            o2, o2b = Ro.next()
                            S.op("dve", lambda e, o2=o2, pc=pc, o1=o1: e.scalar_tensor_tensor(out=o2[:], in0=pc[:], scalar=xz[:, 0, h:h + 1], in1=o1[:], op0=ALU.mult, op1=ALU.add), reads=[pcb, o1b], writes=[o2b])
                            yield
                            stt, sttb = Rst.next()
                            S.op("dve", lambda e, stt=stt, o2=o2: e.bn_stats(out=stt[:, 0:6], in_=o2[:]), reads=[o2b], writes=[sttb])
                            yield
                            S.op("dve", lambda e, stt=stt: e.bn_aggr(out=stt[:, 6:8], in_=stt[:, 0:6]), reads=[sttb], writes=[sttb])
                            yield
                            S.op("dve", lambda e, stt=stt: e.tensor_scalar(out=stt[:, 7:8], in0=stt[:, 7:8], scalar1=LN_EPS, scalar2=None, op0=ALU.add), reads=[sttb], writes=[sttb])
                            yield
                            S.op("act", lambda e, stt=stt: e.activation(out=stt[:, 7:8], in_=stt[:, 7:8], func=AF.Sqrt), reads=[sttb], writes=[sttb])
                            yield
                            S.op("dve", lambda e, stt=stt: e.reciprocal(out=stt[:, 7:8], in_=stt[:, 7:8]), reads=[sttb], writes=[sttb])
                            yield
                            S.op("dve", lambda e, stt=stt, o2=o2: e.tensor_scalar(out=o2[:], in0=o2[:], scalar1=stt[:, 6:7], scalar2=stt[:, 7:8], op0=ALU.subtract, op1=ALU.mult), reads=[sttb, o2b], writes=[o2b])
                            yield
                            S.op("pool", lambda e, o2=o2, gt=gt: e.tensor_tensor(out=o2[:], in0=o2[:], in1=gt[:], op=ALU.mult), reads=[o2b, gtb], writes=[o2b])
                            yield
                            if c == 0:
                                for j in (4, 5):
                                    w, wb_, ws = wq.next()
                                    S.dma("sp", lambda e, w=w, h=h, j=j: e.dma_start(out=w[:], in_=b_in_r[h, j]), ws, reads=[dr_w], writes=[wb_])
                                    yield
                                    wg_.append((w, wb_))
                            pg, pgb = PS.next()
                            for hf in range(2):
                                S.group("pe", [
                                    (lambda e, kc=kc, hf=hf: e.matmul(pg[:, hf * 256:(hf + 1) * 256], lhsT=xb[:, kc, tok], rhs=wg_[hf][0][:, kc, :], start=(kc == 0), stop=(kc == 15)))
                                    for kc in range(16)], reads=[xbb, wg_[hf][1]], writes=[pgb])
                                yield
                            sg, sgb = Rsg.next()
                            S.op("act", lambda e, sg=sg, pg=pg: e.activation(out=sg[:], in_=pg[:], func=AF.Silu), reads=[pgb], writes=[sgb])
                            yield
                            ub, ubb = Rub.next()
                            S.op("dve", lambda e, ub=ub, o2=o2, sg=sg: e.tensor_tensor(out=ub[:], in0=o2[:], in1=sg[:], op=ALU.mult), reads=[o2b, sgb], writes=[ubb])
                            yield
                            pt, ptb_ = PT.next()
                            S.group("pe", [(lambda e, pt=pt, ub=ub, q4=q4: e.transpose(pt[:, q4, :], ub[:, q4 * 128:(q4 + 1) * 128], idb[:])) for q4 in range(4)], reads=[ubb], writes=[ptb_])
                            yield
                            S.op("act", lambda e, pt=pt: e.copy(out=ubt[:, :, tok], in_=pt[:, 0:4, :]), reads=[ptb_], writes=[ubtb])
                            yield
                        for j in range(2):
                            pd, pdb = PS.next()
                            S.op("pe", lambda e, pd=pd, kz=kz, vb=vb, j=j: e.matmul(pd[:], lhsT=kz[:, j * 128:(j + 1) * 128], rhs=vb[:], start=True, stop=True), reads=[kzb, vbb], writes=[pdb])
                            yield
                            S.op("dve", lambda e, pd=pd, j=j: e.scalar_tensor_tensor(out=Sst[:, h, j, :], in0=Sst[:, h, j, :], scalar=gc, in1=pd[:], op0=ALU.mult, op1=ALU.add), reads=[pdb, S_b[h]], writes=[S_b[h]])
                            yield

                    if main:
                        for c in range(4):
                            for _ in chunk(c):
                                pass
                    else:
                        for c0 in (0, 2):
                            gens = [chunk(c0), chunk(c0 + 1)]
                            while gens:
                                for g_ in list(gens):
                                    try:
                                        next(g_)
                                    except StopIteration:
                                        gens.remove(g_)
                    if main:
                        S.dma("act", lambda e, ubt=ubt, h=h, blk=blk: e.dma_start(out=UB[blk - 12, :, h * 4:(h + 1) * 4, :], in_=ubt[:]), ubs[k_], reads=[ubtb], writes=[])
            S.barrier()

        with ExitStack() as st:
            sb = lambda n, s, d=F32: st.enter_context(nc.sbuf_tensor(n, list(s), d))
            cq = S.dma_src("cq1b")
            bgt = sb("bgt2", [128, 2, 16])
            cb_ = Buf("consts1b")
            S.dma("sp", lambda e: e.dma_start(out=bgt[:], in_=bg), cq, writes=[cb_])
            xstg = DRing(S, nc, st, "xstg2", 2, [128, 2, TB], F32)
            xTb = Ring(nc, st, "xTb2", 1, [128, 16, TB], BF16)
            uaT = sb("uaT", [128, 16, TB], BF16)
            ubT = sb("ubT", [128, 32, TB], BF16)
            mxT = sb("mxT", [128, 16, TB], BF16)
            ua_b, ub_b, mx_b = Buf("uaT"), Buf("ubT"), Buf("mxT")
            uq = S.dma_src("uq")
            uq2 = S.dma_src("uq2")
            w3r = DRing(S, nc, st, "w3r", 5, [128, 20, 128], BF16)
            wor = DRing(S, nc, st, "wor", 3, [128, 8, 512], BF16)
            xrr = DRing(S, nc, st, "xrr", 1, [128, 4, D], F32)
            lnt = sb("lnt", [128, 2, D])
            lnb = cb_
            S.dma("act", lambda e: e.dma_start(out=lnt[:], in_=lnp[:, 0:2, :]), cq, writes=[lnb])
            G1 = Ring(nc, st, "g1", 1, [128, TB], F32)
            G2 = Ring(nc, st, "g2", 1, [128, TB], F32)
            Zst = Ring(nc, st, "zst", 2, [128, 32], F32)
            hsrc = S.dma_src("hs")

            def mmgroup(ps, psb, pairs, reads):
                n = len(pairs)
                S.group("pe", [
                    (lambda e, l=l, r=r, i=i: e.matmul(ps, lhsT=l, rhs=r, start=(i == 0), stop=(i == n - 1)))
                    for i, (l, r) in enumerate(pairs)], reads=reads, writes=[psb])

            for bi in range(4 if "1b" in _PH else 0):
                xb, xbb = xTb.next()
                for hf in range(8):
                    t, tb, ts = xstg.next()
                    S.dma("sp", lambda e, t=t, hf=hf, bi=bi: e.dma_start(out=t[:], in_=xT[12 + bi, :, hf * 2:(hf + 1) * 2, :]), ts, writes=[tb])
                    S.op("pool", lambda e, t=t, xb=xb, hf=hf: e.tensor_copy(out=xb[:, hf * 2:(hf + 1) * 2, :], in_=t[:]), reads=[tb], writes=[xbb])
                S.dma("act", lambda e, bi=bi: e.dma_start(out=uaT[:], in_=UA[bi]), uq, writes=[ua_b])
                S.dma("act", lambda e, bi=bi: e.dma_start(out=ubT[:], in_=UB[bi]), uq2, writes=[ub_b])
                for nt in range(16):
                    pcs = []
                    for q_ in range(4):
                        w, wb_, ws = w3r.next()
                        S.dma("sp", lambda e, w=w, nt=nt, q_=q_: e.dma_start(out=w[:], in_=b_w3[nt, :, q_ * 20:(q_ + 1) * 20, :]), ws, writes=[wb_])
                        pcs.append((w, wb_))
                    wsl = lambda kc: pcs[kc // 20][0][:, kc % 20, :]
                    wbs = lambda lo, hi: [pcs[q_][1] for q_ in range(lo // 20, (hi - 1) // 20 + 1)]
                    pa, pab = PS.next()
                    mmgroup(pa[:], pab, [(wsl(kc), uaT[:, kc, :]) for kc in range(16)], wbs(0, 16) + [ua_b])
                    pb, pbb = PS.next()
                    mmgroup(pb[:], pbb, [(wsl(16 + kc), ubT[:, kc, :]) for kc in range(32)], wbs(16, 48) + [ub_b])
                    pga, pgab = PS.next()
                    mmgroup(pga[:], pgab, [(wsl(48 + kc), xb[:, kc, :]) for kc in range(16)], wbs(48, 64) + [xbb])
                    pgb_, pgbb = PS.next()
                    mmgroup(pgb_[:], pgbb, [(wsl(64 + kc), xb[:, kc, :]) for kc in range(16)], wbs(64, 80) + [xbb])
                    s1, s1b = G1.next()
                    S.op("act", lambda e, s1=s1, pga=pga, nt=nt: e.activation(out=s1[:], in_=pga[:], func=AF.Sigmoid, bias=bgt[:, 0, nt:nt + 1]), reads=[pgab], writes=[s1b])
                    s2, s2b = G2.next()
                    S.op("act", lambda e, s2=s2, pgb_=pgb_, nt=nt: e.activation(out=s2[:], in_=pgb_[:], func=AF.Sigmoid, bias=bgt[:, 1, nt:nt + 1]), reads=[pgbb], writes=[s2b])
                    S.op("dve", lambda e, s1=s1, pa=pa: e.tensor_tensor(out=s1[:], in0=s1[:], in1=pa[:], op=ALU.mult), reads=[s1b, pab], writes=[s1b])
                    S.op("dve", lambda e, s2=s2, pb=pb: e.tensor_tensor(out=s2[:], in0=s2[:], in1=pb[:], op=ALU.mult), reads=[s2b, pbb], writes=[s2b])
                    S.op("pool", lambda e, s1=s1, s2=s2, nt=nt: e.tensor_tensor(out=mxT[:, nt, :], in0=s1[:], in1=s2[:], op=ALU.add), reads=[s1b, s2b], writes=[mx_b])
                xr, xrb, xrs = xrr.next()
                S.dma("act", lambda e, xr=xr, bi=bi: e.dma_start(out=xr[:], in_=xres[bi * TB:(bi + 1) * TB, :].rearrange("(t p) d -> p t d", p=128)), xrs, reads=[dr_H], writes=[xrb])
                for nb in range(4):
                    pcs = []
                    for q_ in range(2):
                        w, wb_, ws = wor.next()
                        S.dma("sp", lambda e, w=w, nb=nb, q_=q_: e.dma_start(out=w[:], in_=b_wo[nb, :, q_ * 8:(q_ + 1) * 8, :]), ws, writes=[wb_])
                        pcs.append((w, wb_))
                    for tt in range(4):
                        pz, pzb = PS.next()
                        mmgroup(pz[:], pzb, [(mxT[:, kc, tt * 128:(tt + 1) * 128], pcs[kc // 8][0][:, kc % 8, :]) for kc in range(16)], [pcs[0][1], pcs[1][1], mx_b])
                        S.op("dve", lambda e, xr=xr, pz=pz, tt=tt, nb=nb: e.scalar_tensor_tensor(out=xr[:, tt, nb * 512:(nb + 1) * 512], in0=xr[:, tt, nb * 512:(nb + 1) * 512], scalar=DN_ALPHA, in1=pz[:], op0=ALU.mult, op1=ALU.add), reads=[pzb, xrb], writes=[xrb])
                for tt in range(4):
                    zs, zsb = Zst.next()
                    for q4 in range(4):
                        S.op("dve", lambda e, zs=zs, xr=xr, tt=tt, q4=q4: e.bn_stats(out=zs[:, q4 * 6:(q4 + 1) * 6], in_=xr[:, tt, q4 * 512:(q4 + 1) * 512]), reads=[xrb], writes=[zsb])
                    S.op("dve", lambda e, zs=zs: e.bn_aggr(out=zs[:, 24:26], in_=zs[:, 0:24]), reads=[zsb], writes=[zsb])
                    S.op("dve", lambda e, zs=zs: e.tensor_scalar(out=zs[:, 25:26], in0=zs[:, 25:26], scalar1=LN_EPS, scalar2=None, op0=ALU.add), reads=[zsb], writes=[zsb])
                    S.op("act", lambda e, zs=zs: e.activation(out=zs[:, 25:26], in_=zs[:, 25:26], func=AF.Sqrt), reads=[zsb], writes=[zsb])
                    S.op("dve", lambda e, zs=zs: e.reciprocal(out=zs[:, 25:26], in_=zs[:, 25:26]), reads=[zsb], writes=[zsb])
                    S.op("dve", lambda e, zs=zs, xr=xr, tt=tt: e.tensor_scalar(out=xr[:, tt, :], in0=xr[:, tt, :], scalar1=zs[:, 24:25], scalar2=zs[:, 25:26], op0=ALU.subtract, op1=ALU.mult), reads=[zsb, xrb], writes=[xrb])
                    S.op("pool", lambda e, xr=xr, tt=tt: e.tensor_tensor(out=xr[:, tt, :], in0=xr[:, tt, :], in1=lnt[:, 0, :], op=ALU.mult), reads=[xrb, lnb], writes=[xrb])
                    S.op("pool", lambda e, xr=xr, tt=tt: e.tensor_tensor(out=xr[:, tt, :], in0=xr[:, tt, :], in1=lnt[:, 1, :], op=ALU.add), reads=[xrb, lnb], writes=[xrb])
                S.dma("sp", lambda e, xr=xr, bi=bi: e.dma_start(out=Hs[bi * TB:(bi + 1) * TB, :].rearrange("(t p) d -> p t d", p=128), in_=xr[:]), hsrc, reads=[xrb], writes=[dr_H])
            S.barrier()

        CAP = 512
        with ExitStack() as st2:
            sbp = lambda n, s, d=F32: st2.enter_context(nc.sbuf_tensor(n, list(s), d))
            cq2 = S.dma_src("cq2")
            c2b = Buf("consts2")
            idf2 = sbp("idf2", [128, 128])
            idb2 = sbp("idb2", [128, 128], BF16)
            ust = sbp("ust", [128, 2, 128])
            io512 = sbp("io512", [128, 512])
            io64 = sbp("io64", [128, 2, 64])
            tokc = sbp("tokc", [128, 16, 2])
            wall = sbp("wall", [128, 16, 64])
            selall = sbp("selall", [128, 16, 64])
            posall = sbp("posall", [128, 16, 64])
            eidf = sbp("eidf", [128, 16, 8])
            carry2 = sbp("carry2", [128, 64])
            rt_b = Buf("routing")
            for t_, a_ in ((idf2, ident), (ust, ustrict), (io512, iota512), (io64, iota64), (tokc, tokcol)):
                S.dma("sp", lambda e, t_=t_, a_=a_: e.dma_start(out=t_[:], in_=a_), cq2, writes=[c2b])
            S.op("dve", lambda e: e.tensor_copy(out=idb2[:], in_=idf2[:]), reads=[c2b], writes=[c2b])
            S.op("pool", lambda e: e.memset(carry2[:], 0.0), writes=[rt_b])

            def mmgroup(ps, psb, pairs, reads):
                n = len(pairs)
                S.group("pe", [
                    (lambda e, l=l, r=r, i=i: e.matmul(ps, lhsT=l, rhs=r, start=(i == 0), stop=(i == n - 1)))
                    for i, (l, r) in enumerate(pairs)], reads=reads, writes=[psb])

            def ffn(xT_, xTb_, wg, wgb, wu, wub, wd, wdb, h1, h1b, sgr, emit_y):
                for ft in range(4):
                    pg, pgb = PS.next()
                    mmgroup(pg[:], pgb, [(wg[:, kc, ft * 128:(ft + 1) * 128], xT_[:, kc, :]) for kc in range(16)], [wgb, xTb_])
                    pu, pub = PS.next()
                    mmgroup(pu[:], pub, [(wu[:, kc, ft * 128:(ft + 1) * 128], xT_[:, kc, :]) for kc in range(16)], [wub, xTb_])
                    sg, sgb = sgr.next()
                    S.op("act", lambda e, sg=sg, pg=pg: e.activation(out=sg[:], in_=pg[:], func=AF.Silu), reads=[pgb], writes=[sgb])
                    S.op("dve", lambda e, h1=h1, sg=sg, pu=pu, ft=ft: e.tensor_tensor(out=h1[:, ft, :], in0=sg[:], in1=pu[:], op=ALU.mult), reads=[sgb, pub], writes=[h1b])
                for tt in range(4):
                    for nb in range(4):
                        py, pyb = PS.next()
                        mmgroup(py[:], pyb, [(h1[:, ft, tt * 128:(tt + 1) * 128], wd[:, ft, nb * 512:(nb + 1) * 512]) for ft in range(4)], [h1b, wdb])
                        emit_y(tt, nb, py, pyb)

            with ExitStack() as st:
                sb = lambda n, s, d=F32: st.enter_context(nc.sbuf_tensor(n, list(s), d))
                wrt = sb("wrt", [128, 16, 64])
                rbt = sb("rbt", [128, 64])
                cq2b = S.dma_src("cq2b")
                cq2c = S.dma_src("cq2c")
                swq = S.dma_src("swq")
                r2b = Buf("rconsts")
                for t_, a_ in ((wrt, wr), (rbt, rb_bc)):
                    S.dma("sp", lambda e, t_=t_, a_=a_: e.dma_start(out=t_[:], in_=a_), cq2b, writes=[r2b])
                zr = sb("zr", [128, D], BF16)
                zrb = Buf("zr")
                S.op("pool", lambda e: e.memset(zr[:], 0.0), writes=[zrb])
                S.dma("sp", lambda e: e.dma_start(out=Hb[NB:NB + 128, :], in_=zr[:]), cq2c, reads=[zrb], writes=[])
                hT = Ring(nc, st, "hT32", 1, [128, 16, 128], F32)
                hld = DRing(S, nc, st, "hld", 1, [128, 4, D], F32)
                hbr = Ring(nc, st, "hbr", 2, [128, D], BF16)
                hbs = [S.dma_src("hbs%d" % k) for k in range(2)]
                R1 = Ring(nc, st, "rt1", 2, [128, 64], F32)
                R2 = Ring(nc, st, "rt2", 2, [128, 64], F32)
                R3 = Ring(nc, st, "rt3", 2, [128, 64], F32)
                R8 = Ring(nc, st, "rt8", 2, [128, 4, 8], F32)
                RI = Ring(nc, st, "rti", 2, [128, 8], mybir.dt.uint32)
                hTb = sb("hTb", [128, 16, TB], BF16)
                hTb_b = Buf("hTb")
                h1T = Ring(nc, st, "h1T", 1, [128, 4, TB], BF16)
                Rsg2 = Ring(nc, st, "sg2", 2, [128, TB], F32)
                swg = sb("swg", [128, 16, 512], BF16)
                swu = sb("swu", [128, 16, 512], BF16)
                swd = sb("swd", [128, 4, D], BF16)
                swb = Buf("sw")
                S.dma("sp", lambda e: e.dma_start(out=swg[:], in_=b_wge[0]), swq, writes=[swb])
                S.dma("act", lambda e: e.dma_start(out=swu[:], in_=b_wue[0]), swq, writes=[swb])
                S.dma("sp", lambda e: e.dma_start(out=swd[:], in_=b_wde[0]), swq, writes=[swb])
                ysr = Ring(nc, st, "ysr", 2, [128, D], F32)
                yss = [S.dma_src("yss%d" % k) for k in range(2)]
                hcnt = [0, 0]
                for bi in range(4 if "2" in _PH else 0):
                    hx, hxb, hxs = hld.next()
                    S.dma("sp", lambda e, hx=hx, bi=bi: e.dma_start(out=hx[:], in_=Hs[bi * TB:(bi + 1) * TB, :].rearrange("(t p) d -> p t d", p=128)), hxs, writes=[hxb])
                    for tt in range(4):
                        tg = bi * 4 + tt
                        k_ = hcnt[0] % 2
                        hcnt[0] += 1
                        hb_, hbb = hbr.next()
                        S.op("pool", lambda e, hb_=hb_, hx=hx, tt=tt: e.tensor_copy(out=hb_[:], in_=hx[:, tt, :]), reads=[hxb], writes=[hbb])
                        S.dma("act", lambda e, hb_=hb_, tg=tg: e.dma_start(out=Hb[tg * 128:(tg + 1) * 128, :], in_=hb_[:]), hbs[k_], reads=[hbb], writes=[])
                        h32, h32b = hT.next()
                        for k4 in range(4):
                            pp, ppb = PS.next()
                            S.group("pe", [(lambda e, pp=pp, hx=hx, tt=tt, kc=k4 * 4 + q4, q4=q4: e.transpose(pp[:, q4 * 128:(q4 + 1) * 128], hx[:, tt, kc * 128:(kc + 1) * 128], idf2[:])) for q4 in range(4)], reads=[hxb, c2b], writes=[ppb])
                            S.op("act", lambda e, pp=pp, h32=h32, k4=k4: e.copy(out=h32[:, k4 * 4:(k4 + 1) * 4, :], in_=pp[:].rearrange("p (a b) -> p a b", b=128)), reads=[ppb], writes=[h32b])
                            S.op("pool", lambda e, h32=h32, k4=k4, tt=tt: e.tensor_copy(out=hTb[:, k4 * 4:(k4 + 1) * 4, tt * 128:(tt + 1) * 128], in_=h32[:, k4 * 4:(k4 + 1) * 4, :]), reads=[h32b], writes=[hTb_b])
                        pl, plb = PS.next()
                        mmgroup(pl[:, 0:64], plb, [(h32[:, kc, :], wrt[:, kc, :]) for kc in range(16)], [h32b, r2b])
                        sc, scb = R1.next()
                        S.op("act", lambda e, sc=sc, pl=pl: e.activation(out=sc[:], in_=pl[:, 0:64], func=AF.Sigmoid), reads=[plb], writes=[scb])
                        bs, bsb = R2.next()
                        S.op("dve", lambda e, bs=bs, sc=sc: e.tensor_tensor(out=bs[:], in0=sc[:], in1=rbt[:], op=ALU.add), reads=[scb, r2b], writes=[bsb])
                        t8, t8b = R3.next()
                        for g in range(8):
                            S.op("dve", lambda e, t8=t8, bs=bs, g=g: e.max(out=t8[:, g * 8:(g + 1) * 8], in_=bs[:, g * 8:(g + 1) * 8]), reads=[bsb], writes=[t8b])
                        sm8, sm8b = R8.next()
                        t8v = t8[:].rearrange("p (g k) -> p g k", k=8)
                        S.op("dve", lambda e, sm8=sm8, t8v=t8v: e.tensor_tensor(out=sm8[:, 0, :], in0=t8v[:, :, 0], in1=t8v[:, :, 1], op=ALU.add), reads=[t8b], writes=[sm8b])
                        S.op("dve", lambda e, sm8=sm8: e.max(out=sm8[:, 1, :], in_=sm8[:, 0, :]), reads=[sm8b], writes=[sm8b])
                        S.op("dve", lambda e, sm8=sm8: e.tensor_scalar(out=sm8[:, 2, :], in0=sm8[:, 0, :], scalar1=sm8[:, 1, 3:4], scalar2=None, op0=ALU.is_ge), reads=[sm8b], writes=[sm8b])
                        S.op("dve", lambda e, sm8=sm8: e.tensor_scalar(out=sm8[:, 2, :], in0=sm8[:, 2, :], scalar1=-1.0, scalar2=1e9, op0=ALU.add, op1=ALU.mult), reads=[sm8b], writes=[sm8b])
                        bm, bmb = R3.next()
                        S.op("dve", lambda e, bm=bm, bs=bs, sm8=sm8: e.tensor_tensor(out=bm[:].rearrange("p (g k) -> p g k", k=8), in0=bs[:].rearrange("p (g k) -> p g k", k=8), in1=sm8[:, 2, :].unsqueeze(2).to_broadcast([128, 8, 8]), op=ALU.add), reads=[bsb, sm8b], writes=[bmb])
                        S.op("dve", lambda e, sm8=sm8, bm=bm: e.max(out=sm8[:, 3, :], in_=bm[:]), reads=[bmb], writes=[sm8b])
                        ei, eib = RI.next()
                        S.op("dve", lambda e, ei=ei, sm8=sm8, bm=bm: e.max_index(out=ei[:], in_max=sm8[:, 3, :], in_values=bm[:]), reads=[bmb, sm8b], writes=[eib])
                        S.op("dve", lambda e, ei=ei, tg=tg: e.tensor_copy(out=eidf[:, tg, :], in_=ei[:]), reads=[eib], writes=[rt_b])
                        S.op("dve", lambda e, sm8=sm8, bm=bm, tg=tg: e.tensor_scalar(out=selall[:, tg, :], in0=bm[:], scalar1=sm8[:, 3, 7:8], scalar2=None, op0=ALU.is_ge), reads=[bmb, sm8b], writes=[rt_b])
                        S.op("dve", lambda e, bm=bm, sc=sc, tg=tg: e.tensor_tensor(out=bm[:], in0=selall[:, tg, :], in1=sc[:], op=ALU.mult), reads=[rt_b, scb, bmb], writes=[bmb])
                        S.op("dve", lambda e, sm8=sm8, bm=bm: e.reduce_sum(out=sm8[:, 1, 0:1], in_=bm[:], axis=AX.X), reads=[bmb], writes=[sm8b])
                        S.op("dve", lambda e, sm8=sm8: e.reciprocal(out=sm8[:, 1, 1:2], in_=sm8[:, 1, 0:1]), reads=[sm8b], writes=[sm8b])
                        S.op("dve", lambda e, sm8=sm8, bm=bm, tg=tg: e.tensor_scalar(out=wall[:, tg, :], in0=bm[:], scalar1=sm8[:, 1, 1:2], scalar2=2.5, op0=ALU.mult, op1=ALU.mult), reads=[bmb, sm8b], writes=[rt_b])
                        pq, pqb = PS.next()
                        S.op("pe", lambda e, pq=pq, tg=tg: e.matmul(pq[:, 0:64], lhsT=ust[:, 0, :], rhs=selall[:, tg, :], start=True, stop=True), reads=[rt_b, c2b], writes=[pqb])
                        S.op("dve", lambda e, pq=pq, tg=tg: e.tensor_tensor(out=posall[:, tg, :], in0=pq[:, 0:64], in1=carry2[:], op=ALU.add), reads=[pqb, rt_b], writes=[rt_b])
                        S.op("dve", lambda e, tg=tg: e.tensor_scalar(out=posall[:, tg, :], in0=posall[:, tg, :], scalar1=float(CAP - 1), scalar2=1.0, op0=ALU.min, op1=ALU.add), reads=[rt_b], writes=[rt_b])
                        S.op("dve", lambda e, tg=tg: e.tensor_tensor(out=posall[:, tg, :], in0=posall[:, tg, :], in1=selall[:, tg, :], op=ALU.mult), reads=[rt_b], writes=[rt_b])
                        S.op("dve", lambda e, tg=tg: e.tensor_scalar(out=posall[:, tg, :], in0=posall[:, tg, :], scalar1=-1.0, scalar2=None, op0=ALU.add), reads=[rt_b], writes=[rt_b])
                        pq2, pq2b = PS.next()
                        S.op("pe", lambda e, pq2=pq2, tg=tg: e.matmul(pq2[:, 0:64], lhsT=ust[:, 1, :], rhs=selall[:, tg, :], start=True, stop=True), reads=[rt_b, c2b], writes=[pq2b])
                        S.op("dve", lambda e, pq2=pq2: e.tensor_tensor(out=carry2[:], in0=pq2[:, 0:64], in1=carry2[:], op=ALU.add), reads=[pq2b, rt_b], writes=[rt_b])
                    h1, h1b = h1T.next()
                    ys_cur = {}

                    def emit_sh(tt, nb, py, pyb, bi=bi, ys_cur=ys_cur):
                        if nb == 0:
                            k_ = hcnt[1] % 2
                            hcnt[1] += 1
                            ys_cur["t"] = ysr.next() + (k_,)
                        yt, ytb, k_ = ys_cur["t"]
                        S.op("act", lambda e, yt=yt, py=py, nb=nb: e.copy(out=yt[:, nb * 512:(nb + 1) * 512], in_=py[:]), reads=[pyb], writes=[ytb])
                        if nb == 3:
                            tg = bi * 4 + tt
                            S.dma("act", lambda e, yt=yt, tg=tg: e.dma_start(out=YS[tg * 128:(tg + 1) * 128, :], in_=yt[:]), yss[k_], reads=[ytb], writes=[])

                    ffn(hTb, hTb_b, swg, swb, swu, swb, swd, swb, h1, h1b, Rsg2, emit_sh)
                S.barrier()

            with ExitStack() as st:
                wst = DRing(S, nc, st, "wst", 2, [128, 4096], F32)
                wgr = Ring(nc, st, "wgr", 3, [128, 16, 512], BF16)
                wdr2 = Ring(nc, st, "wdr2", 2, [128, 4, D], BF16)
                xgr = DRing(S, nc, st, "xgr", 2, [128, 4, D], BF16)
                xTr = Ring(nc, st, "xTr", 1, [128, 16, CAP], BF16)
                h1r = Ring(nc, st, "h1r", 1, [128, 4, CAP], BF16)
                sgr = Ring(nc, st, "sgr", 2, [128, CAP], F32)
                ysb = Ring(nc, st, "ysb", 1, [128, 4, D], BF16)
                ysd = S.dma_src("ysd")
                Qr = Ring(nc, st, "Qr", 2, [128, CAP], F32)
                ixf = Ring(nc, st, "ixf", 2, [128, 4, 2], F32)
                ixu = Ring(nc, st, "ixu", 2, [128, 4], mybir.dt.uint32)
                ccnt = [0]

                def load_w(e_):
                    out = []
                    for (src, nk, ring) in ((wge, 16, wgr), (wue, 16, wgr), (wde, 4, wdr2)):
                        wt, wtb = ring.next()
                        flat = wt[:].rearrange("p a b -> p (a b)")
                        srcf = src[e_].rearrange("p a b -> p (a b)")
                        for hf in range(2):
                            t, tb, ts = wst.next()
                            S.dma("sp", lambda e, t=t, srcf=srcf, hf=hf: e.dma_start(out=t[:], in_=srcf[:, hf * 4096:(hf + 1) * 4096]), ts, writes=[tb])
                            eng = ("pool", "act")[ccnt[0] % 2]
                            ccnt[0] += 1
                            if eng == "act":
                                S.op("act", lambda e, t=t, flat=flat, hf=hf: e.copy(out=flat[:, hf * 4096:(hf + 1) * 4096], in_=t[:]), reads=[tb], writes=[wtb])
                            else:
                                S.op("pool", lambda e, t=t, flat=flat, hf=hf: e.tensor_copy(out=flat[:, hf * 4096:(hf + 1) * 4096], in_=t[:]), reads=[tb], writes=[wtb])
                        out += [wt, wtb]
                    return out

                def lists_and_gather(e_):
                    banks = [PS.next() for _ in range(4)]
                    for tg in range(16):
                        q, qb = Qr.next()
                        S.op("dve", lambda e, q=q, tg=tg, e_=e_: e.tensor_scalar(out=q[:], in0=io512[:], scalar1=posall[:, tg, e_:e_ + 1], scalar2=None, op0=ALU.is_equal), reads=[rt_b, c2b], writes=[qb])
                        for rg in range(4):
                            pb_, pbb_ = banks[rg]
                            S.op("pe", lambda e, pb_=pb_, q=q, rg=rg, tg=tg: e.matmul(pb_[:, 0:2], lhsT=q[:, rg * 128:(rg + 1) * 128], rhs=tokc[:, tg, :], start=(tg == 0), stop=(tg == 15)), reads=[qb, c2b], writes=[pbb_])
                    xf, xfb = ixf.next()
                    xu, xub = ixu.next()
                    for rg in range(4):
                        pb_, pbb_ = banks[rg]
                        S.op("dve", lambda e, xf=xf, pb_=pb_, rg=rg: e.tensor_copy(out=xf[:, rg, :], in_=pb_[:, 0:2]), reads=[pbb_], writes=[xfb])
                    S.op("dve", lambda e, xf=xf: e.tensor_scalar(out=xf[:, :, 1], in0=xf[:, :, 1], scalar1=-float(NB), scalar2=float(NB), op0=ALU.mult, op1=ALU.add), reads=[xfb], writes=[xfb])
                    S.op("dve", lambda e, xf=xf: e.tensor_tensor(out=xf[:, :, 0], in0=xf[:, :, 0], in1=xf[:, :, 1], op=ALU.add), reads=[xfb], writes=[xfb])
                    S.op("dve", lambda e, xf=xf: e.tensor_scalar(out=xf[:, :, 0], in0=xf[:, :, 0], scalar1=float(NB), scalar2=0.0, op0=ALU.min, op1=ALU.max), reads=[xfb], writes=[xfb])
                    S.op("dve", lambda e, xf=xf, xu=xu: e.tensor_copy(out=xu[:], in_=xf[:, :, 0]), reads=[xfb], writes=[xub])
                    xg, xgb, xgs = xgr.next()
                    for rg in range(4):
                        S.dma("pool", lambda e, xg=xg, xu=xu, rg=rg: e.indirect_dma_start(out=xg[:, rg, :], out_offset=None, in_=Hb, in_offset=bass.IndirectOffsetOnAxis(ap=xu[:, rg:rg + 1], axis=0)), xgs, reads=[xub], writes=[xgb])
                    return xg, xgb

                nexp = min(N_EXP, 64) if "2" in _PH else 0
                pend = lists_and_gather(0) if nexp else None
                for ex in range(nexp):
                    wg, wgb, wu, wub, wd, wdb = load_w(ex)
                    xg, xgb = pend
                    if ex + 1 < nexp:
                        pend = lists_and_gather(ex + 1)
                    xT_, xTb_ = xTr.next()
                    for rg in range(4):
                        for k8 in range(2):
                            pt, ptb_ = PT.next()
                            S.group("pe", [(lambda e, pt=pt, xg=xg, rg=rg, kc=k8 * 8 + j, j=j: e.transpose(pt[:, j, :], xg[:, rg, kc * 128:(kc + 1) * 128], idb2[:])) for j in range(8)], reads=[xgb, c2b], writes=[ptb_])
                            eng = "act" if (rg * 2 + k8) % 2 == 0 else "dve"
                            if eng == "act":
                                S.op("act", lambda e, pt=pt, xT_=xT_, rg=rg, k8=k8: e.copy(out=xT_[:, k8 * 8:(k8 + 1) * 8, rg * 128:(rg + 1) * 128], in_=pt[:]), reads=[ptb_], writes=[xTb_])
                            else:
                                S.op("dve", lambda e, pt=pt, xT_=xT_, rg=rg, k8=k8: e.tensor_copy(out=xT_[:, k8 * 8:(k8 + 1) * 8, rg * 128:(rg + 1) * 128], in_=pt[:]), reads=[ptb_], writes=[xTb_])
                    h1, h1b = h1r.next()
                    yt, ytb = ysb.next()

                    def emit_y(tt, nb, py, pyb, yt=yt, ytb=ytb):
                        if (tt * 4 + nb) % 2 == 0:
                            S.op("act", lambda e, yt=yt, py=py, tt=tt, nb=nb: e.copy(out=yt[:, tt, nb * 512:(nb + 1) * 512], in_=py[:]), reads=[pyb], writes=[ytb])
                        else:
                            S.op("dve", lambda e, yt=yt, py=py, tt=tt, nb=nb: e.tensor_copy(out=yt[:, tt, nb * 512:(nb + 1) * 512], in_=py[:]), reads=[pyb], writes=[ytb])

                    ffn(xT_, xTb_, wg, wgb, wu, wub, wd, wdb, h1, h1b, sgr, emit_y)
                    S.dma("act", lambda e, yt=yt, ex=ex: e.dma_start(out=Yx[ex * CAP:(ex + 1) * CAP, :].rearrange("(g p) n -> p g n", p=128), in_=yt[:]), ysd, reads=[ytb], writes=[])
                S.barrier()

            with ExitStack() as st:
                sb = lambda n, s, d=F32: st.enter_context(nc.sbuf_tensor(n, list(s), d))
                ln2 = sb("ln2", [128, 2, D])
                cq2d = S.dma_src("cq2d")
                l2b = Buf("ln2")
                S.dma("sp", lambda e: e.dma_start(out=ln2[:], in_=lnp[:, 2:4, :]), cq2d, writes=[l2b])
                hr = DRing(S, nc, st, "hr3", 2, [128, D], F32)
                yr = DRing(S, nc, st, "yr3", 2, [128, D], F32)
                gk = DRing(S, nc, st, "gk3", 4, [128, D], BF16)
                acc = Ring(nc, st, "acc3", 2, [128, D], F32)
                oh = Ring(nc, st, "oh3", 2, [128, 64], F32)
                sw = Ring(nc, st, "sw3", 2, [128, 2, 8], F32)
                su = Ring(nc, st, "su3", 2, [128, 8], mybir.dt.uint32)
                Zs2 = Ring(nc, st, "zs2", 2, [128, 32], F32)
                osr = [S.dma_src("os%d" % k) for k in range(2)]
                for tg in range(16 if "2" in _PH else 0):
                    ht, htb, hts = hr.next()
                    S.dma("sp", lambda e, ht=ht, tg=tg: e.dma_start(out=ht[:], in_=Hs[tg * 128:(tg + 1) * 128, :]), hts, writes=[htb])
                    yt, ytb, yts = yr.next()
                    S.dma("act", lambda e, yt=yt, tg=tg: e.dma_start(out=yt[:], in_=YS[tg * 128:(tg + 1) * 128, :]), yts, writes=[ytb])
                    S.op("dve", lambda e, tg=tg: e.tensor_tensor(out=posall[:, tg, :], in0=posall[:, tg, :], in1=io64[:, 1, :], op=ALU.add), reads=[rt_b, c2b], writes=[rt_b])
                    s_, s_b = sw.next()
                    for k in range(8):
                        o_, o_b = oh.next()
                        S.op("dve", lambda e, o_=o_, tg=tg, k=k: e.tensor_scalar(out=o_[:], in0=io64[:, 0, :], scalar1=eidf[:, tg, k:k + 1], scalar2=None, op0=ALU.is_equal), reads=[rt_b, c2b], writes=[o_b])
                        o2_, o2_b = oh.next()
                        S.op("dve", lambda e, o_=o_, o2_=o2_, tg=tg: e.tensor_tensor(out=o2_[:], in0=o_[:], in1=posall[:, tg, :], op=ALU.mult), reads=[o_b, rt_b], writes=[o2_b])
                        S.op("dve", lambda e, o2_=o2_, s_=s_, k=k: e.reduce_sum(out=s_[:, 0, k:k + 1], in_=o2_[:], axis=AX.X), reads=[o2_b], writes=[s_b])
                        S.op("dve", lambda e, o_=o_, tg=tg: e.tensor_tensor(out=o_[:], in0=o_[:], in1=wall[:, tg, :], op=ALU.mult), reads=[o_b, rt_b], writes=[o_b])
                        S.op("dve", lambda e, o_=o_, s_=s_, k=k: e.reduce_sum(out=s_[:, 1, k:k + 1], in_=o_[:], axis=AX.X), reads=[o_b], writes=[s_b])
                    u_, u_b = su.next()
                    S.op("dve", lambda e, u_=u_, s_=s_: e.tensor_copy(out=u_[:], in_=s_[:, 0, :]), reads=[s_b], writes=[u_b])
                    a_, a_b = acc.next()
                    S.op("dve", lambda e, a_=a_, ht=ht, yt=yt: e.scalar_tensor_tensor(out=a_[:], in0=ht[:], scalar=DN_ALPHA, in1=yt[:], op0=ALU.mult, op1=ALU.add), reads=[htb, ytb], writes=[a_b])
                    for k in range(8):
                        g_, g_b, g_s = gk.next()
                        S.dma("pool", lambda e, g_=g_, u_=u_, k=k: e.indirect_dma_start(out=g_[:], out_offset=None, in_=Yx, in_offset=bass.IndirectOffsetOnAxis(ap=u_[:, k:k + 1], axis=0)), g_s, reads=[u_b], writes=[g_b])
                        S.op("dve", lambda e, a_=a_, g_=g_, s_=s_, k=k: e.scalar_tensor_tensor(out=a_[:], in0=g_[:], scalar=s_[:, 1, k:k + 1], in1=a_[:], op0=ALU.mult, op1=ALU.add), reads=[g_b, s_b, a_b], writes=[a_b])
                    zs, zsb = Zs2.next()
                    for q4 in range(4):
                        S.op("dve", lambda e, zs=zs, a_=a_, q4=q4: e.bn_stats(out=zs[:, q4 * 6:(q4 + 1) * 6], in_=a_[:, q4 * 512:(q4 + 1) * 512]), reads=[a_b], writes=[zsb])
                    S.op("dve", lambda e, zs=zs: e.bn_aggr(out=zs[:, 24:26], in_=zs[:, 0:24]), reads=[zsb], writes=[zsb])
                    S.op("dve", lambda e, zs=zs: e.tensor_scalar(out=zs[:, 25:26], in0=zs[:, 25:26], scalar1=LN_EPS, scalar2=None, op0=ALU.add), reads=[zsb], writes=[zsb])
                    S.op("act", lambda e, zs=zs: e.activation(out=zs[:, 25:26], in_=zs[:, 25:26], func=AF.Sqrt), reads=[zsb], writes=[zsb])
                    S.op("dve", lambda e, zs=zs: e.reciprocal(out=zs[:, 25:26], in_=zs[:, 25:26]), reads=[zsb], writes=[zsb])
                    S.op("dve", lambda e, zs=zs, a_=a_: e.tensor_scalar(out=a_[:], in0=a_[:], scalar1=zs[:, 24:25], scalar2=zs[:, 25:26], op0=ALU.subtract, op1=ALU.mult), reads=[zsb, a_b], writes=[a_b])
                    S.op("pool", lambda e, a_=a_: e.tensor_tensor(out=a_[:], in0=a_[:], in1=ln2[:, 0, :], op=ALU.mult), reads=[a_b, l2b], writes=[a_b])
                    S.op("pool", lambda e, a_=a_: e.tensor_tensor(out=a_[:], in0=a_[:], in1=ln2[:, 1, :], op=ALU.add), reads=[a_b, l2b], writes=[a_b])
                    S.dma("sp", lambda e, a_=a_, tg=tg: e.dma_start(out=out[tg * 128:(tg + 1) * 128, :], in_=a_[:]), osr[tg % 2], reads=[a_b], writes=[])
                S.barrier()
        S.finish()
        print("instructions:", S.n_inst, flush=True)
    return nc


def _consts(p):
    half = 128
    freq = (10000.0 ** (-np.arange(half, dtype=np.float64) / half))
    cosT = np.zeros((16, 128, TB), np.float32)
    sinT = np.zeros((16, 128, TB), np.float32)
    flags = np.zeros((128, 2, 16), np.float32)
    flags[:, 1, :] = 1.0
    for blk in range(16):
        q = p - 3 + blk // 4
        if q < 0:
            continue
        pos = q * NB + (blk % 4) * TB + np.arange(TB, dtype=np.float64)
        ang = (pos[None, :].astype(np.float32) * freq[:, None].astype(np.float32)).astype(np.float32)
        cosT[blk] = np.cos(ang)
        sinT[blk] = np.sin(ang)
        if q == 0 and blk % 4 == 0:
            flags[:, 0, blk] = 1.0
            flags[:, 1, blk] = 0.0
    return cosT, sinT, flags


def kernel(x, w_in, conv_w, conv_b, lru_wa, lru_ba, lru_wi, lru_bi, lru_lambda, ret_gn_gain,
           w_lru_out, w_ret_out, b_gate, w_o, ln1_g, ln1_b, w_router, router_bias,
           w_gate_e, w_up_e, w_down_e, w_gate_s, w_up_s, w_down_s, ln2_g, ln2_b):
    import time
    _t0 = time.time()
    f = lambda a: np.ascontiguousarray(np.asarray(a, dtype=np.float32))
    x = f(x)
    w_in = f(w_in)[0]
    kp = lambda w: w.reshape(16, 128, -1).transpose(1, 0, 2)
    wx, wy = w_in[:, 0:2048], w_in[:, 2048:4096]
    wq_, wk_ = w_in[:, 4096:6144], w_in[:, 6144:8192]
    wv_, wg_ = w_in[:, 8192:12288], w_in[:, 12288:16384]
    wga, wgb = w_in[:, 16384:18432], w_in[:, 18432:20480]
    w_in_l = np.stack([np.stack([kp(wx[:, h * 128:(h + 1) * 128]), kp(wy[:, h * 128:(h + 1) * 128])], 1) for h in range(16)])
    w_in_r = np.stack([np.stack([
        kp(wq_[:, h * 256:(h + 1) * 256]), kp(wk_[:, h * 256:(h + 1) * 256]),
        kp(wv_[:, h * 512:h * 512 + 256]), kp(wv_[:, h * 512 + 256:(h + 1) * 512]),
        kp(wg_[:, h * 512:h * 512 + 256]), kp(wg_[:, h * 512 + 256:(h + 1) * 512])]) for h in range(8)])
    wlo, wro, wo_ = f(w_lru_out)[0], f(w_ret_out)[0], f(w_o)[0]
    kp2 = lambda w, n: w.reshape(n, 128, -1).transpose(1, 0, 2)
    w3 = np.stack([np.concatenate([
        kp2(wlo[:, nt * 128:(nt + 1) * 128], 16), kp2(wro[:, nt * 128:(nt + 1) * 128], 32),
        kp2(wga[:, nt * 128:(nt + 1) * 128], 16), kp2(wgb[:, nt * 128:(nt + 1) * 128], 16)], 1) for nt in range(16)])
    wo_t = np.stack([kp(wo_[:, nb * 512:(nb + 1) * 512]) for nb in range(4)])
    wge = np.concatenate([f(w_gate_e)[0], f(w_gate_s)], 0).reshape(65, 16, 128, 512).transpose(0, 2, 1, 3)
    wue = np.concatenate([f(w_up_e)[0], f(w_up_s)], 0).reshape(65, 16, 128, 512).transpose(0, 2, 1, 3)
    wde = np.concatenate([f(w_down_e)[0], f(w_down_s)], 0).reshape(65, 4, 128, 2048).transpose(0, 2, 1, 3)
    wr_t = kp(f(w_router)[0])
    chp = lambda v: f(v).reshape(16, 128).T
    cw = f(conv_w)[0]
    lrup = np.stack([chp(cw[0]), chp(cw[1]), chp(cw[2]), chp(cw[3]), chp(conv_b), chp(lru_ba), chp(lru_bi), chp(lru_lambda)], 2)
    wai = np.stack([f(lru_wa)[0].transpose(1, 0, 2), f(lru_wi)[0].transpose(1, 0, 2)], 2)
    bgv = f(b_gate)[0]
    bg = np.stack([bgv[:2048].reshape(16, 128).T, bgv[2048:].reshape(16, 128).T], 1)
    bc = lambda v, n: np.ascontiguousarray(np.broadcast_to(f(v).reshape(1, -1), (128, n)))
    lnp = np.stack([bc(ln1_g, D), bc(ln1_b, D), bc(ln2_g, D), bc(ln2_b, D)], 1)
    idx = np.arange(128, dtype=np.float64)
    dmaskT = np.zeros((128, 8, 128), np.float32)
    xizeta = np.zeros((128, 2, 8), np.float32)
    for h in range(8):
        lg = np.log1p(-np.exp2(-5.0 - h))
        diff = idx[None, :] - idx[:, None]
        dmaskT[:, h, :] = np.where(diff >= 0, np.exp(np.maximum(diff, 0) * lg), 0.0) / 16.0
        xizeta[:, 0, h] = np.exp((idx + 1.0) * lg)
        xizeta[:, 1, h] = np.exp((127.0 - idx) * lg) / 16.0
    ustrict = np.zeros((128, 2, 128), np.float32)
    ustrict[:, 0, :] = (np.arange(128)[:, None] < np.arange(128)[None, :]).astype(np.float32)
    ustrict[:, 1, :] = 1.0
    iota512 = np.broadcast_to(np.arange(512, dtype=np.float32)[None, :], (128, 512))
    iota64 = np.stack([np.broadcast_to(np.arange(64, dtype=np.float32)[None, :], (128, 64)),
                       np.broadcast_to(512.0 * np.arange(64, dtype=np.float32)[None, :], (128, 64))], 1)
    tokcol = np.zeros((128, 16, 2), np.float32)
    tokcol[:, :, 0] = np.arange(16)[None, :] * 128 + np.arange(128)[:, None]
    tokcol[:, :, 1] = 1.0
    shared = dict(ustrict=ustrict, iota512=iota512, iota64=iota64, tokcol=tokcol, w_in_l=w_in_l, w_in_r=w_in_r, w3=w3, wo=wo_t, wge=wge, wue=wue, wde=wde, wr=wr_t,
                  lrup=lrup, wai=wai, bg=bg, dmaskT=dmaskT, xizeta=xizeta, gain_bc=bc(ret_gn_gain, 4096),
                  lnp=lnp, rb_bc=bc(router_bias, 64), ident=np.eye(128, dtype=np.float32))
    shared = {k: np.ascontiguousarray(v, dtype=np.float32) for k, v in shared.items()}
    in_maps = []
    for c in range(8):
        b, p = divmod(c, 4)
        cosT, sinT, flags = _consts(p)
        xT = np.zeros((16, 128, 16, TB), np.float32)
        for blk in range(16):
            q = p - 3 + blk // 4
            if q < 0:
                continue
            t0 = q * NB + (blk % 4) * TB
            xT[blk] = x[b, t0:t0 + TB, :].reshape(TB, 16, 128).transpose(2, 1, 0)
        m = dict(shared)
        m.update(xT=xT, xres=np.ascontiguousarray(x[b, p * NB:(p + 1) * NB, :]), cosT=cosT, sinT=sinT, flags=flags)
        in_maps.append(m)
    print("[kernel] host layout %.1fs" % (time.time() - _t0), flush=True)
    nc = build()
    print("[kernel] build %.1fs" % (time.time() - _t0), flush=True)
    res = run_bass_kernel_spmd(nc, in_maps, core_ids=list(range(8)))
    print("[kernel] run done %.1fs" % (time.time() - _t0), flush=True)
    if os.environ.get("MK_DBG", "0") == "1":
        _LAST.clear()
        _LAST.append(res.results)
    outp = np.zeros((2, SEQ, D), np.float32)
    for c in range(8):
        b, p = divmod(c, 4)
        outp[b, p * NB:(p + 1) * NB, :] = res.results[c]["out"]
    return outp
```

```python
import os
import types
import numpy as np
from contextlib import ExitStack
import concourse.bass as bass
import concourse.mybir as mybir
from concourse.bass_utils import run_bass_kernel_spmd

F32 = mybir.dt.float32
BF16 = mybir.dt.bfloat16
AF = mybir.ActivationFunctionType
ALU = mybir.AluOpType
AX = mybir.AxisListType

D = 2048
SEQ = 8192
NB = 2048
TB = 512
NE = 64
DN_ALPHA = 2.0 ** 0.25
LN_EPS = 1e-5
GAMMA = [1.0 - 2.0 ** (-5.0 - h) for h in range(8)]
_LAST = []
N_EXP = int(os.environ.get("MK_NEXP", "65"))
_PH = os.environ.get("MK_PH", "0,1,1b,2").split(",")
_BLKS = [int(v) for v in os.environ.get("MK_BLKS", ",".join(str(i) for i in range(16))).split(",")]
_LH = int(os.environ.get("MK_LH", "16"))
_RH = int(os.environ.get("MK_RH", "8"))


def freeze(fn):
    if fn.__closure__ is None:
        return fn
    cells = []
    for c in fn.__closure__:
        try:
            cells.append(types.CellType(c.cell_contents))
        except ValueError:
            cells.append(c)
    return types.FunctionType(fn.__code__, fn.__globals__, fn.__name__, fn.__defaults__, tuple(cells))


class Src:
    def __init__(self, sem, step, name):
        self.sem, self.step, self.count, self.name = sem, step, 0, name


class Buf:
    __slots__ = ("w", "r", "name")

    def __init__(self, name=""):
        self.w, self.r, self.name = None, {}, name


class Sched:
    ENGS = ("pe", "act", "dve", "pool", "sp")

    def __init__(self, nc, stack):
        self.nc, self.stack = nc, stack
        self.streams = {e: [] for e in self.ENGS}
        self.src = {}
        for e in self.ENGS:
            self.src[e] = Src(stack.enter_context(nc.semaphore("s_" + e)), 1, e)
        self.seen = {e: {} for e in self.ENGS}
        self.dma_srcs = []
        self.n_inst = 0

    def dma_src(self, name):
        s = Src(self.stack.enter_context(self.nc.semaphore("d_" + name)), 16, name)
        self.dma_srcs.append(s)
        return s

    def _waits(self, eng, reads, writes):
        need = {}
        me = self.src[eng]

        def add(ev):
            if ev is None:
                return
            s, c = ev
            if s is me and eng == "pe":
                return
            if need.get(s, 0) < c:
                need[s] = c

        for b in reads:
            add(b.w)
        for b in writes:
            add(b.w)
            for s, c in b.r.items():
                if s is not me:
                    add((s, c))
        out = []
        seen = self.seen[eng]
        for s, c in need.items():
            if seen.get(s, 0) < c:
                seen[s] = c
                out.append((s, c))
        return out

    def op(self, eng, fn, reads=(), writes=()):
        self.group(eng, [fn], reads, writes)

    def group(self, eng, fns, reads=(), writes=()):
        waits = self._waits(eng, reads, writes)
        me = self.src[eng]
        me.count += 1
        cnt = me.count
        st = self.streams[eng]
        for i, fn in enumerate(fns):
            last = i == len(fns) - 1
            st.append((waits if i == 0 else (), freeze(fn), me if last else None, 1))
        for b in reads:
            b.r[me] = cnt
        for b in writes:
            b.w = (me, cnt)
            b.r = {}
        self.n_inst += len(fns)

    def dma(self, q, fn, dsrc, reads=(), writes=()):
        waits = self._waits(q, reads, writes)
        dsrc.count += 16
        cnt = dsrc.count
        self.streams[q].append((waits, freeze(fn), dsrc, 16))
        for b in reads:
            b.r[dsrc] = cnt
        for b in writes:
            b.w = (dsrc, cnt)
            b.r = {}
        self.n_inst += 1

    def barrier(self):
        allsrc = [self.src[e] for e in self.ENGS] + self.dma_srcs
        for e in self.ENGS:
            waits = []
            for s in allsrc:
                if s is self.src[e] or s.count == 0:
                    continue
                if self.seen[e].get(s, 0) < s.count:
                    self.seen[e][s] = s.count
                    waits.append((s, s.count))
            if waits:
                self.streams[e].append((waits, None, None, 0))

    def finish(self):
        nc = self.nc
        eh = {"pe": "tensor", "act": "scalar", "dve": "vector", "pool": "gpsimd", "sp": "sync"}
        self.barrier()
        with nc.Block() as block:
            for e in self.ENGS:
                stream = self.streams[e]

                def body(engine, stream=stream):
                    for waits, fn, src, inc in stream:
                        for s, c in waits:
                            engine.wait_ge(s.sem, c)
                        if fn is not None:
                            ins = fn(engine)
                            if src is not None:
                                ins.then_inc(src.sem, inc)

                getattr(block, eh[e])(body)


class Ring:
    def __init__(self, nc, st, name, n, shape, dt, psum=False):
        self.t, self.b, self.i = [], [], 0
        for k in range(n):
            nm = "%s%d" % (name, k)
            if psum:
                self.t.append(st.enter_context(nc.psum_tensor(nm, shape, dt)))
            else:
                self.t.append(st.enter_context(nc.sbuf_tensor(nm, shape, dt)))
            self.b.append(Buf(nm))

    def next(self):
        k = self.i % len(self.t)
        self.i += 1
        return self.t[k], self.b[k]


class DRing:
    def __init__(self, S, nc, st, name, n, shape, dt):
        self.r = Ring(nc, st, name, n, shape, dt)
        self.s = [S.dma_src("%s%d" % (name, k)) for k in range(n)]

    def next(self):
        k = self.r.i % len(self.s)
        t, b = self.r.next()
        return t, b, self.s[k]


def build():
    nc = bass.Bass("TRN2", target_bir_lowering=False)
    ein = lambda n, s, d=F32: nc.dram_tensor(n, list(s), d, kind="ExternalInput").ap()
    scr = lambda n, s, d=BF16: nc.dram_tensor(n, list(s), d).ap()
    xT = ein("xT", [16, 128, 16, TB])
    xres = ein("xres", [NB, D])
    w_in_l = ein("w_in_l", [16, 128, 2, 16, 128])
    w_in_r = ein("w_in_r", [8, 6, 128, 16, 256])
    w3 = ein("w3", [16, 128, 80, 128])
    wo = ein("wo", [4, 128, 16, 512])
    wge = ein("wge", [65, 128, 16, 512])
    wue = ein("wue", [65, 128, 16, 512])
    wde = ein("wde", [65, 128, 4, 2048])
    wr = ein("wr", [128, 16, 64])
    lrup = ein("lrup", [128, 16, 8])
    wai = ein("wai", [128, 16, 2, 128])
    bg = ein("bg", [128, 2, 16])
    cosT = ein("cosT", [16, 128, TB])
    sinT = ein("sinT", [16, 128, TB])
    flags = ein("flags", [128, 2, 16])
    dmaskT = ein("dmaskT", [128, 8, 128])
    xizeta = ein("xizeta", [128, 2, 8])
    gain_bc = ein("gain_bc", [128, 4096])
    lnp = ein("lnp", [128, 4, D])
    rb_bc = ein("rb_bc", [128, 64])
    ident = ein("ident", [128, 128])
    ustrict = ein("ustrict", [128, 2, 128])
    iota512 = ein("iota512", [128, 512])
    iota64 = ein("iota64", [128, 2, 64])
    tokcol = ein("tokcol", [128, 16, 4])
    out = nc.dram_tensor("out", [NB, D], F32, kind="ExternalOutput").ap()
    b_in_l = scr("b_in_l", [16, 128, 2, 16, 128])
    b_in_r = scr("b_in_r", [8, 6, 128, 16, 256])
    b_w3 = scr("b_w3", [16, 128, 80, 128])
    b_wo = scr("b_wo", [4, 128, 16, 512])
    b_wge = scr("b_wge", [1, 128, 16, 512])
    b_wue = scr("b_wue", [1, 128, 16, 512])
    b_wde = scr("b_wde", [1, 128, 4, 2048])
    _dbg = os.environ.get("MK_DBG", "0") == "1"
    dscr = (lambda n, s, d=BF16: nc.dram_tensor(n, list(s), d, kind="ExternalOutput").ap()) if _dbg else scr
    Hs = dscr("Hs", [NB, D], F32)
    UA = dscr("UA", [4, 128, 16, TB])
    Hb = scr("Hb", [NB + 128, D])
    YS = scr("YS", [NB, D], F32)
    Yx = scr("Yx", [64 * 512, D])
    UB = dscr("UB", [4, 128, 32, TB])

    with ExitStack() as top:
        S = Sched(nc, top)
        dr_w = Buf("dram_w")
        dr_H = Buf("dram_H")
        dr_out = Buf("dram_out")
        PS = Ring(nc, top, "ps", 6, [128, 512], F32, psum=True)
        PT = Ring(nc, top, "pt", 2, [128, 8, 128], BF16, psum=True)

        dout = S.dma_src("dout")

        with ExitStack() as st:
            stg = DRing(S, nc, st, "cst", 3, [128, 4096], F32)
            cvb = Ring(nc, st, "cvb", 3, [128, 4096], BF16)
            cvs = [S.dma_src("cvo%d" % k) for k in range(3)]
            cnt = [0]

            def convert(src2d, dst2d, ncols):
                for c0 in range(0, ncols, 4096):
                    cw = min(4096, ncols - c0)
                    t, tb, ts = stg.next()
                    S.dma("sp", lambda e, t=t, c0=c0, cw=cw: e.dma_start(out=t[:, 0:cw], in_=src2d[:, c0:c0 + cw]), ts, writes=[tb])
                    k = cnt[0] % 3
                    o, ob = cvb.next()
                    eng = ("pool", "act", "dve")[cnt[0] % 3]
                    cnt[0] += 1
                    if eng == "act":
                        S.op("act", lambda e, o=o, t=t, cw=cw: e.copy(out=o[:, 0:cw], in_=t[:, 0:cw]), reads=[tb], writes=[ob])
                    else:
                        S.op(eng, lambda e, o=o, t=t, cw=cw: e.tensor_copy(out=o[:, 0:cw], in_=t[:, 0:cw]), reads=[tb], writes=[ob])
                    S.dma("act" if k == 1 else "sp", lambda e, o=o, c0=c0, cw=cw: e.dma_start(out=dst2d[:, c0:c0 + cw], in_=o[:, 0:cw]), cvs[k], reads=[ob], writes=[])

            for g in range(16 if "0" in _PH else 0):
                convert(w_in_l[g].rearrange("p a k c -> p (a k c)"), b_in_l[g].rearrange("p a k c -> p (a k c)"), 4096)
            for h in range(8 if "0" in _PH else 0):
                for j in range(6):
                    convert(w_in_r[h, j].rearrange("p k c -> p (k c)"), b_in_r[h, j].rearrange("p k c -> p (k c)"), 4096)
            for g in range(16 if "0" in _PH else 0):
                convert(w3[g].rearrange("p k c -> p (k c)"), b_w3[g].rearrange("p k c -> p (k c)"), 10240)
            for g in range(4 if "0" in _PH else 0):
                convert(wo[g].rearrange("p k c -> p (k c)"), b_wo[g].rearrange("p k c -> p (k c)"), 8192)
            if "0" in _PH:
                convert(wge[64].rearrange("p k c -> p (k c)"), b_wge[0].rearrange("p k c -> p (k c)"), 8192)
                convert(wue[64].rearrange("p k c -> p (k c)"), b_wue[0].rearrange("p k c -> p (k c)"), 8192)
                convert(wde[64].rearrange("p k c -> p (k c)"), b_wde[0].rearrange("p k c -> p (k c)"), 8192)
            S.barrier()

        with ExitStack() as st:
            sb = lambda n, s, d=F32: st.enter_context(nc.sbuf_tensor(n, list(s), d))
            cq = S.dma_src("cq")
            cb_ = Buf("consts")
            lp = sb("lp", [128, 16, 8])
            wab = sb("wab", [128, 16, 2, 128], BF16)
            bgt = sb("bgt", [128, 2, 16])
            flg = sb("flg", [128, 2, 16])
            dmk = sb("dmk", [128, 8, 128])
            xz = sb("xz", [128, 2, 8])
            idf = sb("idf", [128, 128])
            idb = sb("idb", [128, 128], BF16)
            lrc = sb("lrc", [128, 2, 16])
            ltmp = sb("ltmp", [128, 16])
            hst = sb("hst", [128, 16])
            carry = sb("carry", [128, 16, 4])
            Sst = sb("Sst", [128, 8, 2, 512])
            for t_, a_ in ((lp, lrup), (bgt, bg), (flg, flags), (dmk, dmaskT), (xz, xizeta), (idf, ident)):
                S.dma("sp", lambda e, t_=t_, a_=a_: e.dma_start(out=t_[:], in_=a_), cq, writes=[cb_])
            S.op("dve", lambda e: e.tensor_copy(out=idb[:], in_=idf[:]), reads=[cb_], writes=[cb_])
            S.op("act", lambda e: e.activation(out=ltmp[:], in_=lp[:, :, 7], func=AF.Exp, scale=-1.0), reads=[cb_], writes=[cb_])
            S.op("act", lambda e: e.activation(out=ltmp[:], in_=ltmp[:], func=AF.Ln, bias=1.0), reads=[cb_], writes=[cb_])
            S.op("dve", lambda e: e.tensor_scalar(out=lrc[:, 0, :], in0=ltmp[:], scalar1=-8.0, scalar2=None, op0=ALU.mult), reads=[cb_], writes=[cb_])
            S.op("dve", lambda e: e.tensor_scalar(out=lrc[:, 1, :], in0=ltmp[:], scalar1=-16.0, scalar2=None, op0=ALU.mult), reads=[cb_], writes=[cb_])
            S.op("pool", lambda e: e.memset(hst[:], 0.0), writes=[cb_])
            S.op("pool", lambda e: e.memset(carry[:], 0.0), writes=[cb_])
            S.op("pool", lambda e: e.memset(Sst[:], 0.0), writes=[cb_])
            hst_b = [Buf("hst%d" % h) for h in range(16)]
            car_b = [Buf("car%d" % h) for h in range(16)]
            S_b = [Buf("S%d" % h) for h in range(8)]
            for bl in hst_b + car_b + S_b:
                bl.w = cb_.w

            xstg = DRing(S, nc, st, "xstg", 2, [128, 2, TB], F32)
            for hf in range(4):
                t, tb, ts = xstg.next()
                S.dma("sp", lambda e, t=t, hf=hf: e.dma_start(out=t[:].rearrange("p a b -> p (a b)"), in_=wai[:, hf * 4:(hf + 1) * 4].rearrange("p h a o -> p (h a o)")), ts, writes=[tb])
                S.op("dve", lambda e, t=t, hf=hf: e.tensor_copy(out=wab[:, hf * 4:(hf + 1) * 4].rearrange("p h a o -> p (h a o)"), in_=t[:].rearrange("p a b -> p (a b)")), reads=[tb], writes=[cb_])
            xTb = Ring(nc, st, "xTb", 1, [128, 16, TB], BF16)
            cs = DRing(S, nc, st, "cs", 1, [128, 2, TB], F32)
            wl = DRing(S, nc, st, "wl", 2, [128, 2, 16, 128], BF16)
            wq = DRing(S, nc, st, "wq", 5, [128, 16, 256], BF16)
            L = {n: Ring(nc, st, "l_" + n, 2, [128, TB], F32) for n in ("xa", "r", "i", "a", "m", "h", "gy")}
            Lxs = Ring(nc, st, "l_xs", 2, [128, TB + 4], F32)
            Lxab = Ring(nc, st, "l_xab", 2, [128, TB], BF16)
            Lin = Ring(nc, st, "l_in", 2, [128, 1], F32)
            Rt = Ring(nc, st, "r_t", 4, [128, TB], F32)
            Rq = Ring(nc, st, "r_q", 2, [128, 2, TB], BF16)
            Rk = Ring(nc, st, "r_k", 2, [128, 2, TB], BF16)
            Rkz = Ring(nc, st, "r_kz", 2, [128, 256], BF16)
            Rv = Ring(nc, st, "r_v", 2, [128, 512], BF16)
            Rs = Ring(nc, st, "r_s", 2, [128, 128], BF16)
            Ro = Ring(nc, st, "r_o", 3, [128, 512], F32)
            Rsg = Ring(nc, st, "r_sg", 2, [128, 512], F32)
            Rub = Ring(nc, st, "r_ub", 2, [128, 512], BF16)
            Rst = Ring(nc, st, "r_st", 2, [128, 8], F32)
            Sb = Ring(nc, st, "Sb", 2, [128, 2, 512], BF16)
            gn = DRing(S, nc, st, "gn", 1, [128, 512], F32)
            uar = Ring(nc, st, "uar", 2, [128, TB], BF16)
            ubr = Ring(nc, st, "ubr", 1, [128, 4, TB], BF16)
            uas = [S.dma_src("uas%d" % k) for k in range(2)]
            ubs = [S.dma_src("ubs%d" % k) for k in range(2)]
            uacnt = [0, 0]

            print("phase1 sbuf remaining", nc.sbuf_bytes_remaining, flush=True)

            def mmgroup(ps, psb, pairs, reads):
                n = len(pairs)
                S.group("pe", [
                    (lambda e, l=l, r=r, i=i: e.matmul(ps, lhsT=l, rhs=r, start=(i == 0), stop=(i == n - 1)))
                    for i, (l, r) in enumerate(pairs)], reads=reads, writes=[psb])

            for blk in (_BLKS if "1" in _PH else []):
                main = blk >= 12
                xb, xbb = xTb.next()
                for hf in range(8):
                    t, tb, ts = xstg.next()
                    S.dma("sp", lambda e, t=t, hf=hf, blk=blk: e.dma_start(out=t[:], in_=xT[blk, :, hf * 2:(hf + 1) * 2, :]), ts, writes=[tb])
                    S.op("pool", lambda e, t=t, xb=xb, hf=hf: e.tensor_copy(out=xb[:, hf * 2:(hf + 1) * 2, :], in_=t[:]), reads=[tb], writes=[xbb])
                ct, ctb, cts = cs.next()
                S.dma("act", lambda e, ct=ct, blk=blk: e.dma_start(out=ct[:, 0, :], in_=cosT[blk]), cts, writes=[ctb])
                S.dma("act", lambda e, ct=ct, blk=blk: e.dma_start(out=ct[:, 1, :], in_=sinT[blk]), cts, writes=[ctb])

                def lru_head(h, blk=blk, main=main, xb=xb, xbb=xbb):
                    w, wb_, ws = wl.next()
                    if main:
                        S.dma("sp", lambda e, w=w, h=h: e.dma_start(out=w[:], in_=b_in_l[h]), ws, reads=[dr_w], writes=[wb_])
                        yield
                    else:
                        S.dma("sp", lambda e, w=w, h=h: e.dma_start(out=w[:, 0], in_=b_in_l[h, :, 0]), ws, reads=[dr_w], writes=[wb_])
                        yield
                    px, pxb = PS.next()
                    mmgroup(px[:], pxb, [(w[:, 0, kc, :], xb[:, kc, :]) for kc in range(16)], [wb_, xbb])
                    yield
                    xs, xsb = Lxs.next()
                    S.op("act", lambda e, xs=xs, px=px: e.copy(out=xs[:, 4:TB + 4], in_=px[:]), reads=[pxb], writes=[xsb])
                    yield
                    S.op("pool", lambda e, xs=xs, h=h: e.tensor_copy(out=xs[:, 0:4], in_=carry[:, h, :]), reads=[car_b[h]], writes=[xsb])
                    yield
                    S.op("pool", lambda e, xs=xs, h=h: e.tensor_copy(out=carry[:, h, :], in_=xs[:, TB:TB + 4]), reads=[xsb], writes=[car_b[h]])
                    yield
                    xa, xab_ = L["xa"].next()
                    S.op("dve", lambda e, xa=xa, xs=xs, h=h: e.tensor_scalar(out=xa[:], in0=xs[:, 4:TB + 4], scalar1=lp[:, h, 3:4], scalar2=lp[:, h, 4:5], op0=ALU.mult, op1=ALU.add), reads=[xsb], writes=[xab_])
                    yield
                    for j in range(3):
                        S.op("dve", lambda e, xa=xa, xs=xs, h=h, j=j: e.scalar_tensor_tensor(out=xa[:], in0=xs[:, 1 + j:TB + 1 + j], scalar=lp[:, h, j:j + 1], in1=xa[:], op0=ALU.mult, op1=ALU.add), reads=[xsb, xab_], writes=[xab_])
                        yield
                    xq, xqb = Lxab.next()
                    S.op("act", lambda e, xq=xq, xa=xa: e.copy(out=xq[:], in_=xa[:]), reads=[xab_], writes=[xqb])
                    yield
                    pr, prb = PS.next()
                    S.op("pe", lambda e, pr=pr, xq=xq, h=h: e.matmul(pr[:], lhsT=wab[:, h, 0, :], rhs=xq[:], start=True, stop=True), reads=[xqb], writes=[prb])
                    yield
                    pi, pib = PS.next()
                    S.op("pe", lambda e, pi=pi, xq=xq, h=h: e.matmul(pi[:], lhsT=wab[:, h, 1, :], rhs=xq[:], start=True, stop=True), reads=[xqb], writes=[pib])
                    yield
                    r, rb = L["r"].next()
                    S.op("act", lambda e, r=r, pr=pr, h=h: e.activation(out=r[:], in_=pr[:], func=AF.Sigmoid, bias=lp[:, h, 5:6]), reads=[prb], writes=[rb])
                    yield
                    ig, igb = L["i"].next()
                    S.op("act", lambda e, ig=ig, pi=pi, h=h: e.activation(out=ig[:], in_=pi[:], func=AF.Sigmoid, bias=lp[:, h, 6:7]), reads=[pib], writes=[igb])
                    yield
                    a, ab = L["a"].next()
                    S.op("act", lambda e, a=a, r=r, h=h: e.activation(out=a[:], in_=r[:], func=AF.Exp, scale=lrc[:, 0, h:h + 1]), reads=[rb], writes=[ab])
                    yield
                    m, mb = L["m"].next()
                    S.op("act", lambda e, m=m, r=r, h=h: e.activation(out=m[:], in_=r[:], func=AF.Exp, scale=lrc[:, 1, h:h + 1]), reads=[rb], writes=[mb])
                    yield
                    S.op("act", lambda e, m=m: e.activation(out=m[:], in_=m[:], func=AF.Sqrt, scale=-1.0, bias=1.0), reads=[mb], writes=[mb])
                    yield
                    S.op("dve", lambda e, m=m, blk=blk: e.tensor_scalar(out=m[:, 0:1], in0=m[:, 0:1], scalar1=flg[:, 1, blk:blk + 1], scalar2=flg[:, 0, blk:blk + 1], op0=ALU.mult, op1=ALU.add), reads=[mb], writes=[mb])
                    yield
                    S.op("dve", lambda e, m=m, ig=ig: e.tensor_tensor(out=m[:], in0=m[:], in1=ig[:], op=ALU.mult), reads=[mb, igb], writes=[mb])
                    yield
                    S.op("dve", lambda e, m=m, xa=xa: e.tensor_tensor(out=m[:], in0=m[:], in1=xa[:], op=ALU.mult), reads=[mb, xab_], writes=[mb])
                    yield
                    ini, inib = Lin.next()
                    S.op("dve", lambda e, ini=ini, h=h, blk=blk: e.tensor_tensor(out=ini[:], in0=hst[:, h:h + 1], in1=flg[:, 1, blk:blk + 1], op=ALU.mult), reads=[hst_b[h]], writes=[inib])
                    yield
                    hh, hb = L["h"].next()
                    S.op("dve", lambda e, hh=hh, a=a, m=m, ini=ini: e.tensor_tensor_scan(out=hh[:], data0=a[:], data1=m[:], initial=ini[:], op0=ALU.mult, op1=ALU.add), reads=[ab, mb, inib], writes=[hb])
                    yield
                    S.op("pool", lambda e, hh=hh, h=h: e.tensor_copy(out=hst[:, h:h + 1], in_=hh[:, TB - 1:TB]), reads=[hb], writes=[hst_b[h]])
                    yield
                    if main:
                        py, pyb = PS.next()
                        mmgroup(py[:], pyb, [(w[:, 1, kc, :], xb[:, kc, :]) for kc in range(16)], [wb_, xbb])
                        yield
                        gy, gyb = L["gy"].next()
                        S.op("act", lambda e, gy=gy, py=py: e.activation(out=gy[:], in_=py[:], func=AF.Gelu_apprx_tanh), reads=[pyb], writes=[gyb])
                        yield
                        k_ = uacnt[0] % 2
                        uacnt[0] += 1
                        ut, utb = uar.next()
                        S.op("dve", lambda e, gy=gy, hh=hh, ut=ut: e.tensor_tensor(out=ut[:], in0=hh[:], in1=gy[:], op=ALU.mult), reads=[hb, gyb], writes=[utb])
                        yield
                        S.dma("act", lambda e, ut=ut, h=h, blk=blk: e.dma_start(out=UA[blk - 12, :, h, :], in_=ut[:]), uas[k_], reads=[utb], writes=[])
                        yield

                for h0 in range(0, _LH, 2):
                    gens = [lru_head(h) for h in range(h0, min(h0 + 2, _LH))]
                    while gens:
                        for g_ in list(gens):
                            try:
                                next(g_)
                            except StopIteration:
                                gens.remove(g_)

                for h in range(_RH):
                    gc = GAMMA[h] ** 128
                    wts = {}
                    for j, nm in enumerate(("q", "k", "v0", "v1", "g0", "g1")):
                        if not main and nm in ("q", "g0", "g1"):
                            continue
                        w, wb_, ws = wq.next() if nm in ("q", "k") else (None, None, None)
                        if nm in ("q", "k"):
                            S.dma("sp", lambda e, w=w, h=h, j=j: e.dma_start(out=w[:], in_=b_in_r[h, j]), ws, reads=[dr_w], writes=[wb_])
                            wts[nm] = (w, wb_)
                    def proj_rope(nm, ring):
                        w, wb_ = wts[nm]
                        p0, p0b = PS.next()
                        mmgroup(p0[:], p0b, [(w[:, kc, 0:128], xb[:, kc, :]) for kc in range(16)], [wb_, xbb])
                        p1, p1b = PS.next()
                        mmgroup(p1[:], p1b, [(w[:, kc, 128:256], xb[:, kc, :]) for kc in range(16)], [wb_, xbb])
                        o, ob = ring.next()
                        t1, t1b = Rt.next()
                        t2, t2b = Rt.next()
                        S.op("dve", lambda e: e.tensor_tensor(out=t1[:], in0=p0[:], in1=ct[:, 0, :], op=ALU.mult), reads=[p0b, ctb], writes=[t1b])
                        S.op("dve", lambda e: e.tensor_tensor(out=t2[:], in0=p1[:], in1=ct[:, 1, :], op=ALU.mult), reads=[p1b, ctb], writes=[t2b])
                        S.op("pool", lambda e: e.tensor_tensor(out=o[:, 0, :], in0=t1[:], in1=t2[:], op=ALU.subtract), reads=[t1b, t2b], writes=[ob])
                        t3, t3b = Rt.next()
                        t4, t4b = Rt.next()
                        S.op("dve", lambda e: e.tensor_tensor(out=t3[:], in0=p0[:], in1=ct[:, 1, :], op=ALU.mult), reads=[p0b, ctb], writes=[t3b])
                        S.op("dve", lambda e: e.tensor_tensor(out=t4[:], in0=p1[:], in1=ct[:, 0, :], op=ALU.mult), reads=[p1b, ctb], writes=[t4b])
                        S.op("pool", lambda e: e.tensor_tensor(out=o[:, 1, :], in0=t3[:], in1=t4[:], op=ALU.add), reads=[t3b, t4b], writes=[ob])
                        return o, ob

                    kr, krb = proj_rope("k", Rk)
                    if main:
                        qr, qrb = proj_rope("q", Rq)
                    wv = []
                    for j in (2, 3):
                        w, wb_, ws = wq.next()
                        S.dma("sp", lambda e, w=w, h=h, j=j: e.dma_start(out=w[:], in_=b_in_r[h, j]), ws, reads=[dr_w], writes=[wb_])
                        wv.append((w, wb_))
                    wg_ = []
                    if main:
                        gt, gtb, gts = gn.next()
                        S.dma("act", lambda e, gt=gt, h=h: e.dma_start(out=gt[:], in_=gain_bc[:, h * 512:(h + 1) * 512]), gts, writes=[gtb])
                    if main:
                        k_ = uacnt[1] % 2
                        uacnt[1] += 1
                        ubt, ubtb = ubr.next()
                    if main:
                        for j in (4, 5):
                            w, wb_, ws = wq.next()
                            S.dma("sp", lambda e, w=w, h=h, j=j: e.dma_start(out=w[:], in_=b_in_r[h, j]), ws, reads=[dr_w], writes=[wb_])
                            wg_.append((w, wb_))

                    def chunk(c):
                        tok = slice(c * 128, (c + 1) * 128)
                        pv, pvb = PS.next()
                        for hf in range(2):
                            n = 16
                            S.group("pe", [
                                (lambda e, kc=kc, hf=hf: e.matmul(pv[:, hf * 256:(hf + 1) * 256], lhsT=xb[:, kc, tok], rhs=wv[hf][0][:, kc, :], start=(kc == 0), stop=(kc == 15)))
                                for kc in range(16)], reads=[xbb, wv[hf][1]], writes=[pvb])
                            yield
                        vb, vbb = Rv.next()
                        S.op("act", lambda e, vb=vb, pv=pv: e.copy(out=vb[:], in_=pv[:]), reads=[pvb], writes=[vbb])
                        yield
                        kz, kzb = Rkz.next()
                        pt, ptb_ = PT.next()
                        S.group("pe", [(lambda e, pt=pt, j=j: e.transpose(pt[:, j, :], kr[:, j, tok], idb[:])) for j in range(2)], reads=[krb], writes=[ptb_])
                        yield
                        S.op("dve", lambda e, pt=pt, kz=kz: e.tensor_scalar(out=kz[:].rearrange("p (a b) -> p a b", b=128), in0=pt[:, 0:2, :], scalar1=xz[:, 1, h:h + 1], scalar2=None, op0=ALU.mult), reads=[ptb_], writes=[kzb])
                        yield
                        if main:
                            sbf, sbfb = Sb.next()
                            S.op("pool", lambda e, sbf=sbf: e.tensor_copy(out=sbf[:], in_=Sst[:, h]), reads=[S_b[h]], writes=[sbfb])
                            yield
                        for j in range(2):
                            pd, pdb = PS.next()
                            S.op("pe", lambda e, pd=pd, kz=kz, vb=vb, j=j: e.matmul(pd[:], lhsT=kz[:, j * 128:(j + 1) * 128], rhs=vb[:], start=True, stop=True), reads=[kzb, vbb], writes=[pdb])
                            yield
                            S.op("dve", lambda e, pd=pd, j=j: e.scalar_tensor_tensor(out=Sst[:, h, j, :], in0=Sst[:, h, j, :], scalar=gc, in1=pd[:], op0=ALU.mult, op1=ALU.add), reads=[pdb, S_b[h]], writes=[S_b[h]])
                            yield
                        yield "S"
                        if main:
                            psc, pscb = PS.next()
                            mmgroup(psc[:, 0:128], pscb, [(kr[:, j, tok], qr[:, j, tok]) for j in range(2)], [krb, qrb])
                            yield
                            sm, smb = Rs.next()
                            S.op("dve", lambda e, sm=sm, psc=psc: e.tensor_tensor(out=sm[:], in0=psc[:, 0:128], in1=dmk[:, h, :], op=ALU.mult), reads=[pscb], writes=[smb])
                            yield
                            po, pob = PS.next()
                            S.op("pe", lambda e, po=po, sm=sm, vb=vb: e.matmul(po[:], lhsT=sm[:], rhs=vb[:], start=True, stop=True), reads=[smb, vbb], writes=[pob])
                            yield
                            pc, pcb = PS.next()
                            mmgroup(pc[:], pcb, [(qr[:, j, tok], sbf[:, j, :]) for j in range(2)], [qrb, sbfb])
                            yield
                            o1, o1b = Ro.next()
                            S.op("act", lambda e, o1=o1, po=po: e.copy(out=o1[:], in_=po[:]), reads=[pob], writes=[o1b])
                            yield
                            o2, o2b = Ro.next()
                            S.op("dve", lambda e, o2=o2, pc=pc, o1=o1: e.scalar_tensor_tensor(out=o2[:], in0=pc[:], scalar=xz[:, 0, h:h + 1], in1=o1[:], op0=ALU.mult, op1=ALU.add), reads=[pcb, o1b], writes=[o2b])
                            yield
                            stt, sttb = Rst.next()
                            S.op("dve", lambda e, stt=stt, o2=o2: e.bn_stats(out=stt[:, 0:6], in_=o2[:]), reads=[o2b], writes=[sttb])
                            yield
                            S.op("dve", lambda e, stt=stt: e.bn_aggr(out=stt[:, 6:8], in_=stt[:, 0:6]), reads=[sttb], writes=[sttb])
                            yield
                            S.op("dve", lambda e, stt=stt: e.tensor_scalar(out=stt[:, 7:8], in0=stt[:, 7:8], scalar1=LN_EPS, scalar2=None, op0=ALU.add), reads=[sttb], writes=[sttb])
                            yield
                            S.op("act", lambda e, stt=stt: e.activation(out=stt[:, 7:8], in_=stt[:, 7:8], func=AF.Sqrt), reads=[sttb], writes=[sttb])
                            yield
                            S.op("dve", lambda e, stt=stt: e.reciprocal(out=stt[:, 7:8], in_=stt[:, 7:8]), reads=[sttb], writes=[sttb])
                            yield
                            S.op("dve", lambda e, stt=stt, o2=o2: e.tensor_scalar(out=o2[:], in0=o2[:], scalar1=stt[:, 6:7], scalar2=stt[:, 7:8], op0=ALU.subtract, op1=ALU.mult), reads=[sttb, o2b], writes=[o2b])
                            yield
                            S.op("pool", lambda e, o2=o2, gt=gt: e.tensor_tensor(out=o2[:], in0=o2[:], in1=gt[:], op=ALU.mult), reads=[o2b, gtb], writes=[o2b])
                            yield
                            pg, pgb = PS.next()
                            for hf in range(2):
                                S.group("pe", [
                                    (lambda e, kc=kc, hf=hf: e.matmul(pg[:, hf * 256:(hf + 1) * 256], lhsT=xb[:, kc, tok], rhs=wg_[hf][0][:, kc, :], start=(kc == 0), stop=(kc == 15)))
                                    for kc in range(16)], reads=[xbb, wg_[hf][1]], writes=[pgb])
                                yield
                            sg, sgb = Rsg.next()
                            S.op("act", lambda e, sg=sg, pg=pg: e.activation(out=sg[:], in_=pg[:], func=AF.Silu), reads=[pgb], writes=[sgb])
                            yield
                            ub, ubb = Rub.next()
                            S.op("dve", lambda e, ub=ub, o2=o2, sg=sg: e.tensor_tensor(out=ub[:], in0=o2[:], in1=sg[:], op=ALU.mult), reads=[o2b, sgb], writes=[ubb])
                            yield
                            pt, ptb_ = PT.next()
                            S.group("pe", [(lambda e, pt=pt, ub=ub, q4=q4: e.transpose(pt[:, q4, :], ub[:, q4 * 128:(q4 + 1) * 128], idb[:])) for q4 in range(4)], reads=[ubb], writes=[ptb_])
                            yield
                            S.op("act", lambda e, pt=pt: e.copy(out=ubt[:, :, tok], in_=pt[:, 0:4, :]), reads=[ptb_], writes=[ubtb])
                            yield

                    if True:
                        for c0 in (0, 2):
                            g0_ = chunk(c0)
                            if main:
                                while next(g0_) != "S":
                                    pass
                            gens = [g0_, chunk(c0 + 1)]
                            while gens:
                                for g_ in list(gens):
                                    try:
                                        next(g_)
                                    except StopIteration:
                                        gens.remove(g_)
                    if main:
                        S.dma("act", lambda e, ubt=ubt, h=h, blk=blk: e.dma_start(out=UB[blk - 12, :, h * 4:(h + 1) * 4, :], in_=ubt[:]), ubs[k_], reads=[ubtb], writes=[])
            S.barrier()

        with ExitStack() as st:
            sb = lambda n, s, d=F32: st.enter_context(nc.sbuf_tensor(n, list(s), d))
            cq = S.dma_src("cq1b")
            bgt = sb("bgt2", [128, 2, 16])
            cb_ = Buf("consts1b")
            S.dma("sp", lambda e: e.dma_start(out=bgt[:], in_=bg), cq, writes=[cb_])
            xstg = DRing(S, nc, st, "xstg2", 2, [128, 2, TB], F32)
            xTb = Ring(nc, st, "xTb2", 1, [128, 16, TB], BF16)
            uaT = sb("uaT", [128, 16, TB], BF16)
            ubT = sb("ubT", [128, 32, TB], BF16)
            mxT = sb("mxT", [128, 16, TB], BF16)
            ua_b, ub_b, mx_b = Buf("uaT"), Buf("ubT"), Buf("mxT")
            uq = S.dma_src("uq")
            uq2 = S.dma_src("uq2")
            w3r = DRing(S, nc, st, "w3r", 5, [128, 20, 128], BF16)
            wor = DRing(S, nc, st, "wor", 3, [128, 8, 512], BF16)
            xrr = DRing(S, nc, st, "xrr", 1, [128, 4, D], F32)
            lnt = sb("lnt", [128, 2, D])
            lnb = cb_
            S.dma("act", lambda e: e.dma_start(out=lnt[:], in_=lnp[:, 0:2, :]), cq, writes=[lnb])
            G1 = Ring(nc, st, "g1", 1, [128, TB], F32)
            G2 = Ring(nc, st, "g2", 1, [128, TB], F32)
            Zst = Ring(nc, st, "zst", 2, [128, 32], F32)
            hsrc = S.dma_src("hs")

            def mmgroup(ps, psb, pairs, reads):
                n = len(pairs)
                S.group("pe", [
                    (lambda e, l=l, r=r, i=i: e.matmul(ps, lhsT=l, rhs=r, start=(i == 0), stop=(i == n - 1)))
                    for i, (l, r) in enumerate(pairs)], reads=reads, writes=[psb])

            for bi in range(4 if "1b" in _PH else 0):
                xb, xbb = xTb.next()
                for hf in range(8):
                    t, tb, ts = xstg.next()
                    S.dma("sp", lambda e, t=t, hf=hf, bi=bi: e.dma_start(out=t[:], in_=xT[12 + bi, :, hf * 2:(hf + 1) * 2, :]), ts, writes=[tb])
                    S.op("pool", lambda e, t=t, xb=xb, hf=hf: e.tensor_copy(out=xb[:, hf * 2:(hf + 1) * 2, :], in_=t[:]), reads=[tb], writes=[xbb])
                S.dma("act", lambda e, bi=bi: e.dma_start(out=uaT[:], in_=UA[bi]), uq, writes=[ua_b])
                S.dma("act", lambda e, bi=bi: e.dma_start(out=ubT[:], in_=UB[bi]), uq2, writes=[ub_b])
                for nt in range(16):
                    pcs = []
                    for q_ in range(4):
                        w, wb_, ws = w3r.next()
                        S.dma("sp", lambda e, w=w, nt=nt, q_=q_: e.dma_start(out=w[:], in_=b_w3[nt, :, q_ * 20:(q_ + 1) * 20, :]), ws, writes=[wb_])
                        pcs.append((w, wb_))
                    wsl = lambda kc: pcs[kc // 20][0][:, kc % 20, :]
                    wbs = lambda lo, hi: [pcs[q_][1] for q_ in range(lo // 20, (hi - 1) // 20 + 1)]
                    pa, pab = PS.next()
                    mmgroup(pa[:], pab, [(wsl(kc), uaT[:, kc, :]) for kc in range(16)], wbs(0, 16) + [ua_b])
                    pb, pbb = PS.next()
                    mmgroup(pb[:], pbb, [(wsl(16 + kc), ubT[:, kc, :]) for kc in range(32)], wbs(16, 48) + [ub_b])
                    pga, pgab = PS.next()
                    mmgroup(pga[:], pgab, [(wsl(48 + kc), xb[:, kc, :]) for kc in range(16)], wbs(48, 64) + [xbb])
                    pgb_, pgbb = PS.next()
                    mmgroup(pgb_[:], pgbb, [(wsl(64 + kc), xb[:, kc, :]) for kc in range(16)], wbs(64, 80) + [xbb])
                    s1, s1b = G1.next()
                    S.op("act", lambda e, s1=s1, pga=pga, nt=nt: e.activation(out=s1[:], in_=pga[:], func=AF.Sigmoid, bias=bgt[:, 0, nt:nt + 1]), reads=[pgab], writes=[s1b])
                    s2, s2b = G2.next()
                    S.op("act", lambda e, s2=s2, pgb_=pgb_, nt=nt: e.activation(out=s2[:], in_=pgb_[:], func=AF.Sigmoid, bias=bgt[:, 1, nt:nt + 1]), reads=[pgbb], writes=[s2b])
                    S.op("dve", lambda e, s1=s1, pa=pa: e.tensor_tensor(out=s1[:], in0=s1[:], in1=pa[:], op=ALU.mult), reads=[s1b, pab], writes=[s1b])
                    S.op("dve", lambda e, s2=s2, pb=pb: e.tensor_tensor(out=s2[:], in0=s2[:], in1=pb[:], op=ALU.mult), reads=[s2b, pbb], writes=[s2b])
                    S.op("pool", lambda e, s1=s1, s2=s2, nt=nt: e.tensor_tensor(out=mxT[:, nt, :], in0=s1[:], in1=s2[:], op=ALU.add), reads=[s1b, s2b], writes=[mx_b])
                xr, xrb, xrs = xrr.next()
                S.dma("act", lambda e, xr=xr, bi=bi: e.dma_start(out=xr[:], in_=xres[bi * TB:(bi + 1) * TB, :].rearrange("(t p) d -> p t d", p=128)), xrs, reads=[dr_H], writes=[xrb])
                for nb in range(4):
                    pcs = []
                    for q_ in range(2):
                        w, wb_, ws = wor.next()
                        S.dma("sp", lambda e, w=w, nb=nb, q_=q_: e.dma_start(out=w[:], in_=b_wo[nb, :, q_ * 8:(q_ + 1) * 8, :]), ws, writes=[wb_])
                        pcs.append((w, wb_))
                    for tt in range(4):
                        pz, pzb = PS.next()
                        mmgroup(pz[:], pzb, [(mxT[:, kc, tt * 128:(tt + 1) * 128], pcs[kc // 8][0][:, kc % 8, :]) for kc in range(16)], [pcs[0][1], pcs[1][1], mx_b])
                        S.op("dve", lambda e, xr=xr, pz=pz, tt=tt, nb=nb: e.scalar_tensor_tensor(out=xr[:, tt, nb * 512:(nb + 1) * 512], in0=xr[:, tt, nb * 512:(nb + 1) * 512], scalar=DN_ALPHA, in1=pz[:], op0=ALU.mult, op1=ALU.add), reads=[pzb, xrb], writes=[xrb])
                for tt in range(4):
                    zs, zsb = Zst.next()
                    for q4 in range(4):
                        S.op("dve", lambda e, zs=zs, xr=xr, tt=tt, q4=q4: e.bn_stats(out=zs[:, q4 * 6:(q4 + 1) * 6], in_=xr[:, tt, q4 * 512:(q4 + 1) * 512]), reads=[xrb], writes=[zsb])
                    S.op("dve", lambda e, zs=zs: e.bn_aggr(out=zs[:, 24:26], in_=zs[:, 0:24]), reads=[zsb], writes=[zsb])
                    S.op("dve", lambda e, zs=zs: e.tensor_scalar(out=zs[:, 25:26], in0=zs[:, 25:26], scalar1=LN_EPS, scalar2=None, op0=ALU.add), reads=[zsb], writes=[zsb])
                    S.op("act", lambda e, zs=zs: e.activation(out=zs[:, 25:26], in_=zs[:, 25:26], func=AF.Sqrt), reads=[zsb], writes=[zsb])
                    S.op("dve", lambda e, zs=zs: e.reciprocal(out=zs[:, 25:26], in_=zs[:, 25:26]), reads=[zsb], writes=[zsb])
                    S.op("dve", lambda e, zs=zs, xr=xr, tt=tt: e.tensor_scalar(out=xr[:, tt, :], in0=xr[:, tt, :], scalar1=zs[:, 24:25], scalar2=zs[:, 25:26], op0=ALU.subtract, op1=ALU.mult), reads=[zsb, xrb], writes=[xrb])
                    S.op("pool", lambda e, xr=xr, tt=tt: e.tensor_tensor(out=xr[:, tt, :], in0=xr[:, tt, :], in1=lnt[:, 0, :], op=ALU.mult), reads=[xrb, lnb], writes=[xrb])
                    S.op("pool", lambda e, xr=xr, tt=tt: e.tensor_tensor(out=xr[:, tt, :], in0=xr[:, tt, :], in1=lnt[:, 1, :], op=ALU.add), reads=[xrb, lnb], writes=[xrb])
                S.dma("sp", lambda e, xr=xr, bi=bi: e.dma_start(out=Hs[bi * TB:(bi + 1) * TB, :].rearrange("(t p) d -> p t d", p=128), in_=xr[:]), hsrc, reads=[xrb], writes=[dr_H])
            S.barrier()

        CAP = 512
        with ExitStack() as st2:
            sbp = lambda n, s, d=F32: st2.enter_context(nc.sbuf_tensor(n, list(s), d))
            cq2 = S.dma_src("cq2")
            c2b = Buf("consts2")
            idf2 = sbp("idf2", [128, 128])
            idb2 = sbp("idb2", [128, 128], BF16)
            ust = sbp("ust", [128, 2, 128])
            io512 = sbp("io512", [128, 512])
            io64 = sbp("io64", [128, 2, 64])
            tokc = sbp("tokc", [128, 16, 4])
            tokb = sbp("tokb", [128, 16, 4], BF16)
            wall = sbp("wall", [128, 16, 64])
            selall = sbp("selall", [128, 16, 64])
            posall = sbp("posall", [128, 16, 64])
            eidf = sbp("eidf", [128, 16, 8])
            carry2 = sbp("carry2", [128, 64])
            rt_b = Buf("routing")
            for t_, a_ in ((idf2, ident), (ust, ustrict), (io512, iota512), (io64, iota64), (tokc, tokcol)):
                S.dma("sp", lambda e, t_=t_, a_=a_: e.dma_start(out=t_[:], in_=a_), cq2, writes=[c2b])
            S.op("dve", lambda e: e.tensor_copy(out=idb2[:], in_=idf2[:]), reads=[c2b], writes=[c2b])
            S.op("dve", lambda e: e.tensor_copy(out=tokb[:], in_=tokc[:]), reads=[c2b], writes=[c2b])
            S.op("pool", lambda e: e.memset(carry2[:], 0.0), writes=[rt_b])

            def mmgroup(ps, psb, pairs, reads):
                n = len(pairs)
                S.group("pe", [
                    (lambda e, l=l, r=r, i=i: e.matmul(ps, lhsT=l, rhs=r, start=(i == 0), stop=(i == n - 1)))
                    for i, (l, r) in enumerate(pairs)], reads=reads, writes=[psb])

            def ffn(xT_, xTb_, wg, wgb, wu, wub, wd, wdb, h1, h1b, sgr, emit_y):
                for ft in range(4):
                    pg, pgb = PS.next()
                    mmgroup(pg[:], pgb, [(wg[:, kc, ft * 128:(ft + 1) * 128], xT_[:, kc, :]) for kc in range(16)], [wgb, xTb_])
                    pu, pub = PS.next()
                    mmgroup(pu[:], pub, [(wu[:, kc, ft * 128:(ft + 1) * 128], xT_[:, kc, :]) for kc in range(16)], [wub, xTb_])
                    sg, sgb = sgr.next()
                    S.op("act", lambda e, sg=sg, pg=pg: e.activation(out=sg[:], in_=pg[:], func=AF.Silu), reads=[pgb], writes=[sgb])
                    S.op("dve", lambda e, h1=h1, sg=sg, pu=pu, ft=ft: e.tensor_tensor(out=h1[:, ft, :], in0=sg[:], in1=pu[:], op=ALU.mult), reads=[sgb, pub], writes=[h1b])
                for tt in range(4):
                    for nb in range(4):
                        py, pyb = PS.next()
                        mmgroup(py[:], pyb, [(h1[:, ft, tt * 128:(tt + 1) * 128], wd[:, ft, nb * 512:(nb + 1) * 512]) for ft in range(4)], [h1b, wdb])
                        emit_y(tt, nb, py, pyb)

            with ExitStack() as st:
                sb = lambda n, s, d=F32: st.enter_context(nc.sbuf_tensor(n, list(s), d))
                wrt = sb("wrt", [128, 16, 64])
                rbt = sb("rbt", [128, 64])
                cq2b = S.dma_src("cq2b")
                cq2c = S.dma_src("cq2c")
                swq = S.dma_src("swq")
                r2b = Buf("rconsts")
                for t_, a_ in ((wrt, wr), (rbt, rb_bc)):
                    S.dma("sp", lambda e, t_=t_, a_=a_: e.dma_start(out=t_[:], in_=a_), cq2b, writes=[r2b])
                zr = sb("zr", [128, D], BF16)
                zrb = Buf("zr")
                S.op("pool", lambda e: e.memset(zr[:], 0.0), writes=[zrb])
                S.dma("sp", lambda e: e.dma_start(out=Hb[NB:NB + 128, :], in_=zr[:]), cq2c, reads=[zrb], writes=[])
                hT = Ring(nc, st, "hT32", 1, [128, 16, 128], F32)
                hld = DRing(S, nc, st, "hld", 1, [128, 4, D], F32)
                hbr = Ring(nc, st, "hbr", 2, [128, D], BF16)
                hbs = [S.dma_src("hbs%d" % k) for k in range(2)]
                R1 = Ring(nc, st, "rt1", 2, [128, 64], F32)
                R2 = Ring(nc, st, "rt2", 2, [128, 64], F32)
                R3 = Ring(nc, st, "rt3", 2, [128, 64], F32)
                R8 = Ring(nc, st, "rt8", 2, [128, 4, 8], F32)
                RI = Ring(nc, st, "rti", 2, [128, 8], mybir.dt.uint32)
                hTb = sb("hTb", [128, 16, TB], BF16)
                hTb_b = Buf("hTb")
                h1T = Ring(nc, st, "h1T", 1, [128, 4, TB], BF16)
                Rsg2 = Ring(nc, st, "sg2", 2, [128, TB], F32)
                swg = sb("swg", [128, 16, 512], BF16)
                swu = sb("swu", [128, 16, 512], BF16)
                swd = sb("swd", [128, 4, D], BF16)
                swb = Buf("sw")
                S.dma("sp", lambda e: e.dma_start(out=swg[:], in_=b_wge[0]), swq, writes=[swb])
                S.dma("act", lambda e: e.dma_start(out=swu[:], in_=b_wue[0]), swq, writes=[swb])
                S.dma("sp", lambda e: e.dma_start(out=swd[:], in_=b_wde[0]), swq, writes=[swb])
                ysr = Ring(nc, st, "ysr", 2, [128, D], F32)
                yss = [S.dma_src("yss%d" % k) for k in range(2)]
                hcnt = [0, 0]
                for bi in range(4 if "2" in _PH else 0):
                    hx, hxb, hxs = hld.next()
                    S.dma("sp", lambda e, hx=hx, bi=bi: e.dma_start(out=hx[:], in_=Hs[bi * TB:(bi + 1) * TB, :].rearrange("(t p) d -> p t d", p=128)), hxs, writes=[hxb])
                    for tt in range(4):
                        tg = bi * 4 + tt
                        k_ = hcnt[0] % 2
                        hcnt[0] += 1
                        hb_, hbb = hbr.next()
                        S.op("pool", lambda e, hb_=hb_, hx=hx, tt=tt: e.tensor_copy(out=hb_[:], in_=hx[:, tt, :]), reads=[hxb], writes=[hbb])
                        S.dma("act", lambda e, hb_=hb_, tg=tg: e.dma_start(out=Hb[tg * 128:(tg + 1) * 128, :], in_=hb_[:]), hbs[k_], reads=[hbb], writes=[])
                        h32, h32b = hT.next()
                        for k4 in range(4):
                            pp, ppb = PS.next()
                            S.group("pe", [(lambda e, pp=pp, hx=hx, tt=tt, kc=k4 * 4 + q4, q4=q4: e.transpose(pp[:, q4 * 128:(q4 + 1) * 128], hx[:, tt, kc * 128:(kc + 1) * 128], idf2[:])) for q4 in range(4)], reads=[hxb, c2b], writes=[ppb])
                            S.op("act", lambda e, pp=pp, h32=h32, k4=k4: e.copy(out=h32[:, k4 * 4:(k4 + 1) * 4, :], in_=pp[:].rearrange("p (a b) -> p a b", b=128)), reads=[ppb], writes=[h32b])
                            S.op("pool", lambda e, h32=h32, k4=k4, tt=tt: e.tensor_copy(out=hTb[:, k4 * 4:(k4 + 1) * 4, tt * 128:(tt + 1) * 128], in_=h32[:, k4 * 4:(k4 + 1) * 4, :]), reads=[h32b], writes=[hTb_b])
                        pl, plb = PS.next()
                        mmgroup(pl[:, 0:64], plb, [(h32[:, kc, :], wrt[:, kc, :]) for kc in range(16)], [h32b, r2b])
                        sc, scb = R1.next()
                        S.op("act", lambda e, sc=sc, pl=pl: e.activation(out=sc[:], in_=pl[:, 0:64], func=AF.Sigmoid), reads=[plb], writes=[scb])
                        bs, bsb = R2.next()
                        S.op("dve", lambda e, bs=bs, sc=sc: e.tensor_tensor(out=bs[:], in0=sc[:], in1=rbt[:], op=ALU.add), reads=[scb, r2b], writes=[bsb])
                        t8, t8b = R3.next()
                        for g in range(8):
                            S.op("dve", lambda e, t8=t8, bs=bs, g=g: e.max(out=t8[:, g * 8:(g + 1) * 8], in_=bs[:, g * 8:(g + 1) * 8]), reads=[bsb], writes=[t8b])
                        sm8, sm8b = R8.next()
                        t8v = t8[:].rearrange("p (g k) -> p g k", k=8)
                        S.op("dve", lambda e, sm8=sm8, t8v=t8v: e.tensor_tensor(out=sm8[:, 0, :], in0=t8v[:, :, 0], in1=t8v[:, :, 1], op=ALU.add), reads=[t8b], writes=[sm8b])
                        S.op("dve", lambda e, sm8=sm8: e.max(out=sm8[:, 1, :], in_=sm8[:, 0, :]), reads=[sm8b], writes=[sm8b])
                        S.op("dve", lambda e, sm8=sm8: e.tensor_scalar(out=sm8[:, 2, :], in0=sm8[:, 0, :], scalar1=sm8[:, 1, 3:4], scalar2=None, op0=ALU.is_ge), reads=[sm8b], writes=[sm8b])
                        S.op("dve", lambda e, sm8=sm8: e.tensor_scalar(out=sm8[:, 2, :], in0=sm8[:, 2, :], scalar1=-1.0, scalar2=1e9, op0=ALU.add, op1=ALU.mult), reads=[sm8b], writes=[sm8b])
                        bm, bmb = R3.next()
                        S.op("dve", lambda e, bm=bm, bs=bs, sm8=sm8: e.tensor_tensor(out=bm[:].rearrange("p (g k) -> p g k", k=8), in0=bs[:].rearrange("p (g k) -> p g k", k=8), in1=sm8[:, 2, :].unsqueeze(2).to_broadcast([128, 8, 8]), op=ALU.add), reads=[bsb, sm8b], writes=[bmb])
                        S.op("dve", lambda e, sm8=sm8, bm=bm: e.max(out=sm8[:, 3, :], in_=bm[:]), reads=[bmb], writes=[sm8b])
                        ei, eib = RI.next()
                        S.op("dve", lambda e, ei=ei, sm8=sm8, bm=bm: e.max_index(out=ei[:], in_max=sm8[:, 3, :], in_values=bm[:]), reads=[bmb, sm8b], writes=[eib])
                        S.op("dve", lambda e, ei=ei, tg=tg: e.tensor_copy(out=eidf[:, tg, :], in_=ei[:]), reads=[eib], writes=[rt_b])
                        S.op("dve", lambda e, sm8=sm8, bm=bm, tg=tg: e.tensor_scalar(out=selall[:, tg, :], in0=bm[:], scalar1=sm8[:, 3, 7:8], scalar2=None, op0=ALU.is_ge), reads=[bmb, sm8b], writes=[rt_b])
                        S.op("dve", lambda e, bm=bm, sc=sc, tg=tg: e.tensor_tensor(out=bm[:], in0=selall[:, tg, :], in1=sc[:], op=ALU.mult), reads=[rt_b, scb, bmb], writes=[bmb])
                        S.op("dve", lambda e, sm8=sm8, bm=bm: e.reduce_sum(out=sm8[:, 1, 0:1], in_=bm[:], axis=AX.X), reads=[bmb], writes=[sm8b])
                        S.op("dve", lambda e, sm8=sm8: e.reciprocal(out=sm8[:, 1, 1:2], in_=sm8[:, 1, 0:1]), reads=[sm8b], writes=[sm8b])
                        S.op("dve", lambda e, sm8=sm8, bm=bm, tg=tg: e.tensor_scalar(out=wall[:, tg, :], in0=bm[:], scalar1=sm8[:, 1, 1:2], scalar2=2.5, op0=ALU.mult, op1=ALU.mult), reads=[bmb, sm8b], writes=[rt_b])
                        pq, pqb = PS.next()
                        S.op("pe", lambda e, pq=pq, tg=tg: e.matmul(pq[:, 0:64], lhsT=ust[:, 0, :], rhs=selall[:, tg, :], start=True, stop=True), reads=[rt_b, c2b], writes=[pqb])
                        S.op("dve", lambda e, pq=pq, tg=tg: e.tensor_tensor(out=posall[:, tg, :], in0=pq[:, 0:64], in1=carry2[:], op=ALU.add), reads=[pqb, rt_b], writes=[rt_b])
                        S.op("dve", lambda e, tg=tg: e.tensor_scalar(out=posall[:, tg, :], in0=posall[:, tg, :], scalar1=float(CAP - 1), scalar2=1.0, op0=ALU.min, op1=ALU.add), reads=[rt_b], writes=[rt_b])
                        S.op("dve", lambda e, tg=tg: e.tensor_tensor(out=posall[:, tg, :], in0=posall[:, tg, :], in1=selall[:, tg, :], op=ALU.mult), reads=[rt_b], writes=[rt_b])
                        S.op("dve", lambda e, tg=tg: e.tensor_scalar(out=posall[:, tg, :], in0=posall[:, tg, :], scalar1=-1.0, scalar2=None, op0=ALU.add), reads=[rt_b], writes=[rt_b])
                        pq2, pq2b = PS.next()
                        S.op("pe", lambda e, pq2=pq2, tg=tg: e.matmul(pq2[:, 0:64], lhsT=ust[:, 1, :], rhs=selall[:, tg, :], start=True, stop=True), reads=[rt_b, c2b], writes=[pq2b])
                        S.op("dve", lambda e, pq2=pq2: e.tensor_tensor(out=carry2[:], in0=pq2[:, 0:64], in1=carry2[:], op=ALU.add), reads=[pq2b, rt_b], writes=[rt_b])
                    h1, h1b = h1T.next()
                    ys_cur = {}

                    def emit_sh(tt, nb, py, pyb, bi=bi, ys_cur=ys_cur):
                        if nb == 0:
                            k_ = hcnt[1] % 2
                            hcnt[1] += 1
                            ys_cur["t"] = ysr.next() + (k_,)
                        yt, ytb, k_ = ys_cur["t"]
                        S.op("act", lambda e, yt=yt, py=py, nb=nb: e.copy(out=yt[:, nb * 512:(nb + 1) * 512], in_=py[:]), reads=[pyb], writes=[ytb])
                        if nb == 3:
                            tg = bi * 4 + tt
                            S.dma("act", lambda e, yt=yt, tg=tg: e.dma_start(out=YS[tg * 128:(tg + 1) * 128, :], in_=yt[:]), yss[k_], reads=[ytb], writes=[])

                    ffn(hTb, hTb_b, swg, swb, swu, swb, swd, swb, h1, h1b, Rsg2, emit_sh)
                S.barrier()

            with ExitStack() as st:
                wst = DRing(S, nc, st, "wst", 2, [128, 4096], F32)
                wgr = Ring(nc, st, "wgr", 3, [128, 16, 512], BF16)
                wdr2 = Ring(nc, st, "wdr2", 2, [128, 4, D], BF16)
                xgr = DRing(S, nc, st, "xgr", 2, [128, 4, D], BF16)
                xTr = Ring(nc, st, "xTr", 1, [128, 16, CAP], BF16)
                h1r = Ring(nc, st, "h1r", 1, [128, 4, CAP], BF16)
                sgr = Ring(nc, st, "sgr", 2, [128, CAP], F32)
                ysb = Ring(nc, st, "ysb", 1, [128, 4, D], BF16)
                ysd = S.dma_src("ysd")
                Qr = Ring(nc, st, "Qr", 2, [128, CAP], BF16)
                ixf = Ring(nc, st, "ixf", 2, [128, 4, 4], F32)
                ixu = Ring(nc, st, "ixu", 2, [128, 4], mybir.dt.uint32)
                ccnt = [0]

                def load_w(e_):
                    out = []
                    for (src, nk, ring) in ((wge, 16, wgr), (wue, 16, wgr), (wde, 4, wdr2)):
                        wt, wtb = ring.next()
                        flat = wt[:].rearrange("p a b -> p (a b)")
                        srcf = src[e_].rearrange("p a b -> p (a b)")
                        for hf in range(2):
                            t, tb, ts = wst.next()
                            S.dma("sp", lambda e, t=t, srcf=srcf, hf=hf: e.dma_start(out=t[:], in_=srcf[:, hf * 4096:(hf + 1) * 4096]), ts, writes=[tb])
                            eng = ("pool", "act")[ccnt[0] % 2]
                            ccnt[0] += 1
                            if eng == "act":
                                S.op("act", lambda e, t=t, flat=flat, hf=hf: e.copy(out=flat[:, hf * 4096:(hf + 1) * 4096], in_=t[:]), reads=[tb], writes=[wtb])
                            else:
                                S.op("pool", lambda e, t=t, flat=flat, hf=hf: e.tensor_copy(out=flat[:, hf * 4096:(hf + 1) * 4096], in_=t[:]), reads=[tb], writes=[wtb])
                        out += [wt, wtb]
                    return out

                def lists_and_gather(e_):
                    banks = [PS.next() for _ in range(4)]
                    for tg in range(16):
                        q, qb = Qr.next()
                        S.op("dve", lambda e, q=q, tg=tg, e_=e_: e.tensor_scalar(out=q[:], in0=io512[:], scalar1=posall[:, tg, e_:e_ + 1], scalar2=None, op0=ALU.is_equal), reads=[rt_b, c2b], writes=[qb])
                        for rg in range(4):
                            pb_, pbb_ = banks[rg]
                            S.op("pe", lambda e, pb_=pb_, q=q, rg=rg, tg=tg: e.matmul(pb_[:, 0:4], lhsT=q[:, rg * 128:(rg + 1) * 128], rhs=tokb[:, tg, :], start=(tg == 0), stop=(tg == 15)), reads=[qb, c2b], writes=[pbb_])
                    xf, xfb = ixf.next()
                    xu, xub = ixu.next()
                    for rg in range(4):
                        pb_, pbb_ = banks[rg]
                        S.op("dve", lambda e, xf=xf, pb_=pb_, rg=rg: e.tensor_copy(out=xf[:, rg, :], in_=pb_[:, 0:4]), reads=[pbb_], writes=[xfb])
                    S.op("dve", lambda e, xf=xf: e.scalar_tensor_tensor(out=xf[:, :, 0], in0=xf[:, :, 1], scalar=128.0, in1=xf[:, :, 0], op0=ALU.mult, op1=ALU.add), reads=[xfb], writes=[xfb])
                    S.op("dve", lambda e, xf=xf: e.tensor_scalar(out=xf[:, :, 2], in0=xf[:, :, 2], scalar1=-float(NB), scalar2=float(NB), op0=ALU.mult, op1=ALU.add), reads=[xfb], writes=[xfb])
                    S.op("dve", lambda e, xf=xf: e.tensor_tensor(out=xf[:, :, 0], in0=xf[:, :, 0], in1=xf[:, :, 2], op=ALU.add), reads=[xfb], writes=[xfb])
                    S.op("dve", lambda e, xf=xf: e.tensor_scalar(out=xf[:, :, 0], in0=xf[:, :, 0], scalar1=float(NB), scalar2=0.0, op0=ALU.min, op1=ALU.max), reads=[xfb], writes=[xfb])
                    S.op("dve", lambda e, xf=xf, xu=xu: e.tensor_copy(out=xu[:], in_=xf[:, :, 0]), reads=[xfb], writes=[xub])
                    xg, xgb, xgs = xgr.next()
                    for rg in range(4):
                        S.dma("pool", lambda e, xg=xg, xu=xu, rg=rg: e.indirect_dma_start(out=xg[:, rg, :], out_offset=None, in_=Hb, in_offset=bass.IndirectOffsetOnAxis(ap=xu[:, rg:rg + 1], axis=0)), xgs, reads=[xub], writes=[xgb])
                    return xg, xgb

                nexp = min(N_EXP, 64) if "2" in _PH else 0
                pend = lists_and_gather(0) if nexp else None
                for ex in range(nexp):
                    wg, wgb, wu, wub, wd, wdb = load_w(ex)
                    xg, xgb = pend
                    if ex + 1 < nexp:
                        pend = lists_and_gather(ex + 1)
                    xT_, xTb_ = xTr.next()
                    for rg in range(4):
                        for k8 in range(2):
                            pt, ptb_ = PT.next()
                            S.group("pe", [(lambda e, pt=pt, xg=xg, rg=rg, kc=k8 * 8 + j, j=j: e.transpose(pt[:, j, :], xg[:, rg, kc * 128:(kc + 1) * 128], idb2[:])) for j in range(8)], reads=[xgb, c2b], writes=[ptb_])
                            eng = "act" if (rg * 2 + k8) % 2 == 0 else "dve"
                            if eng == "act":
                                S.op("act", lambda e, pt=pt, xT_=xT_, rg=rg, k8=k8: e.copy(out=xT_[:, k8 * 8:(k8 + 1) * 8, rg * 128:(rg + 1) * 128], in_=pt[:]), reads=[ptb_], writes=[xTb_])
                            else:
                                S.op("dve", lambda e, pt=pt, xT_=xT_, rg=rg, k8=k8: e.tensor_copy(out=xT_[:, k8 * 8:(k8 + 1) * 8, rg * 128:(rg + 1) * 128], in_=pt[:]), reads=[ptb_], writes=[xTb_])
                    h1, h1b = h1r.next()
                    yt, ytb = ysb.next()

                    def emit_y(tt, nb, py, pyb, yt=yt, ytb=ytb):
                        if (tt * 4 + nb) % 2 == 0:
                            S.op("act", lambda e, yt=yt, py=py, tt=tt, nb=nb: e.copy(out=yt[:, tt, nb * 512:(nb + 1) * 512], in_=py[:]), reads=[pyb], writes=[ytb])
                        else:
                            S.op("dve", lambda e, yt=yt, py=py, tt=tt, nb=nb: e.tensor_copy(out=yt[:, tt, nb * 512:(nb + 1) * 512], in_=py[:]), reads=[pyb], writes=[ytb])

                    ffn(xT_, xTb_, wg, wgb, wu, wub, wd, wdb, h1, h1b, sgr, emit_y)
                    S.dma("act", lambda e, yt=yt, ex=ex: e.dma_start(out=Yx[ex * CAP:(ex + 1) * CAP, :].rearrange("(g p) n -> p g n", p=128), in_=yt[:]), ysd, reads=[ytb], writes=[])
                S.barrier()

            with ExitStack() as st:
                sb = lambda n, s, d=F32: st.enter_context(nc.sbuf_tensor(n, list(s), d))
                ln2 = sb("ln2", [128, 2, D])
                cq2d = S.dma_src("cq2d")
                l2b = Buf("ln2")
                S.dma("sp", lambda e: e.dma_start(out=ln2[:], in_=lnp[:, 2:4, :]), cq2d, writes=[l2b])
                hr = DRing(S, nc, st, "hr3", 2, [128, D], F32)
                yr = DRing(S, nc, st, "yr3", 2, [128, D], F32)
                gk = DRing(S, nc, st, "gk3", 4, [128, D], BF16)
                acc = Ring(nc, st, "acc3", 2, [128, D], F32)
                oh = Ring(nc, st, "oh3", 2, [128, 64], F32)
                sw = Ring(nc, st, "sw3", 2, [128, 2, 8], F32)
                su = Ring(nc, st, "su3", 2, [128, 8], mybir.dt.uint32)
                Zs2 = Ring(nc, st, "zs2", 2, [128, 32], F32)
                osr = [S.dma_src("os%d" % k) for k in range(2)]
                for tg in range(16 if "2" in _PH else 0):
                    ht, htb, hts = hr.next()
                    S.dma("sp", lambda e, ht=ht, tg=tg: e.dma_start(out=ht[:], in_=Hs[tg * 128:(tg + 1) * 128, :]), hts, writes=[htb])
                    yt, ytb, yts = yr.next()
                    S.dma("act", lambda e, yt=yt, tg=tg: e.dma_start(out=yt[:], in_=YS[tg * 128:(tg + 1) * 128, :]), yts, writes=[ytb])
                    S.op("dve", lambda e, tg=tg: e.tensor_tensor(out=posall[:, tg, :], in0=posall[:, tg, :], in1=io64[:, 1, :], op=ALU.add), reads=[rt_b, c2b], writes=[rt_b])
                    s_, s_b = sw.next()
                    for k in range(8):
                        o_, o_b = oh.next()
                        S.op("dve", lambda e, o_=o_, tg=tg, k=k: e.tensor_scalar(out=o_[:], in0=io64[:, 0, :], scalar1=eidf[:, tg, k:k + 1], scalar2=None, op0=ALU.is_equal), reads=[rt_b, c2b], writes=[o_b])
                        o2_, o2_b = oh.next()
                        S.op("dve", lambda e, o_=o_, o2_=o2_, tg=tg: e.tensor_tensor(out=o2_[:], in0=o_[:], in1=posall[:, tg, :], op=ALU.mult), reads=[o_b, rt_b], writes=[o2_b])
                        S.op("dve", lambda e, o2_=o2_, s_=s_, k=k: e.reduce_sum(out=s_[:, 0, k:k + 1], in_=o2_[:], axis=AX.X), reads=[o2_b], writes=[s_b])
                        S.op("dve", lambda e, o_=o_, tg=tg: e.tensor_tensor(out=o_[:], in0=o_[:], in1=wall[:, tg, :], op=ALU.mult), reads=[o_b, rt_b], writes=[o_b])
                        S.op("dve", lambda e, o_=o_, s_=s_, k=k: e.reduce_sum(out=s_[:, 1, k:k + 1], in_=o_[:], axis=AX.X), reads=[o_b], writes=[s_b])
                    u_, u_b = su.next()
                    S.op("dve", lambda e, u_=u_, s_=s_: e.tensor_copy(out=u_[:], in_=s_[:, 0, :]), reads=[s_b], writes=[u_b])
                    a_, a_b = acc.next()
                    S.op("dve", lambda e, a_=a_, ht=ht, yt=yt: e.scalar_tensor_tensor(out=a_[:], in0=ht[:], scalar=DN_ALPHA, in1=yt[:], op0=ALU.mult, op1=ALU.add), reads=[htb, ytb], writes=[a_b])
                    for k in range(8):
                        g_, g_b, g_s = gk.next()
                        S.dma("pool", lambda e, g_=g_, u_=u_, k=k: e.indirect_dma_start(out=g_[:], out_offset=None, in_=Yx, in_offset=bass.IndirectOffsetOnAxis(ap=u_[:, k:k + 1], axis=0)), g_s, reads=[u_b], writes=[g_b])
                        S.op("dve", lambda e, a_=a_, g_=g_, s_=s_, k=k: e.scalar_tensor_tensor(out=a_[:], in0=g_[:], scalar=s_[:, 1, k:k + 1], in1=a_[:], op0=ALU.mult, op1=ALU.add), reads=[g_b, s_b, a_b], writes=[a_b])
                    zs, zsb = Zs2.next()
                    for q4 in range(4):
                        S.op("dve", lambda e, zs=zs, a_=a_, q4=q4: e.bn_stats(out=zs[:, q4 * 6:(q4 + 1) * 6], in_=a_[:, q4 * 512:(q4 + 1) * 512]), reads=[a_b], writes=[zsb])
                    S.op("dve", lambda e, zs=zs: e.bn_aggr(out=zs[:, 24:26], in_=zs[:, 0:24]), reads=[zsb], writes=[zsb])
                    S.op("dve", lambda e, zs=zs: e.tensor_scalar(out=zs[:, 25:26], in0=zs[:, 25:26], scalar1=LN_EPS, scalar2=None, op0=ALU.add), reads=[zsb], writes=[zsb])
                    S.op("act", lambda e, zs=zs: e.activation(out=zs[:, 25:26], in_=zs[:, 25:26], func=AF.Sqrt), reads=[zsb], writes=[zsb])
                    S.op("dve", lambda e, zs=zs: e.reciprocal(out=zs[:, 25:26], in_=zs[:, 25:26]), reads=[zsb], writes=[zsb])
                    S.op("dve", lambda e, zs=zs, a_=a_: e.tensor_scalar(out=a_[:], in0=a_[:], scalar1=zs[:, 24:25], scalar2=zs[:, 25:26], op0=ALU.subtract, op1=ALU.mult), reads=[zsb, a_b], writes=[a_b])
                    S.op("pool", lambda e, a_=a_: e.tensor_tensor(out=a_[:], in0=a_[:], in1=ln2[:, 0, :], op=ALU.mult), reads=[a_b, l2b], writes=[a_b])
                    S.op("pool", lambda e, a_=a_: e.tensor_tensor(out=a_[:], in0=a_[:], in1=ln2[:, 1, :], op=ALU.add), reads=[a_b, l2b], writes=[a_b])
                    S.dma("sp", lambda e, a_=a_, tg=tg: e.dma_start(out=out[tg * 128:(tg + 1) * 128, :], in_=a_[:]), osr[tg % 2], reads=[a_b], writes=[])
                S.barrier()
        S.finish()
        print("instructions:", S.n_inst, flush=True)
    return nc


def _consts(p):
    half = 128
    freq = (10000.0 ** (-np.arange(half, dtype=np.float64) / half))
    cosT = np.zeros((16, 128, TB), np.float32)
    sinT = np.zeros((16, 128, TB), np.float32)
    flags = np.zeros((128, 2, 16), np.float32)
    flags[:, 1, :] = 1.0
    for blk in range(16):
        q = p - 3 + blk // 4
        if q < 0:
            continue
        pos = q * NB + (blk % 4) * TB + np.arange(TB, dtype=np.float64)
        ang = (pos[None, :].astype(np.float32) * freq[:, None].astype(np.float32)).astype(np.float32)
        cosT[blk] = np.cos(ang)
        sinT[blk] = np.sin(ang)
        if q == 0 and blk % 4 == 0:
            flags[:, 0, blk] = 1.0
            flags[:, 1, blk] = 0.0
    return cosT, sinT, flags


def kernel(x, w_in, conv_w, conv_b, lru_wa, lru_ba, lru_wi, lru_bi, lru_lambda, ret_gn_gain,
           w_lru_out, w_ret_out, b_gate, w_o, ln1_g, ln1_b, w_router, router_bias,
           w_gate_e, w_up_e, w_down_e, w_gate_s, w_up_s, w_down_s, ln2_g, ln2_b):
    import time
    _t0 = time.time()
    f = lambda a: np.ascontiguousarray(np.asarray(a, dtype=np.float32))
    x = f(x)
    w_in = f(w_in)[0]
    kp = lambda w: w.reshape(16, 128, -1).transpose(1, 0, 2)
    wx, wy = w_in[:, 0:2048], w_in[:, 2048:4096]
    wq_, wk_ = w_in[:, 4096:6144], w_in[:, 6144:8192]
    wv_, wg_ = w_in[:, 8192:12288], w_in[:, 12288:16384]
    wga, wgb = w_in[:, 16384:18432], w_in[:, 18432:20480]
    w_in_l = np.stack([np.stack([kp(wx[:, h * 128:(h + 1) * 128]), kp(wy[:, h * 128:(h + 1) * 128])], 1) for h in range(16)])
    w_in_r = np.stack([np.stack([
        kp(wq_[:, h * 256:(h + 1) * 256]), kp(wk_[:, h * 256:(h + 1) * 256]),
        kp(wv_[:, h * 512:h * 512 + 256]), kp(wv_[:, h * 512 + 256:(h + 1) * 512]),
        kp(wg_[:, h * 512:h * 512 + 256]), kp(wg_[:, h * 512 + 256:(h + 1) * 512])]) for h in range(8)])
    wlo, wro, wo_ = f(w_lru_out)[0], f(w_ret_out)[0], f(w_o)[0]
    kp2 = lambda w, n: w.reshape(n, 128, -1).transpose(1, 0, 2)
    w3 = np.stack([np.concatenate([
        kp2(wlo[:, nt * 128:(nt + 1) * 128], 16), kp2(wro[:, nt * 128:(nt + 1) * 128], 32),
        kp2(wga[:, nt * 128:(nt + 1) * 128], 16), kp2(wgb[:, nt * 128:(nt + 1) * 128], 16)], 1) for nt in range(16)])
    wo_t = np.stack([kp(wo_[:, nb * 512:(nb + 1) * 512]) for nb in range(4)])
    wge = np.concatenate([f(w_gate_e)[0], f(w_gate_s)], 0).reshape(65, 16, 128, 512).transpose(0, 2, 1, 3)
    wue = np.concatenate([f(w_up_e)[0], f(w_up_s)], 0).reshape(65, 16, 128, 512).transpose(0, 2, 1, 3)
    wde = np.concatenate([f(w_down_e)[0], f(w_down_s)], 0).reshape(65, 4, 128, 2048).transpose(0, 2, 1, 3)
    wr_t = kp(f(w_router)[0])
    chp = lambda v: f(v).reshape(16, 128).T
    cw = f(conv_w)[0]
    lrup = np.stack([chp(cw[0]), chp(cw[1]), chp(cw[2]), chp(cw[3]), chp(conv_b), chp(lru_ba), chp(lru_bi), chp(lru_lambda)], 2)
    wai = np.stack([f(lru_wa)[0].transpose(1, 0, 2), f(lru_wi)[0].transpose(1, 0, 2)], 2)
    bgv = f(b_gate)[0]
    bg = np.stack([bgv[:2048].reshape(16, 128).T, bgv[2048:].reshape(16, 128).T], 1)
    bc = lambda v, n: np.ascontiguousarray(np.broadcast_to(f(v).reshape(1, -1), (128, n)))
    lnp = np.stack([bc(ln1_g, D), bc(ln1_b, D), bc(ln2_g, D), bc(ln2_b, D)], 1)
    idx = np.arange(128, dtype=np.float64)
    dmaskT = np.zeros((128, 8, 128), np.float32)
    xizeta = np.zeros((128, 2, 8), np.float32)
    for h in range(8):
        lg = np.log1p(-np.exp2(-5.0 - h))
        diff = idx[None, :] - idx[:, None]
        dmaskT[:, h, :] = np.where(diff >= 0, np.exp(np.maximum(diff, 0) * lg), 0.0) / 16.0
        xizeta[:, 0, h] = np.exp((idx + 1.0) * lg)
        xizeta[:, 1, h] = np.exp((127.0 - idx) * lg) / 16.0
    ustrict = np.zeros((128, 2, 128), np.float32)
    ustrict[:, 0, :] = (np.arange(128)[:, None] < np.arange(128)[None, :]).astype(np.float32)
    ustrict[:, 1, :] = 1.0
    iota512 = np.broadcast_to(np.arange(512, dtype=np.float32)[None, :], (128, 512))
    iota64 = np.stack([np.broadcast_to(np.arange(64, dtype=np.float32)[None, :], (128, 64)),
                       np.broadcast_to(512.0 * np.arange(64, dtype=np.float32)[None, :], (128, 64))], 1)
    tokcol = np.zeros((128, 16, 4), np.float32)
    tokcol[:, :, 0] = np.arange(128)[:, None]
    tokcol[:, :, 1] = np.arange(16)[None, :]
    tokcol[:, :, 2] = 1.0
    shared = dict(ustrict=ustrict, iota512=iota512, iota64=iota64, tokcol=tokcol, w_in_l=w_in_l, w_in_r=w_in_r, w3=w3, wo=wo_t, wge=wge, wue=wue, wde=wde, wr=wr_t,
                  lrup=lrup, wai=wai, bg=bg, dmaskT=dmaskT, xizeta=xizeta, gain_bc=bc(ret_gn_gain, 4096),
                  lnp=lnp, rb_bc=bc(router_bias, 64), ident=np.eye(128, dtype=np.float32))
    shared = {k: np.ascontiguousarray(v, dtype=np.float32) for k, v in shared.items()}
    in_maps = []
    for c in range(8):
        b, p = divmod(c, 4)
        cosT, sinT, flags = _consts(p)
        xT = np.zeros((16, 128, 16, TB), np.float32)
        for blk in range(16):
            q = p - 3 + blk // 4
            if q < 0:
                continue
            t0 = q * NB + (blk % 4) * TB
            xT[blk] = x[b, t0:t0 + TB, :].reshape(TB, 16, 128).transpose(2, 1, 0)
        m = dict(shared)
        m.update(xT=xT, xres=np.ascontiguousarray(x[b, p * NB:(p + 1) * NB, :]), cosT=cosT, sinT=sinT, flags=flags)
        in_maps.append(m)
    print("[kernel] host layout %.1fs" % (time.time() - _t0), flush=True)
    nc = build()
    print("[kernel] build %.1fs" % (time.time() - _t0), flush=True)
    res = run_bass_kernel_spmd(nc, in_maps, core_ids=list(range(8)))
    print("[kernel] run done %.1fs" % (time.time() - _t0), flush=True)
    if os.environ.get("MK_DBG", "0") == "1":
        _LAST.clear()
        _LAST.append(res.results)
    outp = np.zeros((2, SEQ, D), np.float32)
    for c in range(8):
        b, p = divmod(c, 4)
        outp[b, p * NB:(p + 1) * NB, :] = res.results[c]["out"]
    return outp
```
